# Optimizing a Trainium2 kernel written in Bass

```python
import math
import jax, jax.numpy as jnp
from jax import lax
import numpy as np

D_MODEL = 2048
BATCH = 8
SEQ = 4096
DEPTH = 4

HEAD_DIM = 64
EPS = 1e-6
A_HEADS = 4
A_DK = 128
A_DV = 128
A_CHUNK = 64
A_OUT = A_HEADS * A_DV
B_HEADS = 8
B_WIDTH = B_HEADS * HEAD_DIM
SB_BLOCK = 128
C_PATTERNS = ((128, 1), (512, 4), (2048, 16))
C_GROUPS = 3
C_HPG = 4
C_HEADS = C_GROUPS * C_HPG
C_WIDTH = C_HEADS * HEAD_DIM
C_OUT = C_HPG * HEAD_DIM
C_BLOCK = 128
D_HEADS = 8
D_KV_HEADS = 2
D_GROUP = D_HEADS // D_KV_HEADS
D_WINDOW = 128
D_BLOCK = 128
D_Q = D_HEADS * HEAD_DIM
D_KV = D_KV_HEADS * HEAD_DIM
REL_BUCKETS = 32
REL_MAX_DIST = 2048
REL_HEADS = C_HEADS + D_HEADS
D_FF = 5632
MIX_SIZES = (A_HEADS * A_DK, A_HEADS * A_DK, A_OUT, A_OUT,
             B_WIDTH, B_WIDTH, B_WIDTH,
             C_WIDTH, C_WIDTH, C_WIDTH,
             D_Q, D_KV, D_KV)
MIX_IN = sum(MIX_SIZES)
N_BRANCH = 4
IN_TOTAL = MIX_IN + N_BRANCH * D_MODEL
BRANCH_WIDTHS = (A_OUT, B_WIDTH, C_OUT, D_Q)
BR_TOTAL = sum(BRANCH_WIDTHS)

kernel_name = 'hybrid_gated_parallel_mixers_trunk'


def _rms_norm(x, g):
    xf = x.astype(jnp.float32)
    y = xf * lax.rsqrt(jnp.mean(xf * xf, axis=-1, keepdims=True) + EPS)
    return (y * g.astype(jnp.float32)).astype(x.dtype)


def _swiglu(x, w13, w2):
    gate, up = jnp.split(x @ w13, 2, axis=-1)
    return (jax.nn.silu(gate) * up) @ w2


def _rel_bucket(dist):
    max_exact = REL_BUCKETS // 2
    d = jnp.maximum(dist, 1).astype(jnp.float32)
    large = max_exact + (jnp.log(d / max_exact) / math.log(REL_MAX_DIST / max_exact)
                         * (REL_BUCKETS - max_exact)).astype(jnp.int32)
    large = jnp.minimum(large, REL_BUCKETS - 1)
    return jnp.where(dist < max_exact, dist, large)


def _with_prev_block(t, nb_axis):
    pad = [(0, 0)] * t.ndim
    pad[nb_axis] = (1, 0)
    prev = lax.slice_in_dim(jnp.pad(t, pad), 0, t.shape[nb_axis], axis=nb_axis)
    return jnp.concatenate([prev, t], axis=nb_axis + 1)


def _hgrn2(q, f_logit, i, g, lb, norm_g):
    Bn, T, _ = q.shape
    nc = T // A_CHUNK
    f32 = jnp.float32
    f = lb + (1.0 - lb) * jax.nn.sigmoid(f_logit.astype(f32))

    def chunks(t):
        return t.astype(f32).reshape(Bn, nc, A_CHUNK, A_HEADS, -1).transpose(1, 0, 3, 2, 4)

    qc, kc, vc, gc = chunks(q), chunks(1.0 - f), chunks(i), chunks(jnp.log(f))
    causal = jnp.tril(jnp.ones((A_CHUNK, A_CHUNK), dtype=bool))[:, :, None]

    def step(S, inp):
        qb, kb, vb, gb = inp
        b = jnp.cumsum(gb, axis=2)
        b_last = b[:, :, -1:, :]
        o_inter = jnp.einsum('bhtk,bhkv->bhtv', qb * jnp.exp(b), S)
        decay = jnp.exp(jnp.where(causal, b[:, :, :, None, :] - b[:, :, None, :, :], -jnp.inf))
        scores = jnp.einsum('bhtk,bhsk,bhtsk->bhts', qb, kb, decay)
        o = o_inter + jnp.einsum('bhts,bhsv->bhtv', scores, vb)
        S = (jnp.exp(b_last[:, :, 0, :, None]) * S
             + jnp.einsum('bhsk,bhsv->bhkv', kb * jnp.exp(b_last - b), vb))
        return S, o

    S0 = jnp.zeros((Bn, A_HEADS, A_DK, A_DV), f32)
    _, o = lax.scan(step, S0, (qc, kc, vc, gc))
    o = o.transpose(1, 0, 3, 2, 4).reshape(Bn, T, A_HEADS, A_DV)
    o = _rms_norm(o, norm_g) * jax.nn.silu(g.astype(f32).reshape(Bn, T, A_HEADS, A_DV))
    return o.reshape(Bn, T, A_OUT).astype(q.dtype)


def _stick_breaking(q, k, v):
    Bn, T, _ = q.shape
    nb = T // SB_BLOCK
    scale = HEAD_DIM ** -0.5
    qh = q.reshape(Bn, nb, SB_BLOCK, B_HEADS, HEAD_DIM).transpose(1, 0, 3, 2, 4)
    kh = k.reshape(Bn, T, B_HEADS, HEAD_DIM)
    vh = v.reshape(Bn, T, B_HEADS, HEAD_DIM)
    key_pos = jnp.arange(T)

    def block(args):
        qb, blk = args
        z = jnp.einsum('bhqd,bshd->bhqs', qb, kh).astype(jnp.float32) * scale
        q_pos = blk * SB_BLOCK + jnp.arange(SB_BLOCK)
        past = key_pos[None, :] < q_pos[:, None]
        log_keep = jnp.where(past, -jax.nn.softplus(z), 0.0)
        between = lax.cumsum(log_keep, axis=3, reverse=True) - log_keep
        a = jnp.where(past, jnp.exp(jax.nn.log_sigmoid(z) + between), 0.0)
        return jnp.einsum('bhqs,bshd->bqhd', a.astype(v.dtype), vh)

    o = lax.map(block, (qh, jnp.arange(nb)))
    return o.transpose(1, 0, 2, 3, 4).reshape(Bn, T, B_WIDTH)


def _dilated_group(q, k, v, bias_tab, window, dilation):
    Bn, T, H, d = q.shape
    span = C_BLOCK * dilation
    Tp = -(-T // span) * span
    L = Tp // dilation
    nb = L // C_BLOCK

    def to_sub(t):
        t = jnp.pad(t, ((0, 0), (0, Tp - T), (0, 0), (0, 0)))
        t = t.reshape(Bn, L, dilation, H, d).transpose(0, 2, 1, 3, 4)
        return t.reshape(Bn, dilation, nb, C_BLOCK, H, d)

    def from_sub(t):
        rest = t.shape[4:]
        t = jnp.moveaxis(t.reshape((Bn, dilation, L) + rest), 1, 2)
        return t.reshape((Bn, Tp) + rest)[:, :T]

    qb = to_sub(q)
    kk = _with_prev_block(to_sub(k), 2)
    vv = _with_prev_block(to_sub(v), 2)
    qi = jnp.arange(C_BLOCK)[:, None] + C_BLOCK
    kj = jnp.arange(2 * C_BLOCK)[None, :]
    dist = qi - kj
    valid = (dist >= 0) & (dist <= window // dilation)
    valid = valid[None] & ((jnp.arange(nb)[:, None, None] > 0) | (kj[None] >= C_BLOCK))
    bias = bias_tab[_rel_bucket(jnp.maximum(dist, 0) * dilation)].transpose(2, 0, 1)
    logits = jnp.einsum('brnqhd,brnkhd->brnhqk', qb, kk).astype(jnp.float32) * HEAD_DIM ** -0.5
    logits = jnp.where(valid[None, None, :, None], logits + bias.astype(jnp.float32), -jnp.inf)
    m = jnp.max(logits, axis=-1, keepdims=True)
    p = jnp.exp(logits - m)
    den = jnp.sum(p, axis=-1)
    o = jnp.einsum('brnhqk,brnkhd->brnqhd', p, vv.astype(jnp.float32))
    o = o / jnp.swapaxes(den, -1, -2)[..., None]
    lse = jnp.swapaxes(m[..., 0] + jnp.log(den), -1, -2)
    return from_sub(o), from_sub(lse)


def _dilated_mixture(q, k, v, bias_tab):
    Bn, T, _ = q.shape
    q = q.reshape(Bn, T, C_GROUPS, C_HPG, HEAD_DIM)
    k = k.reshape(Bn, T, C_GROUPS, C_HPG, HEAD_DIM)
    v = v.reshape(Bn, T, C_GROUPS, C_HPG, HEAD_DIM)
    outs, lses = [], []
    for g, (window, dilation) in enumerate(C_PATTERNS):
        o, lse = _dilated_group(q[:, :, g], k[:, :, g], v[:, :, g],
                                bias_tab[:, g * C_HPG:(g + 1) * C_HPG], window, dilation)
        outs.append(o)
        lses.append(lse)
    w = jax.nn.softmax(jnp.stack(lses), axis=0)
    o = jnp.sum(w[..., None] * jnp.stack(outs), axis=0)
    return o.reshape(Bn, T, C_OUT).astype(q.dtype)


def _swa_sinks(q, k, v, sinks, bias_tab):
    Bn, T, _ = q.shape
    nb = T // D_BLOCK
    qb = q.reshape(Bn, nb, D_BLOCK, D_KV_HEADS, D_GROUP, HEAD_DIM)
    kk = _with_prev_block(k.reshape(Bn, nb, D_BLOCK, D_KV_HEADS, HEAD_DIM), 1)
    vv = _with_prev_block(v.reshape(Bn, nb, D_BLOCK, D_KV_HEADS, HEAD_DIM), 1)
    qi = jnp.arange(D_BLOCK)[:, None] + D_BLOCK
    kj = jnp.arange(2 * D_BLOCK)[None, :]
    dist = qi - kj
    valid = (dist >= 0) & (dist < D_WINDOW)
    valid = valid[None] & ((jnp.arange(nb)[:, None, None] > 0) | (kj[None] >= D_BLOCK))
    bias = bias_tab[_rel_bucket(jnp.maximum(dist, 0))].transpose(2, 0, 1)
    bias = bias.reshape(D_KV_HEADS, D_GROUP, D_BLOCK, 2 * D_BLOCK).astype(jnp.float32)
    logits = jnp.einsum('bnqkgd,bnskd->bnkgqs', qb, kk).astype(jnp.float32) * HEAD_DIM ** -0.5
    logits = jnp.where(valid[None, :, None, None], logits + bias, -jnp.inf)
    s = sinks.astype(jnp.float32).reshape(D_KV_HEADS, D_GROUP, 1, 1)
    m = jnp.maximum(jnp.max(logits, axis=-1, keepdims=True), s)
    p = jnp.exp(logits - m)
    den = jnp.sum(p, axis=-1, keepdims=True) + jnp.exp(s - m)
    o = jnp.einsum('bnkgqs,bnskd->bnqkgd', p / den, vv.astype(jnp.float32))
    return o.reshape(Bn, T, D_Q).astype(q.dtype)


def _token_mixing(u, w_in, lb, hgrn_norm_g, sinks, w_branch, w_out, rel_bias):
    proj = u @ w_in[:, :MIX_IN]
    (a_q, a_f, a_i, a_g, b_q, b_k, b_v, c_q, c_k, c_v, d_q, d_k, d_v) = jnp.split(
        proj, np.cumsum(MIX_SIZES)[:-1].tolist(), axis=-1)
    ys = (_hgrn2(a_q, a_f, a_i, a_g, lb, hgrn_norm_g),
          _stick_breaking(b_q, b_k, b_v),
          _dilated_mixture(c_q, c_k, c_v, rel_bias[:, :C_HEADS]),
          _swa_sinks(d_q, d_k, d_v, sinks, rel_bias[:, C_HEADS:]))
    merged = None
    row = 0
    for idx, (y, width) in enumerate(zip(ys, BRANCH_WIDTHS)):
        col = MIX_IN + idx * D_MODEL
        gate = jax.nn.sigmoid(u @ w_in[:, col:col + D_MODEL])
        term = gate * (y @ w_branch[row:row + width])
        merged = term if merged is None else merged + term
        row += width
    return merged @ w_out


def setup_inputs(seed: int = 0) -> dict:
    key = jax.random.key(seed)
    ks = jax.random.split(key, 16)
    D = D_MODEL

    def normal(k, shape, scale):
        return jax.random.normal(k, shape, jnp.float32) * scale

    br_scale = jnp.concatenate([jnp.full((w,), w ** -0.5, jnp.float32) for w in BRANCH_WIDTHS])
    return {
        'x': normal(ks[0], (BATCH, SEQ, D), 1.0),
        'ffn1_norm': 1.0 + normal(ks[1], (DEPTH, 2, D), 0.05),
        'ffn1_w13': normal(ks[2], (DEPTH, D, 2 * D_FF), D ** -0.5),
        'ffn1_w2': normal(ks[3], (DEPTH, D_FF, D), D_FF ** -0.5),
        'mix_norm': 1.0 + normal(ks[4], (DEPTH, 2, D), 0.05),
        'w_in': normal(ks[5], (DEPTH, D, IN_TOTAL), D ** -0.5),
        'hgrn_lb_logits': normal(ks[6], (DEPTH, A_HEADS * A_DK), 0.1),
        'hgrn_out_norm': 1.0 + normal(ks[7], (DEPTH, A_DV), 0.05),
        'attn_sinks': normal(ks[8], (DEPTH, D_HEADS), 0.5),
        'w_branch': normal(ks[9], (DEPTH, BR_TOTAL, D), 1.0) * br_scale[None, :, None],
        'w_out': normal(ks[10], (DEPTH, D, D), D ** -0.5),
        'ffn2_norm': 1.0 + normal(ks[11], (DEPTH, 2, D), 0.05),
        'ffn2_w13': normal(ks[12], (DEPTH, D, 2 * D_FF), D ** -0.5),
        'ffn2_w2': normal(ks[13], (DEPTH, D_FF, D), D_FF ** -0.5),
        'rel_bias': normal(ks[14], (REL_BUCKETS, REL_HEADS), 0.5),
    }


def reference(x, ffn1_norm, ffn1_w13, ffn1_w2, mix_norm, w_in, hgrn_lb_logits, hgrn_out_norm,
              attn_sinks, w_branch, w_out, ffn2_norm, ffn2_w13, ffn2_w2, rel_bias):
    lb_sm = jax.nn.softmax(hgrn_lb_logits.astype(jnp.float32), axis=0)
    lb_all = jnp.cumsum(lb_sm, axis=0) - lb_sm[0:1]
    h = x
    for l in range(DEPTH):
        f1 = _swiglu(_rms_norm(h, ffn1_norm[l, 0]), ffn1_w13[l], ffn1_w2[l])
        h = h + 0.5 * _rms_norm(f1, ffn1_norm[l, 1])
        u = _rms_norm(h, mix_norm[l, 0])
        mix = _token_mixing(u, w_in[l], lb_all[l], hgrn_out_norm[l], attn_sinks[l],
                            w_branch[l], w_out[l], rel_bias)
        h = h + _rms_norm(mix, mix_norm[l, 1])
        f2 = _swiglu(_rms_norm(h, ffn2_norm[l, 0]), ffn2_w13[l], ffn2_w2[l])
        h = h + 0.5 * _rms_norm(f2, ffn2_norm[l, 1])
    return h
```

```python
import numpy as np
import concourse.bass as bass
import concourse.mybir as mybir
from concourse.bass_utils import run_bass_kernel_spmd
from contextlib import ExitStack

F32 = mybir.dt.float32
BF16 = mybir.dt.bfloat16
AF = mybir.ActivationFunctionType
ALU = mybir.AluOpType

import os
SERIAL_PH = [int(x) for x in os.environ.get('MK_SERIAL', '').split(',') if x]
SEM_LIMIT = 30000
NDMA_SEMS = 10


class Buf:
    __slots__ = ("name", "writers", "readers", "t", "lock")

    def __init__(self, name, t=None, lock=None):
        self.name = name
        self.writers = []
        self.readers = []
        self.t = t
        self.lock = lock

    def __getitem__(self, k):
        return self.t[k]


class Op:
    __slots__ = ("eng", "fn", "deps", "is_dma", "needs_inc", "sem", "val", "clock", "idx", "phase")


def _ba(x):
    if isinstance(x, Buf):
        return x, x.t[:]
    return x


class Sched:
    COMPUTE = ("pe", "act", "dve", "pool")
    ALL = ("pe", "act", "dve", "pool", "sp")

    def __init__(self, nc):
        self.nc = nc
        self.ops = []
        self.engs = {"pe": nc.tensor, "act": nc.scalar, "dve": nc.vector,
                     "pool": nc.gpsimd, "sp": nc.sync}
        self.phase = 0
        self.nops = 0
        self.last = {}
        self.dmas = []
        self.clocks = {e: {} for e in self.ALL}
        self.cur_sem = {}
        self.cur_cnt = {}
        self.nsw = {}
        for e in self.COMPUTE:
            self.cur_sem[e] = nc.alloc_semaphore("s_%s_0" % e)
            self.cur_cnt[e] = 0
            self.nsw[e] = 0
        self.dma_sems = {}
        self.dma_cnt = {}
        self.dma_last = {}
        self.dma_rr = {}
        self.nwaits = 0
        self.ninst = {e: 0 for e in self.ALL}
        self.es = None

    def begin_phase(self):
        self.es = ExitStack()
        self.es.__enter__()

    def sb(self, name, shape, dt):
        t = self.es.enter_context(self.nc.sbuf_tensor("%s_p%d" % (name, self.phase), list(shape), dt))
        return Buf(name, t)

    def ps(self, name, shape, dt=F32):
        t = self.es.enter_context(self.nc.psum_tensor("%s_p%d" % (name, self.phase), list(shape), dt))
        b = Buf(name, t)
        b.lock = Buf(name + "_lock")
        return b

    def end_phase(self):
        self.barrier()
        self.emit()
        self.es.__exit__(None, None, None)
        self.es = None
        self.phase += 1

    def op(self, eng, name, kw, reads=(), writes=(), dma=False, disjoint=False):
        o = Op()
        o.eng = eng
        o.fn = (name, kw)
        o.is_dma = dma
        o.needs_inc = dma
        o.sem = None
        o.val = 0
        o.clock = None
        o.idx = self.nops
        o.phase = self.phase
        self.nops += 1
        deps = {}
        locks = []
        for b in reads:
            if b.lock is not None and b.lock not in locks:
                locks.append(b.lock)
        for b in writes:
            if b.lock is not None and b.lock not in locks:
                locks.append(b.lock)
        for b in locks:
            for w in b.writers:
                deps[w.idx] = w
        for b in reads:
            for w in b.writers:
                deps[w.idx] = w
        for b in writes:
            for r in b.readers:
                deps[r.idx] = r
            if (not disjoint) or b.readers:
                for w in b.writers:
                    deps[w.idx] = w
        ph = self.phase
        SERIAL = (ph in SERIAL_PH) or (-1 in SERIAL_PH)
        if SERIAL and self.ops:
            po = self.ops[-1]
            if po.fn is not None:
                deps[po.idx] = po
        if SERIAL:
            o.deps = [d for d in deps.values() if d.phase == ph]
        elif not dma:
            raw = set()
            if eng != "pe":
                for b in reads:
                    for w in b.writers:
                        if (not w.is_dma) and w.eng == eng:
                            raw.add(w.idx)
            o.deps = [d for d in deps.values() if d.phase == ph and (d.is_dma or d.eng != eng or d.idx in raw)]
        else:
            o.deps = [d for d in deps.values() if d.phase == ph]
        for d in o.deps:
            d.needs_inc = True
        for b in reads:
            if not dma:
                b.readers = [r for r in b.readers if r.is_dma or r.eng != eng]
            b.readers.append(o)
        for b in writes:
            if b.readers:
                b.writers = [o]
                b.readers = []
            elif disjoint:
                if not dma:
                    b.writers = [w for w in b.writers if w.is_dma or w.eng != eng]
                b.writers.append(o)
            else:
                b.writers = [o]
        for b in locks:
            b.writers = [o]
        self.ops.append(o)
        if dma:
            self.dmas.append(o)
        else:
            self.last[eng] = o
        return o

    def barrier(self):
        lasts = [o for o in self.last.values() if o.phase == self.phase]
        dmas = self.dmas
        self.dmas = []
        self.last = {}
        for o in lasts:
            o.needs_inc = True
        for e in self.ALL:
            o = Op()
            o.eng = e
            o.fn = None
            o.is_dma = False
            o.needs_inc = False
            o.sem = None
            o.val = 0
            o.clock = None
            o.idx = self.nops
            o.phase = self.phase
            self.nops += 1
            o.deps = [l for l in lasts if l.eng != e] + dmas
            self.ops.append(o)

    def emit(self):
        nc = self.nc
        for o in self.ops:
            e = o.eng
            eng = self.engs[e]
            clk = self.clocks[e]
            deps = list(o.deps)
            slot = None
            if o.is_dma:
                if e not in self.dma_sems:
                    self.dma_sems[e] = [nc.alloc_semaphore("d_%s_%d" % (e, i)) for i in range(NDMA_SEMS)]
                    self.dma_cnt[e] = [0] * NDMA_SEMS
                    self.dma_last[e] = [None] * NDMA_SEMS
                    self.dma_rr[e] = 0
                slot = self.dma_rr[e] % NDMA_SEMS
                self.dma_rr[e] += 1
                if self.dma_last[e][slot] is not None:
                    deps.append(self.dma_last[e][slot])
            if len(deps) > 1:
                deps.sort(key=lambda d: -d.idx)
            for d in deps:
                k = id(d.sem)
                if clk.get(k, (None, 0))[1] >= d.val:
                    continue
                eng.wait_ge(d.sem, d.val)
                self.nwaits += 1
                for kk, vv in d.clock.items():
                    if clk.get(kk, (None, 0))[1] < vv[1]:
                        clk[kk] = vv
            if o.fn is None:
                continue
            ins = getattr(eng, o.fn[0])(**o.fn[1])
            self.ninst[e] += 1
            if o.is_dma:
                sem = self.dma_sems[e][slot]
                self.dma_cnt[e][slot] += 16
                o.sem = sem
                o.val = self.dma_cnt[e][slot]
                ins.then_inc(sem, 16)
                self.dma_last[e][slot] = o
                c = dict(clk)
                c[id(sem)] = (sem, o.val)
                o.clock = c
            elif o.needs_inc:
                if self.cur_cnt[e] >= SEM_LIMIT:
                    self.nsw[e] += 1
                    self.cur_sem[e] = nc.alloc_semaphore("s_%s_%d" % (e, self.nsw[e]))
                    self.cur_cnt[e] = 0
                self.cur_cnt[e] += 1
                o.sem = self.cur_sem[e]
                o.val = self.cur_cnt[e]
                ins.then_inc(o.sem, 1)
                c = dict(clk)
                c[id(o.sem)] = (o.sem, o.val)
                o.clock = c
            o.fn = None
            o.deps = None
        self.ops = []

    def mm(self, out, lhsT, rhs, start=True, stop=True):
        ob, oa = _ba(out)
        lb, la = _ba(lhsT)
        rb, ra = _ba(rhs)
        return self.op("pe", "matmul", dict(out=oa, lhsT=la, rhs=ra, start=start, stop=stop),
                       reads=[lb, rb], writes=[ob], disjoint=True if not start else False)

    def tr(self, out, in_, ident):
        ob, oa = _ba(out)
        ib, ia = _ba(in_)
        db, da = _ba(ident)
        return self.op("pe", "transpose", dict(out=oa, in_=ia, identity=da), reads=[ib, db], writes=[ob], disjoint=True)

    def actv(self, out, in_, func, scale=1.0, bias=None, disjoint=False, eng="act"):
        ob, oa = _ba(out)
        ib, ia = _ba(in_)
        kw = dict(out=oa, in_=ia, func=func)
        reads = [ib]
        if isinstance(scale, tuple) or isinstance(scale, Buf):
            sb_, sa = _ba(scale)
            kw["scale"] = sa
            reads.append(sb_)
        elif scale != 1.0:
            kw["scale"] = float(scale)
        if bias is not None:
            if isinstance(bias, (tuple, Buf)):
                bb, ba = _ba(bias)
                kw["bias"] = ba
                reads.append(bb)
            else:
                kw["bias"] = float(bias)
        return self.op("act", "activation", kw, reads=reads, writes=[ob], disjoint=disjoint)

    def tt(self, eng, out, in0, in1, op, disjoint=False):
        ob, oa = _ba(out)
        ab, aa = _ba(in0)
        bb, ba = _ba(in1)
        return self.op(eng, "tensor_tensor", dict(out=oa, in0=aa, in1=ba, op=op), reads=[ab, bb], writes=[ob], disjoint=disjoint)

    def ts(self, eng, out, in0, s1, s2=None, op0=ALU.mult, op1=None, disjoint=False):
        ob, oa = _ba(out)
        ab, aa = _ba(in0)
        reads = [ab]
        kw = dict(out=oa, in0=aa, op0=op0)
        if isinstance(s1, (tuple, Buf)):
            b_, a_ = _ba(s1)
            reads.append(b_)
            kw["scalar1"] = a_
        else:
            kw["scalar1"] = float(s1)
        if s2 is None:
            kw["scalar2"] = None
        elif isinstance(s2, (tuple, Buf)):
            b_, a_ = _ba(s2)
            reads.append(b_)
            kw["scalar2"] = a_
        else:
            kw["scalar2"] = float(s2)
        if op1 is not None:
            kw["op1"] = op1
        return self.op(eng, "tensor_scalar", kw, reads=reads, writes=[ob], disjoint=disjoint)

    def stt(self, out, in0, scalar, in1, op0, op1, disjoint=False):
        ob, oa = _ba(out)
        ab, aa = _ba(in0)
        bb, ba = _ba(in1)
        reads = [ab, bb]
        if isinstance(scalar, (tuple, Buf)):
            b_, a_ = _ba(scalar)
            reads.append(b_)
            sc = a_
        else:
            sc = float(scalar)
        return self.op("dve", "scalar_tensor_tensor", dict(out=oa, in0=aa, scalar=sc, in1=ba, op0=op0, op1=op1),
                       reads=reads, writes=[ob], disjoint=disjoint)

    def copy(self, eng, out, in_, disjoint=False):
        ob, oa = _ba(out)
        ib, ia = _ba(in_)
        if eng == "act":
            return self.op("act", "activation", dict(out=oa, in_=ia, func=AF.Copy), reads=[ib], writes=[ob], disjoint=disjoint)
        return self.op(eng, "tensor_copy", dict(out=oa, in_=ia), reads=[ib], writes=[ob], disjoint=disjoint)

    def recip(self, out, in_, disjoint=False):
        ob, oa = _ba(out)
        ib, ia = _ba(in_)
        return self.op("dve", "reciprocal", dict(out=oa, in_=ia), reads=[ib], writes=[ob], disjoint=disjoint)

    def memset(self, eng, out, val, disjoint=False):
        ob, oa = _ba(out)
        return self.op(eng, "memset", dict(ap=oa, constant=float(val)), writes=[ob], disjoint=disjoint)

    def dma(self, out, in_, eng="sp", disjoint=True):
        reads, writes = [], []
        if isinstance(out, (tuple, Buf)):
            ob, oa = _ba(out)
            writes.append(ob)
        else:
            oa = out
        if isinstance(in_, (tuple, Buf)):
            ib, ia = _ba(in_)
            reads.append(ib)
        else:
            ia = in_
        return self.op(eng, "dma_start", dict(out=oa, in_=ia), reads=reads, writes=writes, dma=True, disjoint=disjoint)


D = 2048
KC = 16
FF = 5632
FC = 44
TT = 512
EPS = 1e-6
MIXC = 52
NVEC = 104

R_AQ, R_AF, R_AI, R_AG = 0, 512, 1024, 1536
R_BQ, R_BK, R_BV = 2048, 2560, 3072
R_CQ, R_CK, R_CV = 3584, 4352, 5120
R_DQ, R_DK, R_DV = 5888, 6400, 6528
Y_A, Y_B, Y_C, Y_D = 0, 512, 1024, 1280


class G:
    pass


def load_vec(S, g, l):
    vec = S.sb("vec", [128, NVEC + 48], F32)
    S.dma((vec, vec[:, 0:NVEC]), g.vecs[l])
    S.ts("dve", (vec, vec[:, NVEC:NVEC + 16]), (vec, vec[:, 16:32]), 0.5)
    S.ts("dve", (vec, vec[:, NVEC + 16:NVEC + 32]), (vec, vec[:, 80:96]), 0.5)
    return vec


def phase_consts(S, g):
    ones = S.sb("ones", [128, 128], F32)
    S.memset("dve", ones, 1.0)
    epsb = S.sb("epsb", [128, 1], F32)
    S.memset("dve", epsb, EPS)
    return ones, epsb


def rms_rstd(S, ss_ps, rstd, tmp, epsb, n):
    S.actv(tmp, ss_ps, AF.Sqrt, scale=1.0 / n, bias=(epsb, epsb[:, 0:1]))
    S.recip(rstd, tmp)


def prenorm_tile(S, g, hsrc, tok, bufX, xn, sq, tmp, rstd, ones, epsb, ssb, vec, gcol):
    srcv = hsrc.rearrange("(kc p) t -> p kc t", p=128)
    S.dma(bufX, srcv[:, :, tok], disjoint=False)
    for kc in range(KC):
        q = sq[kc % 2]
        S.actv(q, (bufX, bufX[:, kc, :]), AF.Square)
        S.mm(ssb, ones, q, start=(kc == 0), stop=(kc == KC - 1))
    rms_rstd(S, ssb, rstd, tmp, epsb, D)
    for kc in range(KC):
        S.stt((xn, xn[:, kc, :]), (bufX, bufX[:, kc, :]), (vec, vec[:, gcol + kc:gcol + kc + 1]), rstd,
              ALU.mult, ALU.mult, disjoint=True)


def postnorm_residual(S, g, hsrc, hdst, tok, bufX, rstd, tmp, epsb, ssb, vec, gcol, hre, hout):
    rms_rstd(S, ssb, rstd, tmp, epsb, D)
    for dc in range(KC):
        hr = hre[dc % 2]
        ho = hout[dc % 2]
        S.dma(hr, hsrc[dc * 128:(dc + 1) * 128, tok], disjoint=False)
        S.stt(ho, (bufX, bufX[:, dc, :]), (vec, vec[:, gcol + dc:gcol + dc + 1]), rstd, ALU.mult, ALU.mult)
        S.tt("dve", ho, ho, hr, ALU.add)
        S.dma(hdst[dc * 128:(dc + 1) * 128, tok], ho)


def ffn_phase(S, g, l, which, hsrc, hdst):
    NT = g.T // TT
    w13t = g.w13t[which][l]
    w2t = g.w2t[which][l]
    S.begin_phase()
    ones, epsb = phase_consts(S, g)
    vec = load_vec(S, g, l)
    gpre = 0 if which == 0 else 64
    gpost = NVEC if which == 0 else NVEC + 16
    bufX = S.sb("bufX", [128, KC, TT], F32)
    xn = S.sb("xn", [128, KC, TT], BF16)
    act = S.sb("actT", [128, FC, TT], BF16)
    w13b = [S.sb("w13b%d" % i, [128, KC, 128], BF16) for i in range(6)]
    w2b = [S.sb("w2b%d" % i, [128, FC, 128], BF16) for i in range(2)]
    sq = [S.sb("sq%d" % i, [128, TT], F32) for i in range(2)]
    tmp = [S.sb("tmp%d" % i, [128, TT], F32) for i in range(2)]
    rstd = S.sb("rstd", [128, TT], F32)
    hre = [S.sb("hre%d" % i, [128, TT], F32) for i in range(2)]
    hout = [S.sb("hout%d" % i, [128, TT], F32) for i in range(2)]
    psb = [S.ps("psb%d" % i, [128, 512], F32) for i in range(7)]
    ssb = psb[6]
    n13 = 0
    n2 = 0
    for tt in range(NT):
        tok = slice(tt * TT, (tt + 1) * TT)
        prenorm_tile(S, g, hsrc, tok, bufX, xn, sq, tmp[0], rstd, ones, epsb, ssb, vec, gpre)
        for j in range(FC):
            wb = []
            for half in range(2):
                wbf = w13b[n13 % 6]
                n13 += 1
                S.dma((wbf, wbf[:].rearrange("p a b -> p (a b)")), w13t[half * FC + j], eng="pool", disjoint=False)
                wb.append(wbf)
            pg = psb[(j % 2) * 2]
            pu = psb[(j % 2) * 2 + 1]
            for half, pp in ((0, pg), (1, pu)):
                for kc in range(KC):
                    S.mm(pp, (wb[half], wb[half][:, kc, :]), (xn, xn[:, kc, :]), start=(kc == 0), stop=(kc == KC - 1))
            tm = tmp[j % 2]
            S.actv(tm, pg, AF.Silu)
            S.tt("dve", (act, act[:, j, :]), tm, pu, ALU.mult, disjoint=True)
        for dc in range(KC):
            wbf = w2b[n2 % 2]
            n2 += 1
            for q in range(4):
                S.dma((wbf, wbf[:, q * 11:(q + 1) * 11, :].rearrange("p a b -> p (a b)")),
                      w2t[dc][:, q * 1408:(q + 1) * 1408], eng="pool")
            po = psb[4 + (dc % 2)]
            for fc in range(FC):
                S.mm(po, (wbf, wbf[:, fc, :]), (act, act[:, fc, :]), start=(fc == 0), stop=(fc == FC - 1))
            S.actv((bufX, bufX[:, dc, :]), po, AF.Copy, disjoint=True)
            q_ = sq[dc % 2]
            S.actv(q_, po, AF.Square)
            S.mm(ssb, ones, q_, start=(dc == 0), stop=(dc == KC - 1))
        postnorm_residual(S, g, hsrc, hdst, tok, bufX, rstd, tmp[0], epsb, ssb, vec, gpost, hre, hout)
    S.end_phase()


def inproj_phase(S, g, l, h):
    NT = g.T // TT
    S.begin_phase()
    ones, epsb = phase_consts(S, g)
    vec = load_vec(S, g, l)
    bufX = S.sb("bufX", [128, KC, TT], F32)
    xn = S.sb("xn", [128, KC, TT], BF16)
    wb = [S.sb("wb%d" % i, [128, KC, 128], BF16) for i in range(6)]
    sq = [S.sb("sq%d" % i, [128, TT], F32) for i in range(2)]
    tmp = S.sb("tmp", [128, TT], F32)
    rstd = S.sb("rstd", [128, TT], F32)
    ost = [S.sb("ost%d" % i, [128, TT], BF16) for i in range(4)]
    psb = [S.ps("psb%d" % i, [128, 512], F32) for i in range(5)]
    ssb = psb[4]
    nw = 0
    for tt in range(NT):
        tok = slice(tt * TT, (tt + 1) * TT)
        prenorm_tile(S, g, h, tok, bufX, xn, sq, tmp, rstd, ones, epsb, ssb, vec, 32)
        S.dma(g.uT.rearrange("(kc p) t -> p kc t", p=128)[:, :, tok], xn)
        for c in range(MIXC):
            w = wb[nw % 6]
            S.dma((w, w[:].rearrange("p a b -> p (a b)")), g.wint[l][c], eng="pool", disjoint=False)
            pp = psb[nw % 4]
            o = ost[nw % 4]
            for kc in range(KC):
                S.mm(pp, (w, w[:, kc, :]), (xn, xn[:, kc, :]), start=(kc == 0), stop=(kc == KC - 1))
            S.copy("act" if nw % 2 == 0 else "dve", o, pp)
            S.dma(g.projT[c * 128:(c + 1) * 128, tok], o)
            nw += 1
    S.end_phase()


BR_K = (4, 4, 2, 4)


def merge_phase(S, g, l, h):
    NT = g.T // TT
    S.begin_phase()
    ones, epsb = phase_consts(S, g)
    vec = load_vec(S, g, l)
    bufX = S.sb("bufX", [128, KC, TT], F32)
    uT = S.sb("uT", [128, KC, TT], BF16)
    yT = S.sb("yT", [128, 14, TT], BF16)
    mg = S.sb("mg", [128, KC, TT], BF16)
    wg = [S.sb("wg%d" % i, [128, KC, 128], BF16) for i in range(6)]
    wbr = [S.sb("wbr%d" % i, [128, 14, 128], BF16) for i in range(2)]
    wo = [S.sb("wo%d" % i, [128, KC, 128], BF16) for i in range(2)]
    sg = [S.sb("sg%d" % i, [128, TT], F32) for i in range(2)]
    acc = S.sb("acc", [128, TT], F32)
    t2 = S.sb("t2", [128, TT], F32)
    sq = [S.sb("sq%d" % i, [128, TT], F32) for i in range(2)]
    tmp = S.sb("tmp", [128, TT], F32)
    rstd = S.sb("rstd", [128, TT], F32)
    hre = [S.sb("hre%d" % i, [128, TT], F32) for i in range(2)]
    hout = [S.sb("hout%d" % i, [128, TT], F32) for i in range(2)]
    psb = [S.ps("psb%d" % i, [128, 512], F32) for i in range(7)]
    ssb = psb[6]
    ng = 0
    nb = 0
    no = 0
    for tt in range(NT):
        tok = slice(tt * TT, (tt + 1) * TT)
        S.dma(uT, g.uT.rearrange("(kc p) t -> p kc t", p=128)[:, :, tok], disjoint=False)
        S.dma(yT, g.yT.rearrange("(rc p) t -> p rc t", p=128)[:, :, tok], disjoint=False)
        for dc in range(KC):
            wbt = wbr[nb % 2]
            nb += 1
            S.dma((wbt, wbt[:].rearrange("p a b -> p (a b)")), g.wbt[l][dc], eng="pool", disjoint=False)
            rc0 = 0
            for i in range(4):
                w = wg[ng % 6]
                S.dma((w, w[:].rearrange("p a b -> p (a b)")), g.wint[l][MIXC + i * 16 + dc], eng="pool", disjoint=False)
                pgt = psb[(ng % 2) * 2]
                ptm = psb[(ng % 2) * 2 + 1]
                s_ = sg[ng % 2]
                ng += 1
                for kc in range(KC):
                    S.mm(pgt, (w, w[:, kc, :]), (uT, uT[:, kc, :]), start=(kc == 0), stop=(kc == KC - 1))
                nk = BR_K[i]
                for r in range(nk):
                    S.mm(ptm, (wbt, wbt[:, rc0 + r, :]), (yT, yT[:, rc0 + r, :]), start=(r == 0), stop=(r == nk - 1))
                rc0 += nk
                S.actv(s_, pgt, AF.Sigmoid)
                if i == 0:
                    S.tt("dve", acc, s_, ptm, ALU.mult)
                elif i < 3:
                    S.tt("dve", t2, s_, ptm, ALU.mult)
                    S.tt("dve", acc, acc, t2, ALU.add)
                else:
                    S.tt("dve", t2, s_, ptm, ALU.mult)
                    S.tt("dve", (mg, mg[:, dc, :]), acc, t2, ALU.add, disjoint=True)
        for dc in range(KC):
            w = wo[no % 2]
            no += 1
            S.dma((w, w[:].rearrange("p a b -> p (a b)")), g.wot[l][dc], eng="pool", disjoint=False)
            po = psb[4 + (dc % 2)]
            for kc in range(KC):
                S.mm(po, (w, w[:, kc, :]), (mg, mg[:, kc, :]), start=(kc == 0), stop=(kc == KC - 1))
            S.actv((bufX, bufX[:, dc, :]), po, AF.Copy, disjoint=True)
            q_ = sq[dc % 2]
            S.actv(q_, po, AF.Square)
            S.mm(ssb, ones, q_, start=(dc == 0), stop=(dc == KC - 1))
        postnorm_residual(S, g, h, h, tok, bufX, rstd, tmp, epsb, ssb, vec, 48, hre, hout)
    S.end_phase()


import math, os

C_PATTERNS = ((128, 1), (512, 4), (2048, 16))
CST_BI = 0
CST_ID = 1024
CST_NTI = 1152
CST_NSL = 1280
CST_MB = 1408
CST_M2 = 3456
NCST = 3584
NEG = -30000.0
LAYERS_A = 4


def _rel_bucket(dist):
    dist = np.asarray(dist)
    d = np.maximum(dist, 1).astype(np.float32)
    large = 16 + (np.log(d / np.float32(16)) / np.float32(math.log(2048 / 16)) * np.float32(16)).astype(np.int32)
    large = np.minimum(large, 31)
    return np.where(dist < 16, dist, large)


def bi_tile(kind, pc):
    k = np.arange(128)[:, None]
    q = np.arange(128)[None, :]
    du = q - k + (128 if pc == 0 else 0)
    if kind == 'D':
        valid = (du >= 0) & (du < 128)
        r = 1
    else:
        valid = (du >= 0) & (du <= 128)
        r = C_PATTERNS[kind][1]
    b = _rel_bucket(np.maximum(du, 0) * r)
    return np.where(valid, b, -1).astype(np.float32)


BI_KINDS = [('D', 0), ('D', 1), (0, 0), (0, 1), (1, 0), (1, 1), (2, 0), (2, 1)]


def make_consts():
    c = np.zeros((128, NCST), np.float32)
    for i, (kind, pc) in enumerate(BI_KINDS):
        c[:, CST_BI + i * 128:CST_BI + (i + 1) * 128] = bi_tile(kind, pc)
    c[:, CST_ID:CST_ID + 128] = np.eye(128, dtype=np.float32)
    j = np.arange(128)[:, None]
    s = np.arange(128)[None, :]
    c[:, CST_NTI:CST_NTI + 128] = np.where(j >= s, -1.0, 0.0)
    c[:, CST_NSL:CST_NSL + 128] = np.where(j < s, -1.0, 0.0)
    col = np.arange(512)[None, :]
    for m in range(4):
        c[:, CST_MB + m * 512:CST_MB + (m + 1) * 512] = np.where(128 * m + j < col, 1.0, 0.0)
    c[:, CST_M2:CST_M2 + 128] = np.where((j // 64 == s // 64) & (j <= s), 1.0, 0.0)
    return c


def setup_phase(S, g):
    S.begin_phase()
    bi = S.sb("bi", [128, 8, 128], F32)
    S.dma(bi, g.cst[:, CST_BI:CST_BI + 1024].rearrange("p (a b) -> p a b", b=128), disjoint=False)
    relb = S.sb("relb", [128, 640], F32)
    S.dma(relb, g.relb, disjoint=False)
    ebD = S.sb("ebD", [128, 2, 8, 128], F32)
    ebC = S.sb("ebC", [128, 3, 2, 2, 2, 128], F32)
    tmps = [S.sb("tb%d" % i, [128, 128], F32) for i in range(4)]
    n = 0
    for ti, (kind, pc) in enumerate(BI_KINDS):
        tile_np = bi_tile(kind, pc)
        buckets = sorted(set(int(v) for v in np.unique(tile_np) if v >= 0))
        nh = 8 if kind == 'D' else 4
        for h in range(nh):
            if kind == 'D':
                dst = (ebD, ebD[:, pc, h, :])
                col = 12 + h
                eng = "dve"
            else:
                dst = (ebC, ebC[:, kind, h // 2, h % 2, pc, :])
                col = kind * 4 + h
                eng = "dve"
            src = (bi, bi[:, ti, :])
            S.ts(eng, dst, src, 0.0, NEG, op0=ALU.is_lt, op1=ALU.mult, disjoint=True)
            for b in buckets:
                tm = tmps[(n % 2) + (0 if eng == "dve" else 2)]
                n += 1
                S.ts(eng, tm, src, float(b), (relb, relb[:, b * 20 + col:b * 20 + col + 1]), op0=ALU.is_equal, op1=ALU.mult)
                S.tt(eng, dst, dst, tm, ALU.add, disjoint=True)
    S.dma(g.ebD, (ebD, ebD[:].rearrange("p a b c -> p (a b c)")))
    S.dma(g.ebC, (ebC, ebC[:].rearrange("p a b c d e -> p (a b c d e)")))
    S.end_phase()


def load_ident(S, g):
    idf = S.sb("idf", [128, 128], F32)
    S.dma(idf, g.cst[:, CST_ID:CST_ID + 128], disjoint=False)
    idb = S.sb("idb", [128, 128], BF16)
    S.copy("dve", idb, idf)
    return idb


def to_tokmajor(S, src, dst, NB, idb, pT, cnt, engs=("act", "dve")):
    for n in range(NB):
        p = pT[cnt[0] % len(pT)]
        S.tr(p, (src, src[:, n * 128:(n + 1) * 128]), idb)
        S.copy(engs[cnt[0] % len(engs)], (dst, dst[:, n, :]), p, disjoint=True)
        cnt[0] += 1


def mixD_phase(S, g, l):
    T = g.T
    NB = T // 128
    S.begin_phase()
    idb = load_ident(S, g)
    ones64 = S.sb("ones64", [128, 64], BF16)
    S.memset("dve", ones64, 1.0)
    qD = S.sb("qD", [64, 8, T], BF16)
    kD = S.sb("kD", [64, 2, T], BF16)
    vT = S.sb("vT", [128, T], BF16)
    Vtok = S.sb("Vtok", [128, NB, 128], BF16)
    yD = S.sb("yD", [64, 8, T], BF16)
    eb = S.sb("eb", [128, 2, 8, 128], F32)
    S.dma((eb, eb[:].rearrange("p a b c -> p (a b c)")), g.ebD, disjoint=False)
    sk = S.sb("sk", [64, 8], F32)
    S.dma(sk, g.sinks[0:64, l * 8:(l + 1) * 8], disjoint=False)
    es = S.sb("es", [64, 8], F32)
    S.actv(es, sk, AF.Exp)
    esb = S.sb("esb", [64, 8, 128], F32)
    S.copy("dve", esb, (es, es[:, :].unsqueeze(2).to_broadcast([64, 8, 128])))
    for h in range(8):
        S.dma((qD, qD[:, h, :]), g.projT[R_DQ + h * 64:R_DQ + (h + 1) * 64, :])
    for kv in range(2):
        S.dma((kD, kD[:, kv, :]), g.projT[R_DK + kv * 64:R_DK + (kv + 1) * 64, :])
    S.dma(vT, g.projT[R_DV:R_DV + 128, :], disjoint=False)
    pT = [S.ps("pT%d" % i, [128, 128], BF16) for i in range(2)]
    pS = [S.ps("pS%d" % i, [128, 2, 512], F32) for i in range(2)]
    pO = S.ps("pO", [128, 512], F32)
    pD = S.ps("pD", [128, 512], F32)
    Zs = [S.sb("Zs%d" % i, [128, 2, 512], F32) for i in range(2)]
    Pb = [S.sb("Pb%d" % i, [128, 2, 512], BF16) for i in range(2)]
    dt = S.sb("dt", [64, 512], F32)
    cnt = [0]
    to_tokmajor(S, vT, Vtok, NB, idb, pT, cnt)
    it = 0
    for n in range(NB):
        for gk in range(2):
            Sp = pS[it % 2]
            Z = Zs[it % 2]
            P = Pb[it % 2]
            it += 1
            rq = (qD, qD[:, 4 * gk:4 * gk + 4, n * 128:(n + 1) * 128])
            lo = 0 if n > 0 else 1
            if n > 0:
                S.mm((Sp, Sp[:, 0, :]), (kD, kD[:, gk, (n - 1) * 128:n * 128]), rq)
            S.mm((Sp, Sp[:, 1, :]), (kD, kD[:, gk, n * 128:(n + 1) * 128]), rq, start=True)
            S.stt((Z, Z[:, lo:2, :].rearrange("p a (h q) -> p a h q", h=4)),
                  (Sp, Sp[:, lo:2, :].rearrange("p a (h q) -> p a h q", h=4)), 0.125,
                  (eb, eb[:, lo:2, 4 * gk:4 * gk + 4, :]), ALU.mult, ALU.add)
            S.actv((P, P[:, lo:2, :]), (Z, Z[:, lo:2, :]), AF.Exp)
            for (pp, lhs_of) in ((pO, None), (pD, ones64)):
                if n > 0:
                    lh = (Vtok, Vtok[:, n - 1, gk * 64:(gk + 1) * 64]) if lhs_of is None else ones64
                    S.mm((pp, pp[0:64, :]), lh, (P, P[:, 0, :]), start=True, stop=False)
                lh = (Vtok, Vtok[:, n, gk * 64:(gk + 1) * 64]) if lhs_of is None else ones64
                S.mm((pp, pp[0:64, :]), lh, (P, P[:, 1, :]), start=(n == 0), stop=True)
            S.tt("dve", (dt, dt[:, :].rearrange("p (h q) -> p h q", h=4)),
                 (pD, pD[0:64, :].rearrange("p (h q) -> p h q", h=4)), (esb, esb[:, 4 * gk:4 * gk + 4, :]), ALU.add)
            S.recip(dt, dt)
            S.tt("dve", (yD, yD[:, 4 * gk:4 * gk + 4, n * 128:(n + 1) * 128]),
                 (pO, pO[0:64, :].rearrange("p (h q) -> p h q", h=4)),
                 (dt, dt[:, :].rearrange("p (h q) -> p h q", h=4)), ALU.mult, disjoint=True)
    for h in range(8):
        S.dma(g.yT[Y_D + h * 64:Y_D + (h + 1) * 64, :], (yD, yD[:, h, :]))
    S.end_phase()


def mixC_phase(S, g, l):
    T = g.T
    NB = T // 128
    S.begin_phase()
    idb = load_ident(S, g)
    ones64 = S.sb("ones64", [128, 64], BF16)
    S.memset("dve", ones64, 1.0)
    eb = S.sb("eb", [128, 3, 2, 2, 2, 128], F32)
    S.dma((eb, eb[:].rearrange("p a b c d e -> p (a b c d e)")), g.ebC, disjoint=False)
    natq = [S.sb("natq%d" % i, [64, 2, T], BF16) for i in range(2)]
    perq = [S.sb("perq%d" % i, [64, 2, T], BF16) for i in range(2)]
    natv = S.sb("natv", [128, T], BF16)
    perv = S.sb("perv", [128, T], BF16)
    Vtok = S.sb("Vtok", [128, NB, 128], BF16)
    accN = S.sb("accN", [64, 2, T], F32)
    accD = S.sb("accD", [64, 2, T], F32)
    ybf = S.sb("ybf", [64, 2, T], BF16)
    pT = [S.ps("pT%d" % i, [128, 128], BF16) for i in range(2)]
    pS = [S.ps("pS%d" % i, [128, 2, 2, 128], F32) for i in range(2)]
    pO = [S.ps("pO%d" % i, [128, 512], F32) for i in range(2)]
    pD = [S.ps("pD%d" % i, [128, 512], F32) for i in range(2)]
    Zs = [S.sb("Zs%d" % i, [128, 2, 2, 128], F32) for i in range(2)]
    Pb = [S.sb("Pb%d" % i, [128, 2, 2, 128], BF16) for i in range(2)]
    cnt = [0]
    it = 0
    rows = (R_CQ, R_CK, R_CV)
    for hp in range(2):
        for gi, (win, r) in enumerate(C_PATTERNS):
            cur = []
            for j in range(2):
                r0 = rows[j] + gi * 256 + hp * 128
                for hh in range(2):
                    S.dma((natq[j], natq[j][:, hh, :]), g.projT[r0 + hh * 64:r0 + (hh + 1) * 64, :], disjoint=(hh == 1))
                if r > 1:
                    S.copy("act" if j == 0 else "dve", (perq[j], perq[j][:, :, :].rearrange("p h (c i) -> p h c i", c=r)),
                           (natq[j], natq[j][:, :, :].rearrange("p h (i c) -> p h c i", c=r)))
                    cur.append(perq[j])
                else:
                    cur.append(natq[j])
            r0 = rows[2] + gi * 256 + hp * 128
            S.dma(natv, g.projT[r0:r0 + 128, :], disjoint=False)
            if r > 1:
                S.copy("act", (perv, perv[:, :].rearrange("p (c i) -> p c i", c=r)),
                       (natv, natv[:, :].rearrange("p (i c) -> p c i", c=r)))
                vp = perv
            else:
                vp = natv
            qp, kp = cur
            to_tokmajor(S, vp, Vtok, NB, idb, pT, cnt)
            Lb = NB // r
            for c in range(r):
                for n in range(Lb):
                    pb = c * Lb + n
                    Sp = pS[it % 2]
                    Z = Zs[it % 2]
                    P = Pb[it % 2]
                    po = pO[it % 2]
                    pd = pD[it % 2]
                    it += 1
                    for hh in range(2):
                        rq = (qp, qp[:, hh, pb * 128:(pb + 1) * 128])
                        if n > 0:
                            S.mm((Sp, Sp[:, hh, 0, :]), (kp, kp[:, hh, (pb - 1) * 128:pb * 128]), rq)
                        S.mm((Sp, Sp[:, hh, 1, :]), (kp, kp[:, hh, pb * 128:(pb + 1) * 128]), rq)
                    if n > 0:
                        S.stt(Z, Sp, 0.125, (eb, eb[:, gi, hp, :, :, :]), ALU.mult, ALU.add)
                        S.actv(P, Z, AF.Exp)
                    else:
                        S.stt((Z, Z[:, :, 1, :]), (Sp, Sp[:, :, 1, :]), 0.125, (eb, eb[:, gi, hp, :, 1, :]), ALU.mult, ALU.add)
                        S.actv((P, P[:, :, 1, :]), (Z, Z[:, :, 1, :]), AF.Exp)
                    for hh in range(2):
                        vs = slice(hh * 64, (hh + 1) * 64)
                        cs = slice(hh * 128, (hh + 1) * 128)
                        for (pp, isden) in ((po, False), (pd, True)):
                            if n > 0:
                                lh = ones64 if isden else (Vtok, Vtok[:, pb - 1, vs])
                                S.mm((pp, pp[0:64, cs]), lh, (P, P[:, hh, 0, :]), start=True, stop=False)
                            lh = ones64 if isden else (Vtok, Vtok[:, pb, vs])
                            S.mm((pp, pp[0:64, cs]), lh, (P, P[:, hh, 1, :]), start=(n == 0), stop=True)
                    t0 = c + r * 128 * n
                    sl = slice(t0, t0 + r * 127 + 1, r) if r > 1 else slice(t0, t0 + 128)
                    for (acc, pp, eng) in ((accN, po, "dve"), (accD, pd, "act")):
                        av = (acc, acc[:, :, sl])
                        pv = (pp, pp[0:64, 0:256].rearrange("p (h q) -> p h q", h=2))
                        if gi == 0:
                            S.copy(eng, av, pv, disjoint=True)
                        else:
                            S.tt("dve", av, av, pv, ALU.add, disjoint=True)
        S.recip(accD, accD)
        S.tt("dve", ybf, accN, accD, ALU.mult)
        for hh in range(2):
            r0 = Y_C + (2 * hp + hh) * 64
            S.dma(g.yT[r0:r0 + 64, :], (ybf, ybf[:, hh, :]))
    S.end_phase()


def load_cst_bf(S, g, name, c0, n):
    f = S.sb(name + "f", [128, n], F32)
    S.dma(f, g.cst[:, c0:c0 + n], disjoint=False)
    b = S.sb(name + "b", [128, n], BF16)
    S.copy("dve", b, f)
    return f, b


def mixB_phase(S, g, l):
    T = g.T
    NB = T // 128
    NQ = T // 512
    S.begin_phase()
    idb = load_ident(S, g)
    _, nti = load_cst_bf(S, g, "nti", CST_NTI, 128)
    _, nsl = load_cst_bf(S, g, "nsl", CST_NSL, 128)
    maskB = S.sb("maskB", [128, 4, 512], F32)
    S.dma((maskB, maskB[:].rearrange("p a b -> p (a b)")), g.cst[:, CST_MB:CST_MB + 2048], disjoint=False)
    oneb = S.sb("oneb", [128, 1], F32)
    S.memset("dve", oneb, 1.0)
    qh = S.sb("qh", [64, 2, T], BF16)
    kh = S.sb("kh", [64, 2, T], BF16)
    vT = S.sb("vT", [128, T], BF16)
    Vtok = S.sb("Vtok", [128, NB, 128], BF16)
    ybf = S.sb("ybf", [64, 2, T], BF16)
    pT = [S.ps("pT%d" % i, [128, 128], BF16) for i in range(2)]
    NST = 2
    sets = []
    for k in range(NST):
        st = G()
        st.pS = [S.ps("pS%d_%d" % (k, i), [128, 512], F32) for i in range(1)]
        st.pB = S.ps("pB%d" % k, [128, 512], F32)
        st.pO = S.ps("pO%d" % k, [128, 512], F32)
        st.e = [S.sb("e%d_%d" % (k, i), [128, 512], F32) for i in range(2)]
        st.sp = [S.sb("sp%d_%d" % (k, i), [128, 512], BF16) for i in range(2)]
        st.w = [S.sb("w%d_%d" % (k, i), [128, 512], F32) for i in range(2)]
        st.a = [S.sb("a%d_%d" % (k, i), [128, 512], BF16) for i in range(2)]
        sets.append(st)
    cnt = [0]

    def chain(st, hh, qt):
        qs = (qh, qh[:, hh, qt * 512:(qt + 1) * 512])
        first = True
        it = 0
        for kb in range(4 * qt + 3, -1, -1):
            m = kb - 4 * qt
            pS = st.pS[it % len(st.pS)]
            e = st.e[it % 2]
            sp = st.sp[it % 2]
            w = st.w[it % 2]
            a = st.a[it % 2]
            it += 1
            S.mm(pS, (kh, kh[:, hh, kb * 128:(kb + 1) * 128]), qs)
            yield
            S.actv(e, pS, AF.Exp, scale=0.125)
            if m >= 0:
                S.tt("dve", e, e, (maskB, maskB[:, m, :]), ALU.mult)
            yield
            S.actv(sp, e, AF.Ln, bias=(oneb, oneb[:, 0:1]))
            yield
            S.mm(st.pB, nti, sp, start=first, stop=False)
            yield
            S.actv(w, st.pB, AF.Exp)
            yield
            S.mm(st.pB, nsl, sp, start=False, stop=(kb == 0))
            S.tt("dve", a, w, e, ALU.mult)
            yield
            S.mm((st.pO, st.pO[0:64, :]), (Vtok, Vtok[:, kb, hh * 64:(hh + 1) * 64]), a, start=first, stop=(kb == 0))
            first = False
            yield
        S.copy("act", (ybf, ybf[:, hh, qt * 512:(qt + 1) * 512]), (st.pO, st.pO[0:64, :]), disjoint=True)
        yield

    for hp in range(4):
        for hh in range(2):
            S.dma((qh, qh[:, hh, :]), g.projT[R_BQ + hp * 128 + hh * 64:R_BQ + hp * 128 + (hh + 1) * 64, :], disjoint=(hh == 1))
            S.dma((kh, kh[:, hh, :]), g.projT[R_BK + hp * 128 + hh * 64:R_BK + hp * 128 + (hh + 1) * 64, :], disjoint=(hh == 1))
        S.dma(vT, g.projT[R_BV + hp * 128:R_BV + (hp + 1) * 128, :], disjoint=False)
        to_tokmajor(S, vT, Vtok, NB, idb, pT, cnt)
        work = [(hh, qt) for qt in range(NQ - 1, -1, -1) for hh in range(2)]
        active = []
        free_sets = list(sets)
        while work or active:
            while work and free_sets:
                hh, qt = work.pop(0)
                st = free_sets.pop(0)
                active.append((chain(st, hh, qt), st))
            nxt = []
            for gen, st in active:
                try:
                    next(gen)
                    nxt.append((gen, st))
                except StopIteration:
                    free_sets.append(st)
            active = nxt
        for hh in range(2):
            r0 = Y_B + hp * 128 + hh * 64
            S.dma(g.yT[r0:r0 + 64, :], (ybf, ybf[:, hh, :]))
    S.end_phase()


def mixA_phase(S, g, l):
    T = g.T
    SEG = min(1024, T)
    NSEG = T // SEG
    NBS = SEG // 128
    NCH = SEG // 64
    S.begin_phase()
    idb = load_ident(S, g)
    ones, epsb = phase_consts(S, g)
    vec = load_vec(S, g, l)
    m2 = S.sb("m2", [128, 128], F32)
    S.dma(m2, g.cst[:, CST_M2:CST_M2 + 128], disjoint=False)
    cmask = S.sb("cmask", [128, SEG], F32)
    S.memset("dve", cmask, 1.0)
    S.memset("dve", (cmask, cmask[:, 0:SEG:64]), 0.0)
    lbl = S.sb("lbl", [128, 4, LAYERS_A], F32)
    S.dma((lbl, lbl[:].rearrange("p a b -> p (a b)")), g.lbl, disjoint=False)
    le = S.sb("le", [128, 4, LAYERS_A], F32)
    S.actv(le, lbl, AF.Exp)
    lsum = S.sb("lsum", [128, 4], F32)
    S.tt("dve", lsum, (le, le[:, :, 0]), (le, le[:, :, 1]), ALU.add)
    for i in range(2, LAYERS_A):
        S.tt("dve", lsum, lsum, (le, le[:, :, i]), ALU.add)
    S.recip(lsum, lsum)
    lb = S.sb("lb", [128, 4], F32)
    oml = S.sb("oml", [128, 4], F32)
    S.memset("dve", lb, 0.0)
    for i in range(1, l + 1):
        S.tt("dve", lb, lb, (le, le[:, :, i]), ALU.add)
    S.tt("dve", lb, lb, lsum, ALU.mult)
    S.ts("dve", oml, lb, -1.0, 1.0, op0=ALU.mult, op1=ALU.add)
    qn = S.sb("qn", [128, SEG], BF16)
    fn = S.sb("fn", [128, SEG], BF16)
    vn = S.sb("vn", [128, SEG], BF16)
    khat = S.sb("khat", [128, SEG], BF16)
    t1 = S.sb("t1", [128, SEG], F32)
    t2 = S.sb("t2", [128, SEG], F32)
    t3 = S.sb("t3", [128, SEG], F32)
    t4 = S.sb("t4", [128, SEG], F32)
    bb = S.sb("bb", [128, SEG], F32)
    H = []
    for h in range(4):
        hs = G()
        hs.gn = S.sb("gn%d" % h, [128, SEG], BF16)
        hs.ebt = S.sb("ebt%d" % h, [128, SEG], F32)
        hs.qt = S.sb("qt%d" % h, [128, SEG], BF16)
        hs.kt = S.sb("kt%d" % h, [128, SEG], BF16)
        hs.qb = S.sb("qb%d" % h, [128, SEG], BF16)
        hs.Vt = S.sb("Vt%d" % h, [128, NBS, 128], BF16)
        hs.Kt = S.sb("Kt%d" % h, [128, NBS, 128], BF16)
        hs.oT = S.sb("oT%d" % h, [128, SEG], F32)
        hs.Sst = S.sb("Sst%d" % h, [128, 128], F32)
        hs.Sb = [S.sb("Sb%d_%d" % (h, i), [128, 128], BF16) for i in range(2)]
        hs.Pm = S.sb("Pm%d" % h, [128, 128], BF16)
        S.memset("dve", hs.Sst, 0.0)
        S.memset("dve", hs.Sb[0], 0.0)
        H.append(hs)
    pT = [S.ps("pT%d" % i, [128, 128], BF16) for i in range(2)]
    bsc = S.ps("bsc", [128, 4, 128], F32)
    bua = S.ps("bua", [128, 4, 128], F32)
    bub = S.ps("bub", [128, 4, 128], F32)
    bo = S.ps("bo", [128, 4, 128], F32)
    bss = S.ps("bss", [128, 512], F32)
    for h in range(4):
        H[h].psc = Buf("psc%d" % h, bsc.t, bsc.lock)
        H[h].pua = Buf("pua%d" % h, bua.t, bua.lock)
        H[h].pub = Buf("pub%d" % h, bub.t, bub.lock)
        H[h].po = Buf("po%d" % h, bo.t, bo.lock)
    cnt = [0]
    ysb = [S.sb("ysb%d" % i, [128, SEG], BF16) for i in range(2)]
    for seg in range(NSEG):
        cs = slice(seg * SEG, (seg + 1) * SEG)
        for h in range(4):
            hs = H[h]
            S.dma(qn, g.projT[R_AQ + h * 128:R_AQ + (h + 1) * 128, cs], disjoint=False)
            S.dma(fn, g.projT[R_AF + h * 128:R_AF + (h + 1) * 128, cs], disjoint=False)
            S.dma(vn, g.projT[R_AI + h * 128:R_AI + (h + 1) * 128, cs], disjoint=False)
            S.dma(hs.gn, g.projT[R_AG + h * 128:R_AG + (h + 1) * 128, cs], disjoint=False)
            S.actv(t1, fn, AF.Exp, scale=-1.0)
            S.ts("dve", t1, t1, 1.0, None, op0=ALU.add)
            S.recip(t1, t1)
            S.ts("dve", t1, t1, (oml, oml[:, h:h + 1]), (lb, lb[:, h:h + 1]), op0=ALU.mult, op1=ALU.add)
            S.actv(t2, t1, AF.Ln)
            S.ts("dve", t3, t1, -1.0, 1.0, op0=ALU.mult, op1=ALU.add)
            S.op("dve", "tensor_tensor_scan", dict(out=bb[:], data0=cmask[:], data1=t2[:], initial=0.0,
                                                   op0=ALU.mult, op1=ALU.add), reads=[cmask, t2], writes=[bb])
            bv = bb[:, :].rearrange("p (c i) -> p c i", i=64)
            bmid = bv[:, :, 31:32].to_broadcast([128, NCH, 64])
            blast = bv[:, :, 63:64].to_broadcast([128, NCH, 64])
            v3 = lambda b_: (b_, b_[:, :].rearrange("p (c i) -> p c i", i=64))
            S.tt("dve", v3(t2), (bb, bv), (bb, bmid), ALU.subtract)
            S.actv(t4, t2, AF.Exp)
            S.tt("dve", hs.qt, qn, t4, ALU.mult)
            S.actv(t4, t2, AF.Exp, scale=-1.0)
            S.tt("dve", hs.kt, t3, t4, ALU.mult)
            S.tt("dve", v3(t2), (bb, bv), (bb, blast), ALU.subtract)
            S.actv(t4, t2, AF.Exp, scale=-1.0)
            S.tt("dve", khat, t3, t4, ALU.mult)
            S.actv(hs.ebt, bb, AF.Exp)
            S.tt("dve", hs.qb, qn, hs.ebt, ALU.mult)
            if g.dbg is not None and h == 0 and seg == 0:
                S.dma(g.dbg[0], t1); S.dma(g.dbg[1], bb); S.dma(g.dbg[2], hs.ebt); S.dma(g.dbg[3], t3)
                S.copy("dve", t4, hs.qt); S.dma(g.dbg[4], t4)
            to_tokmajor(S, vn, hs.Vt, NBS, idb, pT, cnt)
            to_tokmajor(S, khat, hs.Kt, NBS, idb, pT, cnt)
        for n in range(NBS):
            bs = slice(n * 128, (n + 1) * 128)
            for h in range(4):
                hs = H[h]
                S.mm((hs.psc, bsc[:, h, :]), (hs.kt, hs.kt[:, bs]), (hs.qt, hs.qt[:, bs]))
                S.mm((hs.pua, bua[0:128, h, :]), (hs.Kt, hs.Kt[0:64, n, :]), (hs.Vt, hs.Vt[0:64, n, :]))
                S.mm((hs.pub, bub[0:128, h, :]), (hs.Kt, hs.Kt[64:128, n, :]), (hs.Vt, hs.Vt[64:128, n, :]))
            for h in range(4):
                hs = H[h]
                S.tt("dve", hs.Pm, (hs.psc, bsc[:, h, :]), m2, ALU.mult)
                cA = (2 * n) * 64 + 63
                S.stt(hs.Sst, hs.Sst, (hs.ebt, hs.ebt[:, cA:cA + 1]), (hs.pua, bua[:, h, :]), ALU.mult, ALU.add)
                S.copy("act", hs.Sb[1], hs.Sst)
            for h in range(4):
                hs = H[h]
                S.mm((hs.po, bo[:, h, :]), (hs.Vt, hs.Vt[:, n, :]), hs.Pm, start=True, stop=False)
                S.mm((hs.po, bo[:, h, 0:64]), hs.Sb[0], (hs.qb, hs.qb[:, n * 128:n * 128 + 64]), start=False, stop=False)
                S.mm((hs.po, bo[:, h, 64:128]), hs.Sb[1], (hs.qb, hs.qb[:, n * 128 + 64:(n + 1) * 128]), start=False, stop=True)
            for h in range(4):
                hs = H[h]
                cB = (2 * n + 1) * 64 + 63
                S.stt(hs.Sst, hs.Sst, (hs.ebt, hs.ebt[:, cB:cB + 1]), (hs.pub, bub[:, h, :]), ALU.mult, ALU.add)
                S.copy("act", hs.Sb[0], hs.Sst)
                S.copy("act", (hs.oT, hs.oT[:, bs]), (hs.po, bo[:, h, :]), disjoint=True)
        for h in range(4):
            hs = H[h]
            yb = ysb[h % 2]
            for c in range(SEG // 512):
                c5 = slice(c * 512, (c + 1) * 512)
                S.actv((t1, t1[:, c5]), (hs.oT, hs.oT[:, c5]), AF.Square)
                S.mm(bss, ones, (t1, t1[:, c5]))
                S.actv((t2, t2[:, c5]), bss, AF.Sqrt, scale=1.0 / 128, bias=(epsb, epsb[:, 0:1]))
            S.recip(t2, t2)
            if g.dbg is not None and h == 0 and seg == 0:
                S.dma(g.dbg[5], hs.oT); S.dma(g.dbg[6], t2)
            S.stt(t3, hs.oT, (vec, vec[:, 96:97]), t2, ALU.mult, ALU.mult)
            S.actv(t4, hs.gn, AF.Silu)
            S.tt("dve", yb, t3, t4, ALU.mult)
            S.dma(g.yT[Y_A + h * 128:Y_A + (h + 1) * 128, cs], yb)
    S.end_phase()


LAYERS = 4


def build(T, L, phases=None, debug=False, ext_in=()):
    nc = bass.Bass("TRN2", target_bir_lowering=False)
    g = G()
    g.nc = nc
    g.T = T
    g.L = L

    def din(name, shape, dt=F32):
        return nc.dram_tensor(name, list(shape), dt, kind="ExternalInput").ap()

    g.xT = din("xT", [D, T])
    g.w13t = [din("w13t_a", [L, 88, 128, 2048]), din("w13t_b", [L, 88, 128, 2048])]
    g.w2t = [din("w2t_a", [L, 16, 128, FF]), din("w2t_b", [L, 16, 128, FF])]
    g.wint = din("wint", [L, 116, 128, 2048])
    g.wbt = din("wbt", [L, 16, 128, 1792])
    g.wot = din("wot", [L, 16, 128, 2048])
    g.vecs = din("vecs", [L, 128, NVEC])
    g.lbl = din("lbl", [128, 4 * LAYERS])
    g.sinks = din("sinks", [128, LAYERS * 8])
    g.relb = din("relb", [128, 640])
    g.cst = din("cst", [128, NCST])
    g.outT = nc.dram_tensor("outT", [D, T], F32, kind="ExternalOutput").ap()
    sk = "ExternalOutput" if debug else "Internal"
    def scr(name, shape, dt):
        return nc.dram_tensor(name, shape, dt, kind=("ExternalInput" if name in ext_in else sk)).ap()
    g.projT = scr("projT", [6656, T], BF16)
    g.uT = scr("uT_s", [D, T], BF16)
    g.yT = scr("yT_s", [1792, T], BF16)
    g.ebD = nc.dram_tensor("ebD", [128, 2 * 8 * 128], F32, kind=sk).ap()
    g.ebC = nc.dram_tensor("ebC", [128, 3 * 2 * 2 * 2 * 128], F32, kind=sk).ap()
    g.lbs = nc.dram_tensor("lbs", [128, 4 * LAYERS], F32, kind=sk).ap()
    g.dbg = nc.dram_tensor('dbg', [16, 128, 1024], F32, kind='ExternalOutput').ap() if debug else None
    S = Sched(nc)
    g.S = S
    allp = ["ffn1", "inproj", "A", "B", "C", "D", "merge", "ffn2"]
    if phases is None:
        phases = allp
    if any(p in phases for p in ("A", "C", "D")):
        setup_phase(S, g)
    for l in range(L):
        first = (l == 0)
        for p in phases:
            if p == "ffn1":
                ffn_phase(S, g, l, 0, g.xT if first else g.outT, g.outT)
            elif p == "inproj":
                inproj_phase(S, g, l, g.outT)
            elif p == "A":
                mixA_phase(S, g, l)
            elif p == "B":
                mixB_phase(S, g, l)
            elif p == "C":
                mixC_phase(S, g, l)
            elif p == "D":
                mixD_phase(S, g, l)
            elif p == "merge":
                merge_phase(S, g, l, g.outT)
            elif p == "ffn2":
                ffn_phase(S, g, l, 1, g.outT, g.outT)
    return nc, g


def tile_w(w, kc, nc_):
    return np.ascontiguousarray(w.reshape(kc, 128, nc_, 128).transpose(2, 1, 0, 3).reshape(nc_, 128, kc * 128))


def prep_weights(inp, L):
    out = {}
    out["w13t_a"] = np.stack([tile_w(inp["ffn1_w13"][l], 16, 88) for l in range(L)])
    out["w13t_b"] = np.stack([tile_w(inp["ffn2_w13"][l], 16, 88) for l in range(L)])
    out["w2t_a"] = np.stack([tile_w(inp["ffn1_w2"][l], 44, 16) for l in range(L)])
    out["w2t_b"] = np.stack([tile_w(inp["ffn2_w2"][l], 44, 16) for l in range(L)])
    out["wint"] = np.stack([tile_w(inp["w_in"][l], 16, 116) for l in range(L)])
    out["wbt"] = np.stack([tile_w(inp["w_branch"][l], 14, 16) for l in range(L)])
    out["wot"] = np.stack([tile_w(inp["w_out"][l], 16, 16) for l in range(L)])
    vecs = np.zeros((L, 128, NVEC), np.float32)
    for l in range(L):
        for i, nm in enumerate(("ffn1_norm", "mix_norm", "ffn2_norm")):
            for j in range(2):
                vecs[l, :, (2 * i + j) * 16:(2 * i + j + 1) * 16] = inp[nm][l, j].reshape(16, 128).T
        vecs[l, :, 96] = inp["hgrn_out_norm"][l]
    out["vecs"] = vecs
    lb = np.asarray(inp["hgrn_lb_logits"])
    out["lbl"] = np.ascontiguousarray(lb.reshape(LAYERS, 4, 128).transpose(2, 1, 0).reshape(128, 4 * LAYERS))
    out["sinks"] = np.ascontiguousarray(np.broadcast_to(np.asarray(inp["attn_sinks"]).reshape(1, -1), (128, LAYERS * 8)))
    out["relb"] = np.ascontiguousarray(np.broadcast_to(np.asarray(inp["rel_bias"]).reshape(1, 640), (128, 640)))
    out["cst"] = make_consts()
    return out


_CACHE = {}


def kernel(**inputs):
    x = np.asarray(inputs["x"])
    B, T, _ = x.shape
    L = LAYERS
    key = (T, L)
    if key not in _CACHE:
        _CACHE[key] = build(T, L, None, debug=False)
    nc, g = _CACHE[key]
    w = prep_weights({k: np.asarray(v) for k, v in inputs.items() if k != "x"}, L)
    in_maps = []
    for b in range(B):
        m = dict(w)
        m["xT"] = np.ascontiguousarray(x[b].T)
        in_maps.append(m)
    res = run_bass_kernel_spmd(nc, in_maps, core_ids=list(range(B)))
    out = np.stack([np.ascontiguousarray(res.results[b]["outT"].T) for b in range(B)], axis=0)
    return out.astype(np.float32, copy=False)
```

```python
import numpy as np
import concourse.bass as bass
import concourse.mybir as mybir
from concourse.bass_utils import run_bass_kernel_spmd
from contextlib import ExitStack

F32 = mybir.dt.float32
BF16 = mybir.dt.bfloat16
AF = mybir.ActivationFunctionType
ALU = mybir.AluOpType

import os
SERIAL_PH = [int(x) for x in os.environ.get('MK_SERIAL', '').split(',') if x]
SEM_LIMIT = 30000
NDMA_SEMS = 10


class Buf:
    __slots__ = ("name", "writers", "readers", "t", "lock")

    def __init__(self, name, t=None, lock=None):
        self.name = name
        self.writers = []
        self.readers = []
        self.t = t
        self.lock = lock

    def __getitem__(self, k):
        return self.t[k]


class Op:
    __slots__ = ("eng", "fn", "deps", "is_dma", "needs_inc", "sem", "val", "clock", "idx", "phase")


def _ba(x):
    if isinstance(x, Buf):
        return x, x.t[:]
    return x


class Sched:
    COMPUTE = ("pe", "act", "dve", "pool")
    ALL = ("pe", "act", "dve", "pool", "sp")

    def __init__(self, nc):
        self.nc = nc
        self.ops = []
        self.engs = {"pe": nc.tensor, "act": nc.scalar, "dve": nc.vector,
                     "pool": nc.gpsimd, "sp": nc.sync}
        self.phase = 0
        self.nops = 0
        self.last = {}
        self.dmas = []
        self.clocks = {e: {} for e in self.ALL}
        self.cur_sem = {}
        self.cur_cnt = {}
        self.nsw = {}
        for e in self.COMPUTE:
            self.cur_sem[e] = nc.alloc_semaphore("s_%s_0" % e)
            self.cur_cnt[e] = 0
            self.nsw[e] = 0
        self.dma_sems = {}
        self.dma_cnt = {}
        self.dma_last = {}
        self.dma_rr = {}
        self.nwaits = 0
        self.ninst = {e: 0 for e in self.ALL}
        self.es = None

    def begin_phase(self):
        self.es = ExitStack()
        self.es.__enter__()

    def sb(self, name, shape, dt):
        t = self.es.enter_context(self.nc.sbuf_tensor("%s_p%d" % (name, self.phase), list(shape), dt))
        return Buf(name, t)

    def ps(self, name, shape, dt=F32):
        t = self.es.enter_context(self.nc.psum_tensor("%s_p%d" % (name, self.phase), list(shape), dt))
        b = Buf(name, t)
        b.lock = Buf(name + "_lock")
        return b

    def end_phase(self):
        self.barrier()
        self.emit()
        self.es.__exit__(None, None, None)
        self.es = None
        self.phase += 1

    def op(self, eng, name, kw, reads=(), writes=(), dma=False, disjoint=False):
        o = Op()
        o.eng = eng
        o.fn = (name, kw)
        o.is_dma = dma
        o.needs_inc = dma
        o.sem = None
        o.val = 0
        o.clock = None
        o.idx = self.nops
        o.phase = self.phase
        self.nops += 1
        deps = {}
        locks = []
        for b in reads:
            if b.lock is not None and b.lock not in locks:
                locks.append(b.lock)
        for b in writes:
            if b.lock is not None and b.lock not in locks:
                locks.append(b.lock)
        for b in locks:
            for w in b.writers:
                deps[w.idx] = w
        for b in reads:
            for w in b.writers:
                deps[w.idx] = w
        for b in writes:
            for r in b.readers:
                deps[r.idx] = r
            if (not disjoint) or b.readers:
                for w in b.writers:
                    deps[w.idx] = w
        ph = self.phase
        SERIAL = (ph in SERIAL_PH) or (-1 in SERIAL_PH)
        if SERIAL and self.ops:
            po = self.ops[-1]
            if po.fn is not None:
                deps[po.idx] = po
        if SERIAL:
            o.deps = [d for d in deps.values() if d.phase == ph]
        elif not dma:
            raw = set()
            if eng != "pe":
                for b in reads:
                    for w in b.writers:
                        if (not w.is_dma) and w.eng == eng:
                            raw.add(w.idx)
            o.deps = [d for d in deps.values() if d.phase == ph and (d.is_dma or d.eng != eng or d.idx in raw)]
        else:
            o.deps = [d for d in deps.values() if d.phase == ph]
        for d in o.deps:
            d.needs_inc = True
        for b in reads:
            if not dma:
                b.readers = [r for r in b.readers if r.is_dma or r.eng != eng]
            b.readers.append(o)
        for b in writes:
            if b.readers:
                b.writers = [o]
                b.readers = []
            elif disjoint:
                if not dma:
                    b.writers = [w for w in b.writers if w.is_dma or w.eng != eng]
                b.writers.append(o)
            else:
                b.writers = [o]
        for b in locks:
            b.writers = [o]
        self.ops.append(o)
        if dma:
            self.dmas.append(o)
        else:
            self.last[eng] = o
        return o

    def barrier(self):
        lasts = [o for o in self.last.values() if o.phase == self.phase]
        dmas = self.dmas
        self.dmas = []
        self.last = {}
        for o in lasts:
            o.needs_inc = True
        for e in self.ALL:
            o = Op()
            o.eng = e
            o.fn = None
            o.is_dma = False
            o.needs_inc = False
            o.sem = None
            o.val = 0
            o.clock = None
            o.idx = self.nops
            o.phase = self.phase
            self.nops += 1
            o.deps = [l for l in lasts if l.eng != e] + dmas
            self.ops.append(o)

    def emit(self):
        nc = self.nc
        for o in self.ops:
            e = o.eng
            eng = self.engs[e]
            clk = self.clocks[e]
            deps = list(o.deps)
            slot = None
            if o.is_dma:
                if e not in self.dma_sems:
                    self.dma_sems[e] = [nc.alloc_semaphore("d_%s_%d" % (e, i)) for i in range(NDMA_SEMS)]
                    self.dma_cnt[e] = [0] * NDMA_SEMS
                    self.dma_last[e] = [None] * NDMA_SEMS
                    self.dma_rr[e] = 0
                slot = self.dma_rr[e] % NDMA_SEMS
                self.dma_rr[e] += 1
                if self.dma_last[e][slot] is not None:
                    deps.append(self.dma_last[e][slot])
            if len(deps) > 1:
                deps.sort(key=lambda d: -d.idx)
            for d in deps:
                k = id(d.sem)
                if clk.get(k, (None, 0))[1] >= d.val:
                    continue
                eng.wait_ge(d.sem, d.val)
                self.nwaits += 1
                for kk, vv in d.clock.items():
                    if clk.get(kk, (None, 0))[1] < vv[1]:
                        clk[kk] = vv
            if o.fn is None:
                continue
            ins = getattr(eng, o.fn[0])(**o.fn[1])
            self.ninst[e] += 1
            if o.is_dma:
                sem = self.dma_sems[e][slot]
                self.dma_cnt[e][slot] += 16
                o.sem = sem
                o.val = self.dma_cnt[e][slot]
                ins.then_inc(sem, 16)
                self.dma_last[e][slot] = o
                c = dict(clk)
                c[id(sem)] = (sem, o.val)
                o.clock = c
            elif o.needs_inc:
                if self.cur_cnt[e] >= SEM_LIMIT:
                    self.nsw[e] += 1
                    self.cur_sem[e] = nc.alloc_semaphore("s_%s_%d" % (e, self.nsw[e]))
                    self.cur_cnt[e] = 0
                self.cur_cnt[e] += 1
                o.sem = self.cur_sem[e]
                o.val = self.cur_cnt[e]
                ins.then_inc(o.sem, 1)
                c = dict(clk)
                c[id(o.sem)] = (o.sem, o.val)
                o.clock = c
            o.fn = None
            o.deps = None
        self.ops = []

    def mm(self, out, lhsT, rhs, start=True, stop=True):
        ob, oa = _ba(out)
        lb, la = _ba(lhsT)
        rb, ra = _ba(rhs)
        return self.op("pe", "matmul", dict(out=oa, lhsT=la, rhs=ra, start=start, stop=stop),
                       reads=[lb, rb], writes=[ob], disjoint=True if not start else False)

    def tr(self, out, in_, ident):
        ob, oa = _ba(out)
        ib, ia = _ba(in_)
        db, da = _ba(ident)
        return self.op("pe", "transpose", dict(out=oa, in_=ia, identity=da), reads=[ib, db], writes=[ob], disjoint=True)

    def actv(self, out, in_, func, scale=1.0, bias=None, disjoint=False, eng="act"):
        ob, oa = _ba(out)
        ib, ia = _ba(in_)
        kw = dict(out=oa, in_=ia, func=func)
        reads = [ib]
        if isinstance(scale, tuple) or isinstance(scale, Buf):
            sb_, sa = _ba(scale)
            kw["scale"] = sa
            reads.append(sb_)
        elif scale != 1.0:
            kw["scale"] = float(scale)
        if bias is not None:
            if isinstance(bias, (tuple, Buf)):
                bb, ba = _ba(bias)
                kw["bias"] = ba
                reads.append(bb)
            else:
                kw["bias"] = float(bias)
        return self.op("act", "activation", kw, reads=reads, writes=[ob], disjoint=disjoint)

    def tt(self, eng, out, in0, in1, op, disjoint=False):
        ob, oa = _ba(out)
        ab, aa = _ba(in0)
        bb, ba = _ba(in1)
        return self.op(eng, "tensor_tensor", dict(out=oa, in0=aa, in1=ba, op=op), reads=[ab, bb], writes=[ob], disjoint=disjoint)

    def ts(self, eng, out, in0, s1, s2=None, op0=ALU.mult, op1=None, disjoint=False):
        ob, oa = _ba(out)
        ab, aa = _ba(in0)
        reads = [ab]
        kw = dict(out=oa, in0=aa, op0=op0)
        if isinstance(s1, (tuple, Buf)):
            b_, a_ = _ba(s1)
            reads.append(b_)
            kw["scalar1"] = a_
        else:
            kw["scalar1"] = float(s1)
        if s2 is None:
            kw["scalar2"] = None
        elif isinstance(s2, (tuple, Buf)):
            b_, a_ = _ba(s2)
            reads.append(b_)
            kw["scalar2"] = a_
        else:
            kw["scalar2"] = float(s2)
        if op1 is not None:
            kw["op1"] = op1
        return self.op(eng, "tensor_scalar", kw, reads=reads, writes=[ob], disjoint=disjoint)

    def stt(self, out, in0, scalar, in1, op0, op1, disjoint=False):
        ob, oa = _ba(out)
        ab, aa = _ba(in0)
        bb, ba = _ba(in1)
        reads = [ab, bb]
        if isinstance(scalar, (tuple, Buf)):
            b_, a_ = _ba(scalar)
            reads.append(b_)
            sc = a_
        else:
            sc = float(scalar)
        return self.op("dve", "scalar_tensor_tensor", dict(out=oa, in0=aa, scalar=sc, in1=ba, op0=op0, op1=op1),
                       reads=reads, writes=[ob], disjoint=disjoint)

    def copy(self, eng, out, in_, disjoint=False):
        ob, oa = _ba(out)
        ib, ia = _ba(in_)
        if eng == "act":
            return self.op("act", "activation", dict(out=oa, in_=ia, func=AF.Copy), reads=[ib], writes=[ob], disjoint=disjoint)
        return self.op(eng, "tensor_copy", dict(out=oa, in_=ia), reads=[ib], writes=[ob], disjoint=disjoint)

    def recip(self, out, in_, disjoint=False):
        ob, oa = _ba(out)
        ib, ia = _ba(in_)
        return self.op("dve", "reciprocal", dict(out=oa, in_=ia), reads=[ib], writes=[ob], disjoint=disjoint)

    def memset(self, eng, out, val, disjoint=False):
        ob, oa = _ba(out)
        return self.op(eng, "memset", dict(ap=oa, constant=float(val)), writes=[ob], disjoint=disjoint)

    def dma(self, out, in_, eng="sp", disjoint=True):
        reads, writes = [], []
        if isinstance(out, (tuple, Buf)):
            ob, oa = _ba(out)
            writes.append(ob)
        else:
            oa = out
        if isinstance(in_, (tuple, Buf)):
            ib, ia = _ba(in_)
            reads.append(ib)
        else:
            ia = in_
        return self.op(eng, "dma_start", dict(out=oa, in_=ia), reads=reads, writes=writes, dma=True, disjoint=disjoint)


D = 2048
KC = 16
FF = 5632
FC = 44
TT = 512
EPS = 1e-6
MIXC = 52
NVEC = 104

R_AQ, R_AF, R_AI, R_AG = 0, 512, 1024, 1536
R_BQ, R_BK, R_BV = 2048, 2560, 3072
R_CQ, R_CK, R_CV = 3584, 4352, 5120
R_DQ, R_DK, R_DV = 5888, 6400, 6528
Y_A, Y_B, Y_C, Y_D = 0, 512, 1024, 1280


class G:
    pass


def load_vec(S, g, l):
    vec = S.sb("vec", [128, NVEC + 48], F32)
    S.dma((vec, vec[:, 0:NVEC]), g.vecs[l])
    S.ts("dve", (vec, vec[:, NVEC:NVEC + 16]), (vec, vec[:, 16:32]), 0.5)
    S.ts("dve", (vec, vec[:, NVEC + 16:NVEC + 32]), (vec, vec[:, 80:96]), 0.5)
    return vec


def phase_consts(S, g):
    ones = S.sb("ones", [128, 128], BF16)
    S.memset("dve", ones, 1.0)
    epsb = S.sb("epsb", [128, 1], F32)
    S.memset("dve", epsb, EPS)
    return ones, epsb


def rms_rstd(S, ss_ps, rstd, tmp, epsb, n):
    S.actv(tmp, ss_ps, AF.Sqrt, scale=1.0 / n, bias=(epsb, epsb[:, 0:1]))
    S.recip(rstd, tmp)


def prenorm_tile(S, g, hsrc, tok, bufX, xn, sq, tmp, rstd, ones, epsb, ssb, vec, gcol):
    srcv = hsrc.rearrange("(kc p) t -> p kc t", p=128)
    S.dma(bufX, srcv[:, :, tok], disjoint=False)
    for kc in range(KC):
        q = sq[kc % 2]
        S.actv(q, (bufX, bufX[:, kc, :]), AF.Square)
        S.mm(ssb, ones, q, start=(kc == 0), stop=(kc == KC - 1))
    rms_rstd(S, ssb, rstd, tmp, epsb, D)
    for kc in range(KC):
        S.stt((xn, xn[:, kc, :]), (bufX, bufX[:, kc, :]), (vec, vec[:, gcol + kc:gcol + kc + 1]), rstd,
              ALU.mult, ALU.mult, disjoint=True)


def postnorm_residual(S, g, hsrc, hdst, tok, bufX, rstd, tmp, epsb, ssb, vec, gcol, hre, hout):
    rms_rstd(S, ssb, rstd, tmp, epsb, D)
    for dc in range(KC):
        hr = hre[dc % 2]
        ho = hout[dc % 2]
        S.dma(hr, hsrc[dc * 128:(dc + 1) * 128, tok], disjoint=False)
        S.stt(ho, (bufX, bufX[:, dc, :]), (vec, vec[:, gcol + dc:gcol + dc + 1]), rstd, ALU.mult, ALU.mult)
        S.tt("dve", ho, ho, hr, ALU.add)
        S.dma(hdst[dc * 128:(dc + 1) * 128, tok], ho)


def ffn_phase(S, g, l, which, hsrc, hdst):
    NT = g.T // TT
    w13t = g.w13t[which][l]
    w2t = g.w2t[which][l]
    S.begin_phase()
    ones, epsb = phase_consts(S, g)
    vec = load_vec(S, g, l)
    gpre = 0 if which == 0 else 64
    gpost = NVEC if which == 0 else NVEC + 16
    bufX = S.sb("bufX", [128, KC, TT], F32)
    xn = S.sb("xn", [128, KC, TT], BF16)
    act = S.sb("actT", [128, FC, TT], BF16)
    w13b = [S.sb("w13b%d" % i, [128, KC, 128], BF16) for i in range(6)]
    w2b = [S.sb("w2b%d" % i, [128, FC, 128], BF16) for i in range(2)]
    sq = [S.sb("sq%d" % i, [128, TT], BF16) for i in range(2)]
    tmp = [S.sb("tmp%d" % i, [128, TT], F32) for i in range(2)]
    rstd = S.sb("rstd", [128, TT], F32)
    hre = [S.sb("hre%d" % i, [128, TT], F32) for i in range(2)]
    hout = [S.sb("hout%d" % i, [128, TT], F32) for i in range(2)]
    psb = [S.ps("psb%d" % i, [128, 512], F32) for i in range(7)]
    ssb = psb[6]
    n13 = 0
    n2 = 0
    for tt in range(NT):
        tok = slice(tt * TT, (tt + 1) * TT)
        prenorm_tile(S, g, hsrc, tok, bufX, xn, sq, tmp[0], rstd, ones, epsb, ssb, vec, gpre)
        for j in range(FC):
            wb = []
            for half in range(2):
                wbf = w13b[n13 % 6]
                n13 += 1
                S.dma((wbf, wbf[:].rearrange("p a b -> p (a b)")), w13t[half * FC + j], eng="pool", disjoint=False)
                wb.append(wbf)
            pg = psb[(j % 2) * 2]
            pu = psb[(j % 2) * 2 + 1]
            for half, pp in ((0, pg), (1, pu)):
                for kc in range(KC):
                    S.mm(pp, (wb[half], wb[half][:, kc, :]), (xn, xn[:, kc, :]), start=(kc == 0), stop=(kc == KC - 1))
            tm = tmp[j % 2]
            S.actv(tm, pg, AF.Silu)
            S.tt("dve", (act, act[:, j, :]), tm, pu, ALU.mult, disjoint=True)
        for dc in range(KC):
            wbf = w2b[n2 % 2]
            n2 += 1
            for q in range(4):
                S.dma((wbf, wbf[:, q * 11:(q + 1) * 11, :].rearrange("p a b -> p (a b)")),
                      w2t[dc][:, q * 1408:(q + 1) * 1408], eng="pool")
            po = psb[4 + (dc % 2)]
            for fc in range(FC):
                S.mm(po, (wbf, wbf[:, fc, :]), (act, act[:, fc, :]), start=(fc == 0), stop=(fc == FC - 1))
            S.actv((bufX, bufX[:, dc, :]), po, AF.Copy, disjoint=True)
            q_ = sq[dc % 2]
            S.actv(q_, po, AF.Square)
            S.mm(ssb, ones, q_, start=(dc == 0), stop=(dc == KC - 1))
        postnorm_residual(S, g, hsrc, hdst, tok, bufX, rstd, tmp[0], epsb, ssb, vec, gpost, hre, hout)
    S.end_phase()


def inproj_phase(S, g, l, h):
    NT = g.T // TT
    S.begin_phase()
    ones, epsb = phase_consts(S, g)
    vec = load_vec(S, g, l)
    bufX = S.sb("bufX", [128, KC, TT], F32)
    xn = S.sb("xn", [128, KC, TT], BF16)
    wb = [S.sb("wb%d" % i, [128, KC, 128], BF16) for i in range(6)]
    sq = [S.sb("sq%d" % i, [128, TT], BF16) for i in range(2)]
    tmp = S.sb("tmp", [128, TT], F32)
    rstd = S.sb("rstd", [128, TT], F32)
    ost = [S.sb("ost%d" % i, [128, TT], BF16) for i in range(4)]
    psb = [S.ps("psb%d" % i, [128, 512], F32) for i in range(5)]
    ssb = psb[4]
    nw = 0
    for tt in range(NT):
        tok = slice(tt * TT, (tt + 1) * TT)
        prenorm_tile(S, g, h, tok, bufX, xn, sq, tmp, rstd, ones, epsb, ssb, vec, 32)
        S.dma(g.uT.rearrange("(kc p) t -> p kc t", p=128)[:, :, tok], xn)
        for c in range(MIXC):
            w = wb[nw % 6]
            S.dma((w, w[:].rearrange("p a b -> p (a b)")), g.wint[l][c], eng="pool", disjoint=False)
            pp = psb[nw % 4]
            o = ost[nw % 4]
            for kc in range(KC):
                S.mm(pp, (w, w[:, kc, :]), (xn, xn[:, kc, :]), start=(kc == 0), stop=(kc == KC - 1))
            S.copy("act" if nw % 2 == 0 else "dve", o, pp)
            S.dma(g.projT[c * 128:(c + 1) * 128, tok], o)
            nw += 1
    S.end_phase()


BR_K = (4, 4, 2, 4)


def merge_phase(S, g, l, h):
    NT = g.T // TT
    S.begin_phase()
    ones, epsb = phase_consts(S, g)
    vec = load_vec(S, g, l)
    bufX = S.sb("bufX", [128, KC, TT], F32)
    uT = S.sb("uT", [128, KC, TT], BF16)
    yT = S.sb("yT", [128, 14, TT], BF16)
    mg = S.sb("mg", [128, KC, TT], BF16)
    wg = [S.sb("wg%d" % i, [128, KC, 128], BF16) for i in range(6)]
    wbr = [S.sb("wbr%d" % i, [128, 14, 128], BF16) for i in range(2)]
    wo = [S.sb("wo%d" % i, [128, KC, 128], BF16) for i in range(2)]
    sg = [S.sb("sg%d" % i, [128, TT], F32) for i in range(2)]
    acc = S.sb("acc", [128, TT], F32)
    t2 = S.sb("t2", [128, TT], F32)
    sq = [S.sb("sq%d" % i, [128, TT], BF16) for i in range(2)]
    tmp = S.sb("tmp", [128, TT], F32)
    rstd = S.sb("rstd", [128, TT], F32)
    hre = [S.sb("hre%d" % i, [128, TT], F32) for i in range(2)]
    hout = [S.sb("hout%d" % i, [128, TT], F32) for i in range(2)]
    psb = [S.ps("psb%d" % i, [128, 512], F32) for i in range(7)]
    ssb = psb[6]
    ng = 0
    nb = 0
    no = 0
    for tt in range(NT):
        tok = slice(tt * TT, (tt + 1) * TT)
        S.dma(uT, g.uT.rearrange("(kc p) t -> p kc t", p=128)[:, :, tok], disjoint=False)
        S.dma(yT, g.yT.rearrange("(rc p) t -> p rc t", p=128)[:, :, tok], disjoint=False)
        for dc in range(KC):
            wbt = wbr[nb % 2]
            nb += 1
            S.dma((wbt, wbt[:].rearrange("p a b -> p (a b)")), g.wbt[l][dc], eng="pool", disjoint=False)
            rc0 = 0
            for i in range(4):
                w = wg[ng % 6]
                S.dma((w, w[:].rearrange("p a b -> p (a b)")), g.wint[l][MIXC + i * 16 + dc], eng="pool", disjoint=False)
                pgt = psb[(ng % 2) * 2]
                ptm = psb[(ng % 2) * 2 + 1]
                s_ = sg[ng % 2]
                ng += 1
                for kc in range(KC):
                    S.mm(pgt, (w, w[:, kc, :]), (uT, uT[:, kc, :]), start=(kc == 0), stop=(kc == KC - 1))
                nk = BR_K[i]
                for r in range(nk):
                    S.mm(ptm, (wbt, wbt[:, rc0 + r, :]), (yT, yT[:, rc0 + r, :]), start=(r == 0), stop=(r == nk - 1))
                rc0 += nk
                S.actv(s_, pgt, AF.Sigmoid)
                if i == 0:
                    S.tt("dve", acc, s_, ptm, ALU.mult)
                elif i < 3:
                    S.tt("dve", t2, s_, ptm, ALU.mult)
                    S.tt("dve", acc, acc, t2, ALU.add)
                else:
                    S.tt("dve", t2, s_, ptm, ALU.mult)
                    S.tt("dve", (mg, mg[:, dc, :]), acc, t2, ALU.add, disjoint=True)
        for dc in range(KC):
            w = wo[no % 2]
            no += 1
            S.dma((w, w[:].rearrange("p a b -> p (a b)")), g.wot[l][dc], eng="pool", disjoint=False)
            po = psb[4 + (dc % 2)]
            for kc in range(KC):
                S.mm(po, (w, w[:, kc, :]), (mg, mg[:, kc, :]), start=(kc == 0), stop=(kc == KC - 1))
            S.actv((bufX, bufX[:, dc, :]), po, AF.Copy, disjoint=True)
            q_ = sq[dc % 2]
            S.actv(q_, po, AF.Square)
            S.mm(ssb, ones, q_, start=(dc == 0), stop=(dc == KC - 1))
        postnorm_residual(S, g, h, h, tok, bufX, rstd, tmp, epsb, ssb, vec, 48, hre, hout)
    S.end_phase()


import math, os

C_PATTERNS = ((128, 1), (512, 4), (2048, 16))
CST_BI = 0
CST_ID = 1024
CST_NTI = 1152
CST_NSL = 1280
CST_MB = 1408
CST_M2 = 3456
NCST = 3584
NEG = -30000.0
LAYERS_A = 4


def _rel_bucket(dist):
    dist = np.asarray(dist)
    d = np.maximum(dist, 1).astype(np.float32)
    large = 16 + (np.log(d / np.float32(16)) / np.float32(math.log(2048 / 16)) * np.float32(16)).astype(np.int32)
    large = np.minimum(large, 31)
    return np.where(dist < 16, dist, large)


def bi_tile(kind, pc):
    k = np.arange(128)[:, None]
    q = np.arange(128)[None, :]
    du = q - k + (128 if pc == 0 else 0)
    if kind == 'D':
        valid = (du >= 0) & (du < 128)
        r = 1
    else:
        valid = (du >= 0) & (du <= 128)
        r = C_PATTERNS[kind][1]
    b = _rel_bucket(np.maximum(du, 0) * r)
    return np.where(valid, b, -1).astype(np.float32)


BI_KINDS = [('D', 0), ('D', 1), (0, 0), (0, 1), (1, 0), (1, 1), (2, 0), (2, 1)]


def make_consts():
    c = np.zeros((128, NCST), np.float32)
    for i, (kind, pc) in enumerate(BI_KINDS):
        c[:, CST_BI + i * 128:CST_BI + (i + 1) * 128] = bi_tile(kind, pc)
    c[:, CST_ID:CST_ID + 128] = np.eye(128, dtype=np.float32)
    j = np.arange(128)[:, None]
    s = np.arange(128)[None, :]
    c[:, CST_NTI:CST_NTI + 128] = np.where(j >= s, -1.0, 0.0)
    c[:, CST_NSL:CST_NSL + 128] = np.where(j < s, -1.0, 0.0)
    col = np.arange(512)[None, :]
    for m in range(4):
        c[:, CST_MB + m * 512:CST_MB + (m + 1) * 512] = np.where(128 * m + j < col, 1.0, 0.0)
    c[:, CST_M2:CST_M2 + 128] = np.where((j // 64 == s // 64) & (j <= s), 1.0, 0.0)
    return c


def setup_phase(S, g):
    S.begin_phase()
    bi = S.sb("bi", [128, 8, 128], F32)
    S.dma(bi, g.cst[:, CST_BI:CST_BI + 1024].rearrange("p (a b) -> p a b", b=128), disjoint=False)
    relb = S.sb("relb", [128, 640], F32)
    S.dma(relb, g.relb, disjoint=False)
    ebD = S.sb("ebD", [128, 2, 8, 128], F32)
    ebC = S.sb("ebC", [128, 3, 2, 2, 2, 128], F32)
    tmps = [S.sb("tb%d" % i, [128, 128], F32) for i in range(4)]
    n = 0
    for ti, (kind, pc) in enumerate(BI_KINDS):
        tile_np = bi_tile(kind, pc)
        buckets = sorted(set(int(v) for v in np.unique(tile_np) if v >= 0))
        nh = 8 if kind == 'D' else 4
        for h in range(nh):
            if kind == 'D':
                dst = (ebD, ebD[:, pc, h, :])
                col = 12 + h
                eng = "dve"
            else:
                dst = (ebC, ebC[:, kind, h // 2, h % 2, pc, :])
                col = kind * 4 + h
                eng = "dve"
            src = (bi, bi[:, ti, :])
            S.ts(eng, dst, src, 0.0, NEG, op0=ALU.is_lt, op1=ALU.mult, disjoint=True)
            for b in buckets:
                tm = tmps[(n % 2) + (0 if eng == "dve" else 2)]
                n += 1
                S.ts(eng, tm, src, float(b), (relb, relb[:, b * 20 + col:b * 20 + col + 1]), op0=ALU.is_equal, op1=ALU.mult)
                S.tt(eng, dst, dst, tm, ALU.add, disjoint=True)
    S.dma(g.ebD, (ebD, ebD[:].rearrange("p a b c -> p (a b c)")))
    S.dma(g.ebC, (ebC, ebC[:].rearrange("p a b c d e -> p (a b c d e)")))
    S.end_phase()


def load_ident(S, g):
    idf = S.sb("idf", [128, 128], F32)
    S.dma(idf, g.cst[:, CST_ID:CST_ID + 128], disjoint=False)
    idb = S.sb("idb", [128, 128], BF16)
    S.copy("dve", idb, idf)
    return idb


def to_tokmajor(S, src, dst, NB, idb, pT, cnt, engs=("act", "dve")):
    for n in range(NB):
        p = pT[cnt[0] % len(pT)]
        S.tr(p, (src, src[:, n * 128:(n + 1) * 128]), idb)
        S.copy(engs[cnt[0] % len(engs)], (dst, dst[:, n, :]), p, disjoint=True)
        cnt[0] += 1


def mixD_phase(S, g, l):
    T = g.T
    NB = T // 128
    S.begin_phase()
    idb = load_ident(S, g)
    ones64 = S.sb("ones64", [128, 64], BF16)
    S.memset("dve", ones64, 1.0)
    qD = S.sb("qD", [64, 8, T], BF16)
    kD = S.sb("kD", [64, 2, T], BF16)
    vT = S.sb("vT", [128, T], BF16)
    Vtok = S.sb("Vtok", [128, NB, 128], BF16)
    yD = S.sb("yD", [64, 8, T], BF16)
    eb = S.sb("eb", [128, 2, 8, 128], F32)
    S.dma((eb, eb[:].rearrange("p a b c -> p (a b c)")), g.ebD, disjoint=False)
    sk = S.sb("sk", [64, 8], F32)
    S.dma(sk, g.sinks[0:64, l * 8:(l + 1) * 8], disjoint=False)
    es = S.sb("es", [64, 8], F32)
    S.actv(es, sk, AF.Exp)
    esb = S.sb("esb", [64, 8, 128], F32)
    S.copy("dve", esb, (es, es[:, :].unsqueeze(2).to_broadcast([64, 8, 128])))
    for h in range(8):
        S.dma((qD, qD[:, h, :]), g.projT[R_DQ + h * 64:R_DQ + (h + 1) * 64, :])
    for kv in range(2):
        S.dma((kD, kD[:, kv, :]), g.projT[R_DK + kv * 64:R_DK + (kv + 1) * 64, :])
    S.dma(vT, g.projT[R_DV:R_DV + 128, :], disjoint=False)
    pT = [S.ps("pT%d" % i, [128, 128], BF16) for i in range(2)]
    pS = [S.ps("pS%d" % i, [128, 2, 512], F32) for i in range(2)]
    pO = S.ps("pO", [128, 512], F32)
    pD = S.ps("pD", [128, 512], F32)
    Zs = [S.sb("Zs%d" % i, [128, 2, 512], F32) for i in range(2)]
    Pb = [S.sb("Pb%d" % i, [128, 2, 512], BF16) for i in range(2)]
    dt = S.sb("dt", [64, 512], F32)
    cnt = [0]
    to_tokmajor(S, vT, Vtok, NB, idb, pT, cnt)
    it = 0
    for n in range(NB):
        for gk in range(2):
            Sp = pS[it % 2]
            Z = Zs[it % 2]
            P = Pb[it % 2]
            it += 1
            rq = (qD, qD[:, 4 * gk:4 * gk + 4, n * 128:(n + 1) * 128])
            lo = 0 if n > 0 else 1
            if n > 0:
                S.mm((Sp, Sp[:, 0, :]), (kD, kD[:, gk, (n - 1) * 128:n * 128]), rq)
            S.mm((Sp, Sp[:, 1, :]), (kD, kD[:, gk, n * 128:(n + 1) * 128]), rq, start=True)
            S.stt((Z, Z[:, lo:2, :].rearrange("p a (h q) -> p a h q", h=4)),
                  (Sp, Sp[:, lo:2, :].rearrange("p a (h q) -> p a h q", h=4)), 0.125,
                  (eb, eb[:, lo:2, 4 * gk:4 * gk + 4, :]), ALU.mult, ALU.add)
            S.actv((P, P[:, lo:2, :]), (Z, Z[:, lo:2, :]), AF.Exp)
            for (pp, lhs_of) in ((pO, None), (pD, ones64)):
                if n > 0:
                    lh = (Vtok, Vtok[:, n - 1, gk * 64:(gk + 1) * 64]) if lhs_of is None else ones64
                    S.mm((pp, pp[0:64, :]), lh, (P, P[:, 0, :]), start=True, stop=False)
                lh = (Vtok, Vtok[:, n, gk * 64:(gk + 1) * 64]) if lhs_of is None else ones64
                S.mm((pp, pp[0:64, :]), lh, (P, P[:, 1, :]), start=(n == 0), stop=True)
            S.tt("dve", (dt, dt[:, :].rearrange("p (h q) -> p h q", h=4)),
                 (pD, pD[0:64, :].rearrange("p (h q) -> p h q", h=4)), (esb, esb[:, 4 * gk:4 * gk + 4, :]), ALU.add)
            S.recip(dt, dt)
            S.tt("dve", (yD, yD[:, 4 * gk:4 * gk + 4, n * 128:(n + 1) * 128]),
                 (pO, pO[0:64, :].rearrange("p (h q) -> p h q", h=4)),
                 (dt, dt[:, :].rearrange("p (h q) -> p h q", h=4)), ALU.mult, disjoint=True)
    for h in range(8):
        S.dma(g.yT[Y_D + h * 64:Y_D + (h + 1) * 64, :], (yD, yD[:, h, :]))
    S.end_phase()


def mixC_phase(S, g, l):
    T = g.T
    NB = T // 128
    S.begin_phase()
    idb = load_ident(S, g)
    ones64 = S.sb("ones64", [128, 64], BF16)
    S.memset("dve", ones64, 1.0)
    eb = S.sb("eb", [128, 3, 2, 2, 2, 128], F32)
    S.dma((eb, eb[:].rearrange("p a b c d e -> p (a b c d e)")), g.ebC, disjoint=False)
    natq = [S.sb("natq%d" % i, [64, 2, T], BF16) for i in range(2)]
    perq = [S.sb("perq%d" % i, [64, 2, T], BF16) for i in range(2)]
    natv = S.sb("natv", [128, T], BF16)
    perv = S.sb("perv", [128, T], BF16)
    Vtok = S.sb("Vtok", [128, NB, 128], BF16)
    accN = S.sb("accN", [64, 2, T], F32)
    accD = S.sb("accD", [64, 2, T], F32)
    ybf = S.sb("ybf", [64, 2, T], BF16)
    pT = [S.ps("pT%d" % i, [128, 128], BF16) for i in range(2)]
    pS = [S.ps("pS%d" % i, [128, 2, 2, 128], F32) for i in range(2)]
    pO = [S.ps("pO%d" % i, [128, 512], F32) for i in range(2)]
    pD = [S.ps("pD%d" % i, [128, 512], F32) for i in range(2)]
    Zs = [S.sb("Zs%d" % i, [128, 2, 2, 128], F32) for i in range(2)]
    Pb = [S.sb("Pb%d" % i, [128, 2, 2, 128], BF16) for i in range(2)]
    cnt = [0]
    it = 0
    rows = (R_CQ, R_CK, R_CV)
    for hp in range(2):
        for gi, (win, r) in enumerate(C_PATTERNS):
            cur = []
            for j in range(2):
                r0 = rows[j] + gi * 256 + hp * 128
                for hh in range(2):
                    S.dma((natq[j], natq[j][:, hh, :]), g.projT[r0 + hh * 64:r0 + (hh + 1) * 64, :], disjoint=(hh == 1))
                if r > 1:
                    S.copy("act" if j == 0 else "dve", (perq[j], perq[j][:, :, :].rearrange("p h (c i) -> p h c i", c=r)),
                           (natq[j], natq[j][:, :, :].rearrange("p h (i c) -> p h c i", c=r)))
                    cur.append(perq[j])
                else:
                    cur.append(natq[j])
            r0 = rows[2] + gi * 256 + hp * 128
            S.dma(natv, g.projT[r0:r0 + 128, :], disjoint=False)
            if r > 1:
                S.copy("act", (perv, perv[:, :].rearrange("p (c i) -> p c i", c=r)),
                       (natv, natv[:, :].rearrange("p (i c) -> p c i", c=r)))
                vp = perv
            else:
                vp = natv
            qp, kp = cur
            to_tokmajor(S, vp, Vtok, NB, idb, pT, cnt)
            Lb = NB // r
            for c in range(r):
                for n in range(Lb):
                    pb = c * Lb + n
                    Sp = pS[it % 2]
                    Z = Zs[it % 2]
                    P = Pb[it % 2]
                    po = pO[it % 2]
                    pd = pD[it % 2]
                    it += 1
                    for hh in range(2):
                        rq = (qp, qp[:, hh, pb * 128:(pb + 1) * 128])
                        if n > 0:
                            S.mm((Sp, Sp[:, hh, 0, :]), (kp, kp[:, hh, (pb - 1) * 128:pb * 128]), rq)
                        S.mm((Sp, Sp[:, hh, 1, :]), (kp, kp[:, hh, pb * 128:(pb + 1) * 128]), rq)
                    if n > 0:
                        S.stt(Z, Sp, 0.125, (eb, eb[:, gi, hp, :, :, :]), ALU.mult, ALU.add)
                        S.actv(P, Z, AF.Exp)
                    else:
                        S.stt((Z, Z[:, :, 1, :]), (Sp, Sp[:, :, 1, :]), 0.125, (eb, eb[:, gi, hp, :, 1, :]), ALU.mult, ALU.add)
                        S.actv((P, P[:, :, 1, :]), (Z, Z[:, :, 1, :]), AF.Exp)
                    for hh in range(2):
                        vs = slice(hh * 64, (hh + 1) * 64)
                        cs = slice(hh * 128, (hh + 1) * 128)
                        for (pp, isden) in ((po, False), (pd, True)):
                            if n > 0:
                                lh = ones64 if isden else (Vtok, Vtok[:, pb - 1, vs])
                                S.mm((pp, pp[0:64, cs]), lh, (P, P[:, hh, 0, :]), start=True, stop=False)
                            lh = ones64 if isden else (Vtok, Vtok[:, pb, vs])
                            S.mm((pp, pp[0:64, cs]), lh, (P, P[:, hh, 1, :]), start=(n == 0), stop=True)
                    t0 = c + r * 128 * n
                    sl = slice(t0, t0 + r * 127 + 1, r) if r > 1 else slice(t0, t0 + 128)
                    for (acc, pp, eng) in ((accN, po, "dve"), (accD, pd, "act")):
                        av = (acc, acc[:, :, sl])
                        pv = (pp, pp[0:64, 0:256].rearrange("p (h q) -> p h q", h=2))
                        if gi == 0:
                            S.copy(eng, av, pv, disjoint=True)
                        else:
                            S.tt("dve", av, av, pv, ALU.add, disjoint=True)
        S.recip(accD, accD)
        S.tt("dve", ybf, accN, accD, ALU.mult)
        for hh in range(2):
            r0 = Y_C + (2 * hp + hh) * 64
            S.dma(g.yT[r0:r0 + 64, :], (ybf, ybf[:, hh, :]))
    S.end_phase()


def load_cst_bf(S, g, name, c0, n):
    f = S.sb(name + "f", [128, n], F32)
    S.dma(f, g.cst[:, c0:c0 + n], disjoint=False)
    b = S.sb(name + "b", [128, n], BF16)
    S.copy("dve", b, f)
    return f, b


def mixB_phase(S, g, l):
    T = g.T
    NB = T // 128
    NQ = T // 512
    S.begin_phase()
    idb = load_ident(S, g)
    _, nti = load_cst_bf(S, g, "nti", CST_NTI, 128)
    _, nsl = load_cst_bf(S, g, "nsl", CST_NSL, 128)
    maskB = S.sb("maskB", [128, 4, 512], F32)
    S.dma((maskB, maskB[:].rearrange("p a b -> p (a b)")), g.cst[:, CST_MB:CST_MB + 2048], disjoint=False)
    oneb = S.sb("oneb", [128, 1], F32)
    S.memset("dve", oneb, 1.0)
    qh = S.sb("qh", [64, 2, T], BF16)
    kh = S.sb("kh", [64, 2, T], BF16)
    vT = S.sb("vT", [128, T], BF16)
    Vtok = S.sb("Vtok", [128, NB, 128], BF16)
    ybf = S.sb("ybf", [64, 2, T], BF16)
    NPS = int(os.environ.get("MK_BNPS", "3"))
    NST = int(os.environ.get("MK_BNST", "3" if NPS == 1 else "2"))
    pSs = [S.ps("pS%d" % i, [128, 512], F32) for i in range(NPS)]
    pT = [S.ps("pT%d" % i, [128, 128], BF16) for i in range(1)]
    sets = []
    pscnt = [0]
    for k in range(NST):
        st = G()
        st.pB = S.ps("pB%d" % k, [128, 512], F32)
        st.pO = S.ps("pO%d" % k, [128, 512], F32)
        st.e = [S.sb("e%d_%d" % (k, i), [128, 512], F32) for i in range(2)]
        st.sp = [S.sb("sp%d_%d" % (k, i), [128, 512], BF16) for i in range(2)]
        st.w = [S.sb("w%d_%d" % (k, i), [128, 512], F32) for i in range(2)]
        st.a = [S.sb("a%d_%d" % (k, i), [128, 512], BF16) for i in range(2)]
        sets.append(st)
    cnt = [0]

    def chain(st, hh, qt):
        qs = (qh, qh[:, hh, qt * 512:(qt + 1) * 512])
        kbs = list(range(4 * qt + 3, -1, -1))
        nk = len(kbs)

        def s_and_exp(i):
            kb = kbs[i]
            m = kb - 4 * qt
            pS = pSs[pscnt[0] % len(pSs)]
            pscnt[0] += 1
            e = st.e[i % 2]
            S.mm(pS, (kh, kh[:, hh, kb * 128:(kb + 1) * 128]), qs)
            S.actv(e, pS, AF.Exp, scale=0.125)
            if m >= 0:
                S.tt("dve", e, e, (maskB, maskB[:, m, :]), ALU.mult)

        s_and_exp(0)
        yield
        for i in range(nk):
            kb = kbs[i]
            e = st.e[i % 2]
            sp = st.sp[i % 2]
            w = st.w[i % 2]
            a = st.a[i % 2]
            first = (i == 0)
            last = (i == nk - 1)
            S.actv(sp, e, AF.Ln, bias=(oneb, oneb[:, 0:1]))
            yield
            if not last:
                s_and_exp(i + 1)
            yield
            S.mm(st.pB, nti, sp, start=first, stop=False)
            yield
            S.actv(w, st.pB, AF.Exp)
            yield
            S.mm(st.pB, nsl, sp, start=False, stop=last)
            S.tt("dve", a, w, e, ALU.mult)
            yield
            S.mm((st.pO, st.pO[0:64, :]), (Vtok, Vtok[:, kb, hh * 64:(hh + 1) * 64]), a, start=first, stop=last)
            yield
        S.copy("act", (ybf, ybf[:, hh, qt * 512:(qt + 1) * 512]), (st.pO, st.pO[0:64, :]), disjoint=True)
        yield

    for hp in range(4):
        for hh in range(2):
            S.dma((qh, qh[:, hh, :]), g.projT[R_BQ + hp * 128 + hh * 64:R_BQ + hp * 128 + (hh + 1) * 64, :], disjoint=(hh == 1))
            S.dma((kh, kh[:, hh, :]), g.projT[R_BK + hp * 128 + hh * 64:R_BK + hp * 128 + (hh + 1) * 64, :], disjoint=(hh == 1))
        S.dma(vT, g.projT[R_BV + hp * 128:R_BV + (hp + 1) * 128, :], disjoint=False)
        to_tokmajor(S, vT, Vtok, NB, idb, pT, cnt)
        work = [(hh, qt) for qt in range(NQ - 1, -1, -1) for hh in range(2)]
        active = []
        free_sets = list(sets)
        while work or active:
            while work and free_sets:
                hh, qt = work.pop(0)
                st = free_sets.pop(0)
                active.append((chain(st, hh, qt), st))
            nxt = []
            for gen, st in active:
                try:
                    next(gen)
                    nxt.append((gen, st))
                except StopIteration:
                    free_sets.append(st)
            active = nxt
        for hh in range(2):
            r0 = Y_B + hp * 128 + hh * 64
            S.dma(g.yT[r0:r0 + 64, :], (ybf, ybf[:, hh, :]))
    S.end_phase()


def mixA_phase(S, g, l):
    T = g.T
    SEG = min(1024, T)
    NSEG = T // SEG
    NBS = SEG // 128
    NCH = SEG // 64
    S.begin_phase()
    idb = load_ident(S, g)
    ones, epsb = phase_consts(S, g)
    vec = load_vec(S, g, l)
    m2 = S.sb("m2", [128, 128], F32)
    S.dma(m2, g.cst[:, CST_M2:CST_M2 + 128], disjoint=False)
    cmask = S.sb("cmask", [128, SEG], F32)
    S.memset("dve", cmask, 1.0)
    S.memset("dve", (cmask, cmask[:, 0:SEG:64]), 0.0)
    lbl = S.sb("lbl", [128, 4, LAYERS_A], F32)
    S.dma((lbl, lbl[:].rearrange("p a b -> p (a b)")), g.lbl, disjoint=False)
    le = S.sb("le", [128, 4, LAYERS_A], F32)
    S.actv(le, lbl, AF.Exp)
    lsum = S.sb("lsum", [128, 4], F32)
    S.tt("dve", lsum, (le, le[:, :, 0]), (le, le[:, :, 1]), ALU.add)
    for i in range(2, LAYERS_A):
        S.tt("dve", lsum, lsum, (le, le[:, :, i]), ALU.add)
    S.recip(lsum, lsum)
    lb = S.sb("lb", [128, 4], F32)
    oml = S.sb("oml", [128, 4], F32)
    S.memset("dve", lb, 0.0)
    for i in range(1, l + 1):
        S.tt("dve", lb, lb, (le, le[:, :, i]), ALU.add)
    S.tt("dve", lb, lb, lsum, ALU.mult)
    S.ts("dve", oml, lb, -1.0, 1.0, op0=ALU.mult, op1=ALU.add)
    qn = S.sb("qn", [128, SEG], BF16)
    fn = S.sb("fn", [128, SEG], BF16)
    vn = S.sb("vn", [128, SEG], BF16)
    khat = S.sb("khat", [128, SEG], BF16)
    t1 = S.sb("t1", [128, SEG], F32)
    t2 = S.sb("t2", [128, SEG], F32)
    t3 = S.sb("t3", [128, SEG], F32)
    t4 = S.sb("t4", [128, SEG], F32)
    bb = S.sb("bb", [128, SEG], F32)
    sqb = S.sb("sqb", [128, SEG], BF16)
    H = []
    for h in range(4):
        hs = G()
        hs.gn = S.sb("gn%d" % h, [128, SEG], BF16)
        hs.ebt = S.sb("ebt%d" % h, [128, SEG], F32)
        hs.qt = S.sb("qt%d" % h, [128, SEG], BF16)
        hs.kt = S.sb("kt%d" % h, [128, SEG], BF16)
        hs.qb = S.sb("qb%d" % h, [128, SEG], BF16)
        hs.Vt = S.sb("Vt%d" % h, [128, NBS, 128], BF16)
        hs.Kt = S.sb("Kt%d" % h, [128, NBS, 128], BF16)
        hs.oT = S.sb("oT%d" % h, [128, SEG], F32)
        hs.Sst = S.sb("Sst%d" % h, [128, 128], F32)
        hs.Sb = [S.sb("Sb%d_%d" % (h, i), [128, 128], BF16) for i in range(2)]
        hs.Pm = S.sb("Pm%d" % h, [128, 128], BF16)
        S.memset("dve", hs.Sst, 0.0)
        S.memset("dve", hs.Sb[0], 0.0)
        H.append(hs)
    pT = [S.ps("pT%d" % i, [128, 128], BF16) for i in range(2)]
    bsc = S.ps("bsc", [128, 4, 128], F32)
    bua = S.ps("bua", [128, 4, 128], F32)
    bub = S.ps("bub", [128, 4, 128], F32)
    bo = S.ps("bo", [128, 4, 128], F32)
    bss = S.ps("bss", [128, 512], F32)
    for h in range(4):
        H[h].psc = Buf("psc%d" % h, bsc.t, bsc.lock)
        H[h].pua = Buf("pua%d" % h, bua.t, bua.lock)
        H[h].pub = Buf("pub%d" % h, bub.t, bub.lock)
        H[h].po = Buf("po%d" % h, bo.t, bo.lock)
    cnt = [0]
    ysb = [S.sb("ysb%d" % i, [128, SEG], BF16) for i in range(2)]
    for seg in range(NSEG):
        cs = slice(seg * SEG, (seg + 1) * SEG)
        for h in range(4):
            hs = H[h]
            S.dma(qn, g.projT[R_AQ + h * 128:R_AQ + (h + 1) * 128, cs], disjoint=False)
            S.dma(fn, g.projT[R_AF + h * 128:R_AF + (h + 1) * 128, cs], disjoint=False)
            S.dma(vn, g.projT[R_AI + h * 128:R_AI + (h + 1) * 128, cs], disjoint=False)
            S.dma(hs.gn, g.projT[R_AG + h * 128:R_AG + (h + 1) * 128, cs], disjoint=False)
            S.actv(t1, fn, AF.Exp, scale=-1.0)
            S.ts("dve", t1, t1, 1.0, None, op0=ALU.add)
            S.recip(t1, t1)
            S.ts("dve", t1, t1, (oml, oml[:, h:h + 1]), (lb, lb[:, h:h + 1]), op0=ALU.mult, op1=ALU.add)
            S.actv(t2, t1, AF.Ln)
            S.ts("dve", t3, t1, -1.0, 1.0, op0=ALU.mult, op1=ALU.add)
            S.op("dve", "tensor_tensor_scan", dict(out=bb[:], data0=cmask[:], data1=t2[:], initial=0.0,
                                                   op0=ALU.mult, op1=ALU.add), reads=[cmask, t2], writes=[bb])
            bv = bb[:, :].rearrange("p (c i) -> p c i", i=64)
            bmid = bv[:, :, 31:32].to_broadcast([128, NCH, 64])
            blast = bv[:, :, 63:64].to_broadcast([128, NCH, 64])
            v3 = lambda b_: (b_, b_[:, :].rearrange("p (c i) -> p c i", i=64))
            S.tt("dve", v3(t2), (bb, bv), (bb, bmid), ALU.subtract)
            S.actv(t4, t2, AF.Exp)
            S.tt("dve", hs.qt, qn, t4, ALU.mult)
            S.actv(t4, t2, AF.Exp, scale=-1.0)
            S.tt("dve", hs.kt, t3, t4, ALU.mult)
            S.tt("dve", v3(t2), (bb, bv), (bb, blast), ALU.subtract)
            S.actv(t4, t2, AF.Exp, scale=-1.0)
            S.tt("dve", khat, t3, t4, ALU.mult)
            S.actv(hs.ebt, bb, AF.Exp)
            S.tt("dve", hs.qb, qn, hs.ebt, ALU.mult)
            if g.dbg is not None and h == 0 and seg == 0:
                S.dma(g.dbg[0], t1); S.dma(g.dbg[1], bb); S.dma(g.dbg[2], hs.ebt); S.dma(g.dbg[3], t3)
                S.copy("dve", t4, hs.qt); S.dma(g.dbg[4], t4)
            to_tokmajor(S, vn, hs.Vt, NBS, idb, pT, cnt)
            to_tokmajor(S, khat, hs.Kt, NBS, idb, pT, cnt)
        for n in range(NBS):
            bs = slice(n * 128, (n + 1) * 128)
            for h in range(4):
                hs = H[h]
                S.mm((hs.psc, bsc[:, h, :]), (hs.kt, hs.kt[:, bs]), (hs.qt, hs.qt[:, bs]))
                S.mm((hs.pua, bua[0:128, h, :]), (hs.Kt, hs.Kt[0:64, n, :]), (hs.Vt, hs.Vt[0:64, n, :]))
                S.mm((hs.pub, bub[0:128, h, :]), (hs.Kt, hs.Kt[64:128, n, :]), (hs.Vt, hs.Vt[64:128, n, :]))
            for h in range(4):
                hs = H[h]
                S.tt("dve", hs.Pm, (hs.psc, bsc[:, h, :]), m2, ALU.mult)
                cA = (2 * n) * 64 + 63
                S.stt(hs.Sst, hs.Sst, (hs.ebt, hs.ebt[:, cA:cA + 1]), (hs.pua, bua[:, h, :]), ALU.mult, ALU.add)
                S.copy("act", hs.Sb[1], hs.Sst)
            for h in range(4):
                hs = H[h]
                S.mm((hs.po, bo[:, h, :]), (hs.Vt, hs.Vt[:, n, :]), hs.Pm, start=True, stop=False)
                S.mm((hs.po, bo[:, h, 0:64]), hs.Sb[0], (hs.qb, hs.qb[:, n * 128:n * 128 + 64]), start=False, stop=False)
                S.mm((hs.po, bo[:, h, 64:128]), hs.Sb[1], (hs.qb, hs.qb[:, n * 128 + 64:(n + 1) * 128]), start=False, stop=True)
            for h in range(4):
                hs = H[h]
                cB = (2 * n + 1) * 64 + 63
                S.stt(hs.Sst, hs.Sst, (hs.ebt, hs.ebt[:, cB:cB + 1]), (hs.pub, bub[:, h, :]), ALU.mult, ALU.add)
                S.copy("act", hs.Sb[0], hs.Sst)
                S.copy("act", (hs.oT, hs.oT[:, bs]), (hs.po, bo[:, h, :]), disjoint=True)
        for h in range(4):
            hs = H[h]
            yb = ysb[h % 2]
            for c in range(SEG // 512):
                c5 = slice(c * 512, (c + 1) * 512)
                S.actv((sqb, sqb[:, c5]), (hs.oT, hs.oT[:, c5]), AF.Square)
                S.mm(bss, ones, (sqb, sqb[:, c5]))
                S.actv((t2, t2[:, c5]), bss, AF.Sqrt, scale=1.0 / 128, bias=(epsb, epsb[:, 0:1]))
            S.recip(t2, t2)
            if g.dbg is not None and h == 0 and seg == 0:
                S.dma(g.dbg[5], hs.oT); S.dma(g.dbg[6], t2)
            S.stt(t3, hs.oT, (vec, vec[:, 96:97]), t2, ALU.mult, ALU.mult)
            S.actv(t4, hs.gn, AF.Silu)
            S.tt("dve", yb, t3, t4, ALU.mult)
            S.dma(g.yT[Y_A + h * 128:Y_A + (h + 1) * 128, cs], yb)
    S.end_phase()


LAYERS = 4


def build(T, L, phases=None, debug=False, ext_in=()):
    nc = bass.Bass("TRN2", target_bir_lowering=False)
    g = G()
    g.nc = nc
    g.T = T
    g.L = L

    def din(name, shape, dt=F32):
        return nc.dram_tensor(name, list(shape), dt, kind="ExternalInput").ap()

    g.xT = din("xT", [D, T])
    g.w13t = [din("w13t_a", [L, 88, 128, 2048]), din("w13t_b", [L, 88, 128, 2048])]
    g.w2t = [din("w2t_a", [L, 16, 128, FF]), din("w2t_b", [L, 16, 128, FF])]
    g.wint = din("wint", [L, 116, 128, 2048])
    g.wbt = din("wbt", [L, 16, 128, 1792])
    g.wot = din("wot", [L, 16, 128, 2048])
    g.vecs = din("vecs", [L, 128, NVEC])
    g.lbl = din("lbl", [128, 4 * LAYERS])
    g.sinks = din("sinks", [128, LAYERS * 8])
    g.relb = din("relb", [128, 640])
    g.cst = din("cst", [128, NCST])
    g.outT = nc.dram_tensor("outT", [D, T], F32, kind="ExternalOutput").ap()
    sk = "ExternalOutput" if debug else "Internal"
    def scr(name, shape, dt):
        return nc.dram_tensor(name, shape, dt, kind=("ExternalInput" if name in ext_in else sk)).ap()
    g.projT = scr("projT", [6656, T], BF16)
    g.uT = scr("uT_s", [D, T], BF16)
    g.yT = scr("yT_s", [1792, T], BF16)
    g.ebD = nc.dram_tensor("ebD", [128, 2 * 8 * 128], F32, kind=sk).ap()
    g.ebC = nc.dram_tensor("ebC", [128, 3 * 2 * 2 * 2 * 128], F32, kind=sk).ap()
    g.lbs = nc.dram_tensor("lbs", [128, 4 * LAYERS], F32, kind=sk).ap()
    g.dbg = nc.dram_tensor('dbg', [16, 128, 1024], F32, kind='ExternalOutput').ap() if debug else None
    S = Sched(nc)
    g.S = S
    allp = ["ffn1", "inproj", "A", "B", "C", "D", "merge", "ffn2"]
    if phases is None:
        phases = allp
    if any(p in phases for p in ("A", "C", "D")):
        setup_phase(S, g)
    for l in range(L):
        first = (l == 0)
        for p in phases:
            if p == "ffn1":
                ffn_phase(S, g, l, 0, g.xT if first else g.outT, g.outT)
            elif p == "inproj":
                inproj_phase(S, g, l, g.outT)
            elif p == "A":
                mixA_phase(S, g, l)
            elif p == "B":
                mixB_phase(S, g, l)
            elif p == "C":
                mixC_phase(S, g, l)
            elif p == "D":
                mixD_phase(S, g, l)
            elif p == "merge":
                merge_phase(S, g, l, g.outT)
            elif p == "ffn2":
                ffn_phase(S, g, l, 1, g.outT, g.outT)
    return nc, g


def tile_w(w, kc, nc_):
    return np.ascontiguousarray(w.reshape(kc, 128, nc_, 128).transpose(2, 1, 0, 3).reshape(nc_, 128, kc * 128))


def prep_weights(inp, L):
    out = {}
    out["w13t_a"] = np.stack([tile_w(inp["ffn1_w13"][l], 16, 88) for l in range(L)])
    out["w13t_b"] = np.stack([tile_w(inp["ffn2_w13"][l], 16, 88) for l in range(L)])
    out["w2t_a"] = np.stack([tile_w(inp["ffn1_w2"][l], 44, 16) for l in range(L)])
    out["w2t_b"] = np.stack([tile_w(inp["ffn2_w2"][l], 44, 16) for l in range(L)])
    out["wint"] = np.stack([tile_w(inp["w_in"][l], 16, 116) for l in range(L)])
    out["wbt"] = np.stack([tile_w(inp["w_branch"][l], 14, 16) for l in range(L)])
    out["wot"] = np.stack([tile_w(inp["w_out"][l], 16, 16) for l in range(L)])
    vecs = np.zeros((L, 128, NVEC), np.float32)
    for l in range(L):
        for i, nm in enumerate(("ffn1_norm", "mix_norm", "ffn2_norm")):
            for j in range(2):
                vecs[l, :, (2 * i + j) * 16:(2 * i + j + 1) * 16] = inp[nm][l, j].reshape(16, 128).T
        vecs[l, :, 96] = inp["hgrn_out_norm"][l]
    out["vecs"] = vecs
    lb = np.asarray(inp["hgrn_lb_logits"])
    out["lbl"] = np.ascontiguousarray(lb.reshape(LAYERS, 4, 128).transpose(2, 1, 0).reshape(128, 4 * LAYERS))
    out["sinks"] = np.ascontiguousarray(np.broadcast_to(np.asarray(inp["attn_sinks"]).reshape(1, -1), (128, LAYERS * 8)))
    out["relb"] = np.ascontiguousarray(np.broadcast_to(np.asarray(inp["rel_bias"]).reshape(1, 640), (128, 640)))
    out["cst"] = make_consts()
    return out


_CACHE = {}


def kernel(**inputs):
    x = np.asarray(inputs["x"])
    B, T, _ = x.shape
    L = LAYERS
    key = (T, L)
    if key not in _CACHE:
        _CACHE[key] = build(T, L, None, debug=False)
    nc, g = _CACHE[key]
    w = prep_weights({k: np.asarray(v) for k, v in inputs.items() if k != "x"}, L)
    in_maps = []
    for b in range(B):
        m = dict(w)
        m["xT"] = np.ascontiguousarray(x[b].T)
        in_maps.append(m)
    res = run_bass_kernel_spmd(nc, in_maps, core_ids=list(range(B)))
    out = np.stack([np.ascontiguousarray(res.results[b]["outT"].T) for b in range(B)], axis=0)
    return out.astype(np.float32, copy=False)
```

```python
import numpy as np
import concourse.bass as bass
import concourse.mybir as mybir
from concourse.bass_utils import run_bass_kernel_spmd
from contextlib import ExitStack

F32 = mybir.dt.float32
BF16 = mybir.dt.bfloat16
AF = mybir.ActivationFunctionType
ALU = mybir.AluOpType

import os
SERIAL_PH = [int(x) for x in os.environ.get('MK_SERIAL', '').split(',') if x]
SEM_LIMIT = 30000
NDMA_SEMS = 10


class Buf:
    __slots__ = ("name", "writers", "readers", "t", "lock")

    def __init__(self, name, t=None, lock=None):
        self.name = name
        self.writers = []
        self.readers = []
        self.t = t
        self.lock = lock

    def __getitem__(self, k):
        return self.t[k]


class Op:
    __slots__ = ("eng", "fn", "deps", "is_dma", "needs_inc", "sem", "val", "clock", "idx", "phase")


def _ba(x):
    if isinstance(x, Buf):
        return x, x.t[:]
    return x


class Sched:
    COMPUTE = ("pe", "act", "dve", "pool")
    ALL = ("pe", "act", "dve", "pool", "sp")

    def __init__(self, nc):
        self.nc = nc
        self.ops = []
        self.engs = {"pe": nc.tensor, "act": nc.scalar, "dve": nc.vector,
                     "pool": nc.gpsimd, "sp": nc.sync}
        self.phase = 0
        self.nops = 0
        self.last = {}
        self.dmas = []
        self.clocks = {e: {} for e in self.ALL}
        self.cur_sem = {}
        self.cur_cnt = {}
        self.nsw = {}
        for e in self.COMPUTE:
            self.cur_sem[e] = nc.alloc_semaphore("s_%s_0" % e)
            self.cur_cnt[e] = 0
            self.nsw[e] = 0
        self.dma_sems = {}
        self.dma_cnt = {}
        self.dma_last = {}
        self.dma_rr = {}
        self.nwaits = 0
        self.ninst = {e: 0 for e in self.ALL}
        self.es = None

    def begin_phase(self):
        self.es = ExitStack()
        self.es.__enter__()

    def sb(self, name, shape, dt):
        t = self.es.enter_context(self.nc.sbuf_tensor("%s_p%d" % (name, self.phase), list(shape), dt))
        return Buf(name, t)

    def ps(self, name, shape, dt=F32):
        t = self.es.enter_context(self.nc.psum_tensor("%s_p%d" % (name, self.phase), list(shape), dt))
        b = Buf(name, t)
        b.lock = Buf(name + "_lock")
        return b

    def end_phase(self):
        self.barrier()
        self.emit()
        self.es.__exit__(None, None, None)
        self.es = None
        self.phase += 1

    def op(self, eng, name, kw, reads=(), writes=(), dma=False, disjoint=False):
        o = Op()
        o.eng = eng
        o.fn = (name, kw)
        o.is_dma = dma
        o.needs_inc = dma
        o.sem = None
        o.val = 0
        o.clock = None
        o.idx = self.nops
        o.phase = self.phase
        self.nops += 1
        deps = {}
        locks = []
        for b in reads:
            if b.lock is not None and b.lock not in locks:
                locks.append(b.lock)
        for b in writes:
            if b.lock is not None and b.lock not in locks:
                locks.append(b.lock)
        for b in locks:
            for w in b.writers:
                deps[w.idx] = w
        for b in reads:
            for w in b.writers:
                deps[w.idx] = w
        for b in writes:
            for r in b.readers:
                deps[r.idx] = r
            if (not disjoint) or b.readers:
                for w in b.writers:
                    deps[w.idx] = w
        ph = self.phase
        SERIAL = (ph in SERIAL_PH) or (-1 in SERIAL_PH)
        if SERIAL and self.ops:
            po = self.ops[-1]
            if po.fn is not None:
                deps[po.idx] = po
        if SERIAL:
            o.deps = [d for d in deps.values() if d.phase == ph]
        elif not dma:
            raw = set()
            if eng != "pe":
                for b in reads:
                    for w in b.writers:
                        if (not w.is_dma) and w.eng == eng:
                            raw.add(w.idx)
            o.deps = [d for d in deps.values() if d.phase == ph and (d.is_dma or d.eng != eng or d.idx in raw)]
        else:
            o.deps = [d for d in deps.values() if d.phase == ph]
        for d in o.deps:
            d.needs_inc = True
        for b in reads:
            if not dma:
                b.readers = [r for r in b.readers if r.is_dma or r.eng != eng]
            b.readers.append(o)
        for b in writes:
            if b.readers:
                b.writers = [o]
                b.readers = []
            elif disjoint:
                if not dma:
                    b.writers = [w for w in b.writers if w.is_dma or w.eng != eng]
                b.writers.append(o)
            else:
                b.writers = [o]
        for b in locks:
            b.writers = [o]
        self.ops.append(o)
        if dma:
            self.dmas.append(o)
        else:
            self.last[eng] = o
        return o

    def barrier(self):
        lasts = [o for o in self.last.values() if o.phase == self.phase]
        dmas = self.dmas
        self.dmas = []
        self.last = {}
        for o in lasts:
            o.needs_inc = True
        for e in self.ALL:
            o = Op()
            o.eng = e
            o.fn = None
            o.is_dma = False
            o.needs_inc = False
            o.sem = None
            o.val = 0
            o.clock = None
            o.idx = self.nops
            o.phase = self.phase
            self.nops += 1
            o.deps = [l for l in lasts if l.eng != e] + dmas
            self.ops.append(o)

    def emit(self):
        nc = self.nc
        for o in self.ops:
            e = o.eng
            eng = self.engs[e]
            clk = self.clocks[e]
            deps = list(o.deps)
            slot = None
            if o.is_dma:
                if e not in self.dma_sems:
                    self.dma_sems[e] = [nc.alloc_semaphore("d_%s_%d" % (e, i)) for i in range(NDMA_SEMS)]
                    self.dma_cnt[e] = [0] * NDMA_SEMS
                    self.dma_last[e] = [None] * NDMA_SEMS
                    self.dma_rr[e] = 0
                slot = self.dma_rr[e] % NDMA_SEMS
                self.dma_rr[e] += 1
                if self.dma_last[e][slot] is not None:
                    deps.append(self.dma_last[e][slot])
            if len(deps) > 1:
                deps.sort(key=lambda d: -d.idx)
            for d in deps:
                k = id(d.sem)
                if clk.get(k, (None, 0))[1] >= d.val:
                    continue
                eng.wait_ge(d.sem, d.val)
                self.nwaits += 1
                for kk, vv in d.clock.items():
                    if clk.get(kk, (None, 0))[1] < vv[1]:
                        clk[kk] = vv
            if o.fn is None:
                continue
            ins = getattr(eng, o.fn[0])(**o.fn[1])
            self.ninst[e] += 1
            if o.is_dma:
                sem = self.dma_sems[e][slot]
                self.dma_cnt[e][slot] += 16
                o.sem = sem
                o.val = self.dma_cnt[e][slot]
                ins.then_inc(sem, 16)
                self.dma_last[e][slot] = o
                c = dict(clk)
                c[id(sem)] = (sem, o.val)
                o.clock = c
            elif o.needs_inc:
                if self.cur_cnt[e] >= SEM_LIMIT:
                    self.nsw[e] += 1
                    self.cur_sem[e] = nc.alloc_semaphore("s_%s_%d" % (e, self.nsw[e]))
                    self.cur_cnt[e] = 0
                self.cur_cnt[e] += 1
                o.sem = self.cur_sem[e]
                o.val = self.cur_cnt[e]
                ins.then_inc(o.sem, 1)
                c = dict(clk)
                c[id(o.sem)] = (o.sem, o.val)
                o.clock = c
            o.fn = None
            o.deps = None
        self.ops = []

    def mm(self, out, lhsT, rhs, start=True, stop=True):
        ob, oa = _ba(out)
        lb, la = _ba(lhsT)
        rb, ra = _ba(rhs)
        return self.op("pe", "matmul", dict(out=oa, lhsT=la, rhs=ra, start=start, stop=stop),
                       reads=[lb, rb], writes=[ob], disjoint=True if not start else False)

    def tr(self, out, in_, ident):
        ob, oa = _ba(out)
        ib, ia = _ba(in_)
        db, da = _ba(ident)
        return self.op("pe", "transpose", dict(out=oa, in_=ia, identity=da), reads=[ib, db], writes=[ob], disjoint=True)

    def actv(self, out, in_, func, scale=1.0, bias=None, disjoint=False, eng="act"):
        ob, oa = _ba(out)
        ib, ia = _ba(in_)
        kw = dict(out=oa, in_=ia, func=func)
        reads = [ib]
        if isinstance(scale, tuple) or isinstance(scale, Buf):
            sb_, sa = _ba(scale)
            kw["scale"] = sa
            reads.append(sb_)
        elif scale != 1.0:
            kw["scale"] = float(scale)
        if bias is not None:
            if isinstance(bias, (tuple, Buf)):
                bb, ba = _ba(bias)
                kw["bias"] = ba
                reads.append(bb)
            else:
                kw["bias"] = float(bias)
        return self.op("act", "activation", kw, reads=reads, writes=[ob], disjoint=disjoint)

    def tt(self, eng, out, in0, in1, op, disjoint=False):
        ob, oa = _ba(out)
        ab, aa = _ba(in0)
        bb, ba = _ba(in1)
        return self.op(eng, "tensor_tensor", dict(out=oa, in0=aa, in1=ba, op=op), reads=[ab, bb], writes=[ob], disjoint=disjoint)

    def ts(self, eng, out, in0, s1, s2=None, op0=ALU.mult, op1=None, disjoint=False):
        ob, oa = _ba(out)
        ab, aa = _ba(in0)
        reads = [ab]
        kw = dict(out=oa, in0=aa, op0=op0)
        if isinstance(s1, (tuple, Buf)):
            b_, a_ = _ba(s1)
            reads.append(b_)
            kw["scalar1"] = a_
        else:
            kw["scalar1"] = float(s1)
        if s2 is None:
            kw["scalar2"] = None
        elif isinstance(s2, (tuple, Buf)):
            b_, a_ = _ba(s2)
            reads.append(b_)
            kw["scalar2"] = a_
        else:
            kw["scalar2"] = float(s2)
        if op1 is not None:
            kw["op1"] = op1
        return self.op(eng, "tensor_scalar", kw, reads=reads, writes=[ob], disjoint=disjoint)

    def stt(self, out, in0, scalar, in1, op0, op1, disjoint=False):
        ob, oa = _ba(out)
        ab, aa = _ba(in0)
        bb, ba = _ba(in1)
        reads = [ab, bb]
        if isinstance(scalar, (tuple, Buf)):
            b_, a_ = _ba(scalar)
            reads.append(b_)
            sc = a_
        else:
            sc = float(scalar)
        return self.op("dve", "scalar_tensor_tensor", dict(out=oa, in0=aa, scalar=sc, in1=ba, op0=op0, op1=op1),
                       reads=reads, writes=[ob], disjoint=disjoint)

    def copy(self, eng, out, in_, disjoint=False):
        ob, oa = _ba(out)
        ib, ia = _ba(in_)
        if eng == "act":
            return self.op("act", "activation", dict(out=oa, in_=ia, func=AF.Copy), reads=[ib], writes=[ob], disjoint=disjoint)
        return self.op(eng, "tensor_copy", dict(out=oa, in_=ia), reads=[ib], writes=[ob], disjoint=disjoint)

    def recip(self, out, in_, disjoint=False):
        ob, oa = _ba(out)
        ib, ia = _ba(in_)
        return self.op("dve", "reciprocal", dict(out=oa, in_=ia), reads=[ib], writes=[ob], disjoint=disjoint)

    def memset(self, eng, out, val, disjoint=False):
        ob, oa = _ba(out)
        return self.op(eng, "memset", dict(ap=oa, constant=float(val)), writes=[ob], disjoint=disjoint)

    def dma(self, out, in_, eng="sp", disjoint=True):
        reads, writes = [], []
        if isinstance(out, (tuple, Buf)):
            ob, oa = _ba(out)
            writes.append(ob)
        else:
            oa = out
        if isinstance(in_, (tuple, Buf)):
            ib, ia = _ba(in_)
            reads.append(ib)
        else:
            ia = in_
        return self.op(eng, "dma_start", dict(out=oa, in_=ia), reads=reads, writes=writes, dma=True, disjoint=disjoint)


D = 2048
KC = 16
FF = 5632
FC = 44
TT = 512
EPS = 1e-6
MIXC = 52
NVEC = 104

R_AQ, R_AF, R_AI, R_AG = 0, 512, 1024, 1536
R_BQ, R_BK, R_BV = 2048, 2560, 3072
R_CQ, R_CK, R_CV = 3584, 4352, 5120
R_DQ, R_DK, R_DV = 5888, 6400, 6528
Y_A, Y_B, Y_C, Y_D = 0, 512, 1024, 1280


class G:
    pass


def load_vec(S, g, l):
    vec = S.sb("vec", [128, NVEC + 48], F32)
    S.dma((vec, vec[:, 0:NVEC]), g.vecs[l])
    S.ts("dve", (vec, vec[:, NVEC:NVEC + 16]), (vec, vec[:, 16:32]), 0.5)
    S.ts("dve", (vec, vec[:, NVEC + 16:NVEC + 32]), (vec, vec[:, 80:96]), 0.5)
    return vec


def phase_consts(S, g):
    ones = S.sb("ones", [128, 128], BF16)
    S.memset("dve", ones, 1.0)
    epsb = S.sb("epsb", [128, 1], F32)
    S.memset("dve", epsb, EPS)
    return ones, epsb


def rms_rstd(S, ss_ps, rstd, tmp, epsb, n):
    S.actv(tmp, ss_ps, AF.Sqrt, scale=1.0 / n, bias=(epsb, epsb[:, 0:1]))
    S.recip(rstd, tmp)


def prenorm_tile(S, g, hsrc, tok, bufX, xn, sq, tmp, rstd, ones, epsb, ssb, vec, gcol):
    srcv = hsrc.rearrange("(kc p) t -> p kc t", p=128)
    S.dma(bufX, srcv[:, :, tok], disjoint=False)
    for kc in range(KC):
        q = sq[kc % 2]
        S.actv(q, (bufX, bufX[:, kc, :]), AF.Square)
        S.mm(ssb, ones, q, start=(kc == 0), stop=(kc == KC - 1))
    rms_rstd(S, ssb, rstd, tmp, epsb, D)
    for kc in range(KC):
        S.stt((xn, xn[:, kc, :]), (bufX, bufX[:, kc, :]), (vec, vec[:, gcol + kc:gcol + kc + 1]), rstd,
              ALU.mult, ALU.mult, disjoint=True)


def postnorm_residual(S, g, hsrc, hdst, tok, bufX, rstd, tmp, epsb, ssb, vec, gcol, hre, hout):
    rms_rstd(S, ssb, rstd, tmp, epsb, D)
    for dc in range(KC):
        hr = hre[dc % 2]
        ho = hout[dc % 2]
        S.dma(hr, hsrc[dc * 128:(dc + 1) * 128, tok], disjoint=False)
        S.stt(ho, (bufX, bufX[:, dc, :]), (vec, vec[:, gcol + dc:gcol + dc + 1]), rstd, ALU.mult, ALU.mult)
        S.tt("dve", ho, ho, hr, ALU.add)
        S.dma(hdst[dc * 128:(dc + 1) * 128, tok], ho)


def prenorm_compute(S, bufX, xn, sq, tmp, rstd, ones, epsb, ssb, vec, gcol):
    for kc in range(KC):
        q = sq[kc % 2]
        S.actv(q, (bufX, bufX[:, kc, :]), AF.Square)
        S.mm(ssb, ones, q, start=(kc == 0), stop=(kc == KC - 1))
    rms_rstd(S, ssb, rstd, tmp, epsb, D)
    for kc in range(KC):
        S.stt((xn, xn[:, kc, :]), (bufX, bufX[:, kc, :]), (vec, vec[:, gcol + kc:gcol + kc + 1]), rstd,
              ALU.mult, ALU.mult, disjoint=True)


class ResidualPipe:
    def __init__(self, S, hsrc, hdst, vec, gcol, hre, hout):
        self.S, self.hsrc, self.hdst, self.vec, self.gcol, self.hre, self.hout = S, hsrc, hdst, vec, gcol, hre, hout
        self.pending = None

    def start(self, tok, bufX, rstd):
        assert self.pending is None
        self.pending = [tok, bufX, rstd, 0, 0]

    def step(self):
        if self.pending is None:
            return False
        S = self.S
        tok, bufX, rstd, nl, ncp = self.pending
        while nl < KC and nl < ncp + 2:
            hr = self.hre[nl % 2]
            S.dma(hr, self.hsrc[nl * 128:(nl + 1) * 128, tok], disjoint=False)
            nl += 1
        if nl > ncp:
            dc = ncp
            hr = self.hre[dc % 2]
            ho = self.hout[dc % 2]
            S.stt(ho, (bufX, bufX[:, dc, :]), (self.vec, self.vec[:, self.gcol + dc:self.gcol + dc + 1]), rstd,
                  ALU.mult, ALU.mult)
            S.tt("dve", ho, ho, hr, ALU.add)
            S.dma(self.hdst[dc * 128:(dc + 1) * 128, tok], ho)
            ncp += 1
        self.pending[3], self.pending[4] = nl, ncp
        if ncp >= KC:
            self.pending = None
        return True

    def flush(self):
        while self.step():
            pass


def ffn_phase(S, g, l, which, hsrc, hdst):
    NT = g.T // TT
    w13t = g.w13t[which][l]
    w2t = g.w2t[which][l]
    S.begin_phase()
    ones, epsb = phase_consts(S, g)
    vec = load_vec(S, g, l)
    gpre = 0 if which == 0 else 64
    gpost = NVEC if which == 0 else NVEC + 16
    bufX = [S.sb("bufX%d" % i, [128, KC, TT], F32) for i in range(2)]
    xn = [S.sb("xn%d" % i, [128, KC, TT], BF16) for i in range(2)]
    act = S.sb("actT", [128, FC, TT], BF16)
    w13b = [S.sb("w13b%d" % i, [128, KC, 128], BF16) for i in range(4)]
    w2b = [S.sb("w2b%d" % i, [128, FC, 128], BF16) for i in range(2)]
    sq = [S.sb("sq%d" % i, [128, TT], BF16) for i in range(2)]
    sq2 = [S.sb("sqb%d" % i, [128, TT], BF16) for i in range(2)]
    tmp = [S.sb("tmp%d" % i, [128, TT], F32) for i in range(2)]
    tpre = S.sb("tpre", [128, TT], F32)
    tpost = S.sb("tpost", [128, TT], F32)
    rpre = S.sb("rpre", [128, TT], F32)
    rpost = S.sb("rpost", [128, TT], F32)
    hre = [S.sb("hre%d" % i, [128, TT], F32) for i in range(2)]
    hout = [S.sb("hout%d" % i, [128, TT], F32) for i in range(2)]
    psb = [S.ps("psb%d" % i, [128, 512], F32) for i in range(8)]
    ss_pre = psb[6]
    ss_post = psb[7]
    srcv = hsrc.rearrange("(kc p) t -> p kc t", p=128)
    res = ResidualPipe(S, hsrc, hdst, vec, gpost, hre, hout)

    def load_prenorm(tt):
        tok = slice(tt * TT, (tt + 1) * TT)
        X = bufX[tt % 2]
        S.dma(X, srcv[:, :, tok], disjoint=False)
        prenorm_compute(S, X, xn[tt % 2], sq, tpre, rpre, ones, epsb, ss_pre, vec, gpre)

    n13 = 0
    n2 = 0
    load_prenorm(0)
    for tt in range(NT):
        tok = slice(tt * TT, (tt + 1) * TT)
        X = bufX[tt % 2]
        XN = xn[tt % 2]
        for j in range(FC):
            wb = []
            for half in range(2):
                wbf = w13b[n13 % 4]
                n13 += 1
                S.dma((wbf, wbf[:].rearrange("p a b -> p (a b)")), w13t[half * FC + j], eng="pool", disjoint=False)
                wb.append(wbf)
            pg = psb[(j % 2) * 2]
            pu = psb[(j % 2) * 2 + 1]
            for half, pp in ((0, pg), (1, pu)):
                for kc in range(KC):
                    S.mm(pp, (wb[half], wb[half][:, kc, :]), (XN, XN[:, kc, :]), start=(kc == 0), stop=(kc == KC - 1))
            tm = tmp[j % 2]
            S.actv(tm, pg, AF.Silu)
            S.tt("dve", (act, act[:, j, :]), tm, pu, ALU.mult, disjoint=True)
            if j >= 2:
                res.step()
        res.flush()
        if tt + 1 < NT:
            load_prenorm(tt + 1)
        prev_sq = None
        for dc in range(KC):
            wbf = w2b[n2 % 2]
            n2 += 1
            for q in range(4):
                S.dma((wbf, wbf[:, q * 11:(q + 1) * 11, :].rearrange("p a b -> p (a b)")),
                      w2t[dc][:, q * 1408:(q + 1) * 1408], eng="pool")
            po = psb[4 + (dc % 2)]
            for fc in range(FC):
                S.mm(po, (wbf, wbf[:, fc, :]), (act, act[:, fc, :]), start=(fc == 0), stop=(fc == FC - 1))
            if prev_sq is not None:
                S.mm(ss_post, ones, prev_sq, start=(dc == 1), stop=False)
            S.actv((X, X[:, dc, :]), po, AF.Copy, disjoint=True)
            q_ = sq2[dc % 2]
            S.actv(q_, po, AF.Square)
            prev_sq = q_
        S.mm(ss_post, ones, prev_sq, start=False, stop=True)
        rms_rstd(S, ss_post, rpost, tpost, epsb, D)
        res.start(tok, X, rpost)
    res.flush()
    S.end_phase()


def inproj_phase(S, g, l, h):
    NT = g.T // TT
    S.begin_phase()
    ones, epsb = phase_consts(S, g)
    vec = load_vec(S, g, l)
    bufX = [S.sb("bufX%d" % i, [128, KC, TT], F32) for i in range(2)]
    xn = [S.sb("xn%d" % i, [128, KC, TT], BF16) for i in range(2)]
    wb = [S.sb("wb%d" % i, [128, KC, 128], BF16) for i in range(6)]
    sq = [S.sb("sq%d" % i, [128, TT], BF16) for i in range(2)]
    tmp = S.sb("tmp", [128, TT], F32)
    rstd = S.sb("rstd", [128, TT], F32)
    ost = [S.sb("ost%d" % i, [128, TT], BF16) for i in range(4)]
    psb = [S.ps("psb%d" % i, [128, 512], F32) for i in range(5)]
    ssb = psb[4]
    srcv = h.rearrange("(kc p) t -> p kc t", p=128)
    uv = g.uT.rearrange("(kc p) t -> p kc t", p=128)

    def load_prenorm(tt):
        tok = slice(tt * TT, (tt + 1) * TT)
        X = bufX[tt % 2]
        S.dma(X, srcv[:, :, tok], disjoint=False)
        prenorm_compute(S, X, xn[tt % 2], sq, tmp, rstd, ones, epsb, ssb, vec, 32)
        S.dma(uv[:, :, tok], xn[tt % 2])

    nw = 0
    load_prenorm(0)
    for tt in range(NT):
        tok = slice(tt * TT, (tt + 1) * TT)
        XN = xn[tt % 2]
        for c in range(MIXC):
            if c == MIXC - 8 and tt + 1 < NT:
                load_prenorm(tt + 1)
            w = wb[nw % 6]
            S.dma((w, w[:].rearrange("p a b -> p (a b)")), g.wint[l][c], eng="pool", disjoint=False)
            pp = psb[nw % 4]
            o = ost[nw % 4]
            for kc in range(KC):
                S.mm(pp, (w, w[:, kc, :]), (XN, XN[:, kc, :]), start=(kc == 0), stop=(kc == KC - 1))
            S.copy("act" if nw % 2 == 0 else "dve", o, pp)
            S.dma(g.projT[c * 128:(c + 1) * 128, tok], o)
            nw += 1
    S.end_phase()


BR_K = (4, 4, 2, 4)


def merge_phase(S, g, l, h):
    NT = g.T // TT
    S.begin_phase()
    ones, epsb = phase_consts(S, g)
    vec = load_vec(S, g, l)
    bufX = [S.sb("bufX%d" % i, [128, KC, TT], F32) for i in range(2)]
    uT = [S.sb("uT%d" % i, [128, KC, TT], BF16) for i in range(2)]
    yT = [S.sb("yT%d" % i, [128, 14, TT], BF16) for i in range(2)]
    mg = S.sb("mg", [128, KC, TT], BF16)
    wg = [S.sb("wg%d" % i, [128, KC, 128], BF16) for i in range(6)]
    wbr = [S.sb("wbr%d" % i, [128, 14, 128], BF16) for i in range(2)]
    wo = [S.sb("wo%d" % i, [128, KC, 128], BF16) for i in range(2)]
    sg = [S.sb("sg%d" % i, [128, TT], F32) for i in range(2)]
    acc = S.sb("acc", [128, TT], F32)
    t2 = S.sb("t2", [128, TT], F32)
    sq = [S.sb("sq%d" % i, [128, TT], BF16) for i in range(2)]
    tmp = S.sb("tmp", [128, TT], F32)
    rstd = S.sb("rstd", [128, TT], F32)
    hre = [S.sb("hre%d" % i, [128, TT], F32) for i in range(2)]
    hout = [S.sb("hout%d" % i, [128, TT], F32) for i in range(2)]
    psb = [S.ps("psb%d" % i, [128, 512], F32) for i in range(7)]
    ssb = psb[6]
    res = ResidualPipe(S, h, h, vec, 48, hre, hout)
    uv = g.uT.rearrange("(kc p) t -> p kc t", p=128)
    yv = g.yT.rearrange("(rc p) t -> p rc t", p=128)

    def load_in(tt):
        tok = slice(tt * TT, (tt + 1) * TT)
        S.dma(uT[tt % 2], uv[:, :, tok], disjoint=False)
        S.dma(yT[tt % 2], yv[:, :, tok], disjoint=False)

    ng = 0
    nb = 0
    no = 0
    load_in(0)
    for tt in range(NT):
        tok = slice(tt * TT, (tt + 1) * TT)
        U = uT[tt % 2]
        Y = yT[tt % 2]
        X = bufX[tt % 2]
        for dc in range(KC):
            wbt = wbr[nb % 2]
            nb += 1
            S.dma((wbt, wbt[:].rearrange("p a b -> p (a b)")), g.wbt[l][dc], eng="pool", disjoint=False)
            rc0 = 0
            for i in range(4):
                w = wg[ng % 6]
                S.dma((w, w[:].rearrange("p a b -> p (a b)")), g.wint[l][MIXC + i * 16 + dc], eng="pool", disjoint=False)
                pgt = psb[(ng % 2) * 2]
                ptm = psb[(ng % 2) * 2 + 1]
                s_ = sg[ng % 2]
                ng += 1
                for kc in range(KC):
                    S.mm(pgt, (w, w[:, kc, :]), (U, U[:, kc, :]), start=(kc == 0), stop=(kc == KC - 1))
                nk = BR_K[i]
                for r in range(nk):
                    S.mm(ptm, (wbt, wbt[:, rc0 + r, :]), (Y, Y[:, rc0 + r, :]), start=(r == 0), stop=(r == nk - 1))
                rc0 += nk
                S.actv(s_, pgt, AF.Sigmoid)
                if i == 0:
                    S.tt("dve", acc, s_, ptm, ALU.mult)
                elif i < 3:
                    S.tt("dve", t2, s_, ptm, ALU.mult)
                    S.tt("dve", acc, acc, t2, ALU.add)
                else:
                    S.tt("dve", t2, s_, ptm, ALU.mult)
                    S.tt("dve", (mg, mg[:, dc, :]), acc, t2, ALU.add, disjoint=True)
            res.step()
        res.flush()
        if tt + 1 < NT:
            load_in(tt + 1)
        prev_sq = None
        for dc in range(KC):
            w = wo[no % 2]
            no += 1
            S.dma((w, w[:].rearrange("p a b -> p (a b)")), g.wot[l][dc], eng="pool", disjoint=False)
            po = psb[4 + (dc % 2)]
            for kc in range(KC):
                S.mm(po, (w, w[:, kc, :]), (mg, mg[:, kc, :]), start=(kc == 0), stop=(kc == KC - 1))
            if prev_sq is not None:
                S.mm(ssb, ones, prev_sq, start=(dc == 1), stop=False)
            S.actv((X, X[:, dc, :]), po, AF.Copy, disjoint=True)
            q_ = sq[dc % 2]
            S.actv(q_, po, AF.Square)
            prev_sq = q_
        S.mm(ssb, ones, prev_sq, start=False, stop=True)
        rms_rstd(S, ssb, rstd, tmp, epsb, D)
        res.start(tok, X, rstd)
    res.flush()
    S.end_phase()


import math, os

C_PATTERNS = ((128, 1), (512, 4), (2048, 16))
CST_BI = 0
CST_ID = 1024
CST_NTI = 1152
CST_NSL = 1280
CST_MB = 1408
CST_M2 = 3456
NCST = 3584
NEG = -30000.0
LAYERS_A = 4


def _rel_bucket(dist):
    dist = np.asarray(dist)
    d = np.maximum(dist, 1).astype(np.float32)
    large = 16 + (np.log(d / np.float32(16)) / np.float32(math.log(2048 / 16)) * np.float32(16)).astype(np.int32)
    large = np.minimum(large, 31)
    return np.where(dist < 16, dist, large)


def bi_tile(kind, pc):
    k = np.arange(128)[:, None]
    q = np.arange(128)[None, :]
    du = q - k + (128 if pc == 0 else 0)
    if kind == 'D':
        valid = (du >= 0) & (du < 128)
        r = 1
    else:
        valid = (du >= 0) & (du <= 128)
        r = C_PATTERNS[kind][1]
    b = _rel_bucket(np.maximum(du, 0) * r)
    return np.where(valid, b, -1).astype(np.float32)


BI_KINDS = [('D', 0), ('D', 1), (0, 0), (0, 1), (1, 0), (1, 1), (2, 0), (2, 1)]


def make_consts():
    c = np.zeros((128, NCST), np.float32)
    for i, (kind, pc) in enumerate(BI_KINDS):
        c[:, CST_BI + i * 128:CST_BI + (i + 1) * 128] = bi_tile(kind, pc)
    c[:, CST_ID:CST_ID + 128] = np.eye(128, dtype=np.float32)
    j = np.arange(128)[:, None]
    s = np.arange(128)[None, :]
    c[:, CST_NTI:CST_NTI + 128] = np.where(j >= s, -1.0, 0.0)
    c[:, CST_NSL:CST_NSL + 128] = np.where(j < s, -1.0, 0.0)
    col = np.arange(512)[None, :]
    for m in range(4):
        c[:, CST_MB + m * 512:CST_MB + (m + 1) * 512] = np.where(128 * m + j < col, 1.0, 0.0)
    c[:, CST_M2:CST_M2 + 128] = np.where((j // 64 == s // 64) & (j <= s), 1.0, 0.0)
    return c


def setup_phase(S, g):
    S.begin_phase()
    bi = S.sb("bi", [128, 8, 128], F32)
    S.dma(bi, g.cst[:, CST_BI:CST_BI + 1024].rearrange("p (a b) -> p a b", b=128), disjoint=False)
    relb = S.sb("relb", [128, 640], F32)
    S.dma(relb, g.relb, disjoint=False)
    ebD = S.sb("ebD", [128, 2, 8, 128], F32)
    ebC = S.sb("ebC", [128, 3, 2, 2, 2, 128], F32)
    tmps = [S.sb("tb%d" % i, [128, 128], F32) for i in range(4)]
    n = 0
    for ti, (kind, pc) in enumerate(BI_KINDS):
        tile_np = bi_tile(kind, pc)
        buckets = sorted(set(int(v) for v in np.unique(tile_np) if v >= 0))
        nh = 8 if kind == 'D' else 4
        for h in range(nh):
            if kind == 'D':
                dst = (ebD, ebD[:, pc, h, :])
                col = 12 + h
                eng = "dve"
            else:
                dst = (ebC, ebC[:, kind, h // 2, h % 2, pc, :])
                col = kind * 4 + h
                eng = "dve"
            src = (bi, bi[:, ti, :])
            S.ts(eng, dst, src, 0.0, NEG, op0=ALU.is_lt, op1=ALU.mult, disjoint=True)
            for b in buckets:
                tm = tmps[(n % 2) + (0 if eng == "dve" else 2)]
                n += 1
                S.ts(eng, tm, src, float(b), (relb, relb[:, b * 20 + col:b * 20 + col + 1]), op0=ALU.is_equal, op1=ALU.mult)
                S.tt(eng, dst, dst, tm, ALU.add, disjoint=True)
    S.dma(g.ebD, (ebD, ebD[:].rearrange("p a b c -> p (a b c)")))
    S.dma(g.ebC, (ebC, ebC[:].rearrange("p a b c d e -> p (a b c d e)")))
    S.end_phase()


def load_ident(S, g):
    idf = S.sb("idf", [128, 128], F32)
    S.dma(idf, g.cst[:, CST_ID:CST_ID + 128], disjoint=False)
    idb = S.sb("idb", [128, 128], BF16)
    S.copy("dve", idb, idf)
    return idb


def to_tokmajor(S, src, dst, NB, idb, pT, cnt, engs=("act", "dve")):
    for n in range(NB):
        p = pT[cnt[0] % len(pT)]
        S.tr(p, (src, src[:, n * 128:(n + 1) * 128]), idb)
        S.copy(engs[cnt[0] % len(engs)], (dst, dst[:, n, :]), p, disjoint=True)
        cnt[0] += 1


def mixD_phase(S, g, l):
    T = g.T
    NB = T // 128
    S.begin_phase()
    idb = load_ident(S, g)
    ones64 = S.sb("ones64", [128, 64], BF16)
    S.memset("dve", ones64, 1.0)
    qD = S.sb("qD", [64, 8, T], BF16)
    kD = S.sb("kD", [64, 2, T], BF16)
    vT = S.sb("vT", [128, T], BF16)
    Vtok = S.sb("Vtok", [128, NB, 128], BF16)
    yD = S.sb("yD", [64, 8, T], BF16)
    eb = S.sb("eb", [128, 2, 8, 128], F32)
    S.dma((eb, eb[:].rearrange("p a b c -> p (a b c)")), g.ebD, disjoint=False)
    sk = S.sb("sk", [64, 8], F32)
    S.dma(sk, g.sinks[0:64, l * 8:(l + 1) * 8], disjoint=False)
    es = S.sb("es", [64, 8], F32)
    S.actv(es, sk, AF.Exp)
    esb = S.sb("esb", [64, 8, 128], F32)
    S.copy("dve", esb, (es, es[:, :].unsqueeze(2).to_broadcast([64, 8, 128])))
    for h in range(8):
        S.dma((qD, qD[:, h, :]), g.projT[R_DQ + h * 64:R_DQ + (h + 1) * 64, :])
    for kv in range(2):
        S.dma((kD, kD[:, kv, :]), g.projT[R_DK + kv * 64:R_DK + (kv + 1) * 64, :])
    S.dma(vT, g.projT[R_DV:R_DV + 128, :], disjoint=False)
    pT = [S.ps("pT%d" % i, [128, 128], BF16) for i in range(2)]
    pS = [S.ps("pS%d" % i, [128, 2, 512], F32) for i in range(2)]
    pO = S.ps("pO", [128, 512], F32)
    pD = S.ps("pD", [128, 512], F32)
    Zs = [S.sb("Zs%d" % i, [128, 2, 512], F32) for i in range(2)]
    Pb = [S.sb("Pb%d" % i, [128, 2, 512], BF16) for i in range(2)]
    dt = S.sb("dt", [64, 512], F32)
    cnt = [0]
    to_tokmajor(S, vT, Vtok, NB, idb, pT, cnt)
    it = 0
    for n in range(NB):
        for gk in range(2):
            Sp = pS[it % 2]
            Z = Zs[it % 2]
            P = Pb[it % 2]
            it += 1
            rq = (qD, qD[:, 4 * gk:4 * gk + 4, n * 128:(n + 1) * 128])
            lo = 0 if n > 0 else 1
            if n > 0:
                S.mm((Sp, Sp[:, 0, :]), (kD, kD[:, gk, (n - 1) * 128:n * 128]), rq)
            S.mm((Sp, Sp[:, 1, :]), (kD, kD[:, gk, n * 128:(n + 1) * 128]), rq, start=True)
            S.stt((Z, Z[:, lo:2, :].rearrange("p a (h q) -> p a h q", h=4)),
                  (Sp, Sp[:, lo:2, :].rearrange("p a (h q) -> p a h q", h=4)), 0.125,
                  (eb, eb[:, lo:2, 4 * gk:4 * gk + 4, :]), ALU.mult, ALU.add)
            S.actv((P, P[:, lo:2, :]), (Z, Z[:, lo:2, :]), AF.Exp)
            for (pp, lhs_of) in ((pO, None), (pD, ones64)):
                if n > 0:
                    lh = (Vtok, Vtok[:, n - 1, gk * 64:(gk + 1) * 64]) if lhs_of is None else ones64
                    S.mm((pp, pp[0:64, :]), lh, (P, P[:, 0, :]), start=True, stop=False)
                lh = (Vtok, Vtok[:, n, gk * 64:(gk + 1) * 64]) if lhs_of is None else ones64
                S.mm((pp, pp[0:64, :]), lh, (P, P[:, 1, :]), start=(n == 0), stop=True)
            S.tt("dve", (dt, dt[:, :].rearrange("p (h q) -> p h q", h=4)),
                 (pD, pD[0:64, :].rearrange("p (h q) -> p h q", h=4)), (esb, esb[:, 4 * gk:4 * gk + 4, :]), ALU.add)
            S.recip(dt, dt)
            S.tt("dve", (yD, yD[:, 4 * gk:4 * gk + 4, n * 128:(n + 1) * 128]),
                 (pO, pO[0:64, :].rearrange("p (h q) -> p h q", h=4)),
                 (dt, dt[:, :].rearrange("p (h q) -> p h q", h=4)), ALU.mult, disjoint=True)
    for h in range(8):
        S.dma(g.yT[Y_D + h * 64:Y_D + (h + 1) * 64, :], (yD, yD[:, h, :]))
    S.end_phase()


def mixC_phase(S, g, l):
    T = g.T
    NB = T // 128
    S.begin_phase()
    idb = load_ident(S, g)
    ones64 = S.sb("ones64", [128, 64], BF16)
    S.memset("dve", ones64, 1.0)
    eb = S.sb("eb", [128, 3, 2, 2, 2, 128], F32)
    S.dma((eb, eb[:].rearrange("p a b c d e -> p (a b c d e)")), g.ebC, disjoint=False)
    natq = [S.sb("natq%d" % i, [64, 2, T], BF16) for i in range(2)]
    perq = [S.sb("perq%d" % i, [64, 2, T], BF16) for i in range(2)]
    natv = S.sb("natv", [128, T], BF16)
    perv = S.sb("perv", [128, T], BF16)
    Vtok = S.sb("Vtok", [128, NB, 128], BF16)
    accN = S.sb("accN", [64, 2, T], F32)
    accD = S.sb("accD", [64, 2, T], F32)
    ybf = S.sb("ybf", [64, 2, T], BF16)
    pT = [S.ps("pT%d" % i, [128, 128], BF16) for i in range(2)]
    pS = [S.ps("pS%d" % i, [128, 2, 2, 128], F32) for i in range(2)]
    pO = [S.ps("pO%d" % i, [128, 512], F32) for i in range(2)]
    pD = [S.ps("pD%d" % i, [128, 512], F32) for i in range(2)]
    Zs = [S.sb("Zs%d" % i, [128, 2, 2, 128], F32) for i in range(2)]
    Pb = [S.sb("Pb%d" % i, [128, 2, 2, 128], BF16) for i in range(2)]
    cnt = [0]
    it = 0
    rows = (R_CQ, R_CK, R_CV)
    for hp in range(2):
        for gi, (win, r) in enumerate(C_PATTERNS):
            cur = []
            for j in range(2):
                r0 = rows[j] + gi * 256 + hp * 128
                for hh in range(2):
                    S.dma((natq[j], natq[j][:, hh, :]), g.projT[r0 + hh * 64:r0 + (hh + 1) * 64, :], disjoint=(hh == 1))
                if r > 1:
                    S.copy("act" if j == 0 else "dve", (perq[j], perq[j][:, :, :].rearrange("p h (c i) -> p h c i", c=r)),
                           (natq[j], natq[j][:, :, :].rearrange("p h (i c) -> p h c i", c=r)))
                    cur.append(perq[j])
                else:
                    cur.append(natq[j])
            r0 = rows[2] + gi * 256 + hp * 128
            S.dma(natv, g.projT[r0:r0 + 128, :], disjoint=False)
            if r > 1:
                S.copy("act", (perv, perv[:, :].rearrange("p (c i) -> p c i", c=r)),
                       (natv, natv[:, :].rearrange("p (i c) -> p c i", c=r)))
                vp = perv
            else:
                vp = natv
            qp, kp = cur
            to_tokmajor(S, vp, Vtok, NB, idb, pT, cnt)
            Lb = NB // r
            for c in range(r):
                for n in range(Lb):
                    pb = c * Lb + n
                    Sp = pS[it % 2]
                    Z = Zs[it % 2]
                    P = Pb[it % 2]
                    po = pO[it % 2]
                    pd = pD[it % 2]
                    it += 1
                    for hh in range(2):
                        rq = (qp, qp[:, hh, pb * 128:(pb + 1) * 128])
                        if n > 0:
                            S.mm((Sp, Sp[:, hh, 0, :]), (kp, kp[:, hh, (pb - 1) * 128:pb * 128]), rq)
                        S.mm((Sp, Sp[:, hh, 1, :]), (kp, kp[:, hh, pb * 128:(pb + 1) * 128]), rq)
                    if n > 0:
                        S.stt(Z, Sp, 0.125, (eb, eb[:, gi, hp, :, :, :]), ALU.mult, ALU.add)
                        S.actv(P, Z, AF.Exp)
                    else:
                        S.stt((Z, Z[:, :, 1, :]), (Sp, Sp[:, :, 1, :]), 0.125, (eb, eb[:, gi, hp, :, 1, :]), ALU.mult, ALU.add)
                        S.actv((P, P[:, :, 1, :]), (Z, Z[:, :, 1, :]), AF.Exp)
                    for hh in range(2):
                        vs = slice(hh * 64, (hh + 1) * 64)
                        cs = slice(hh * 128, (hh + 1) * 128)
                        for (pp, isden) in ((po, False), (pd, True)):
                            if n > 0:
                                lh = ones64 if isden else (Vtok, Vtok[:, pb - 1, vs])
                                S.mm((pp, pp[0:64, cs]), lh, (P, P[:, hh, 0, :]), start=True, stop=False)
                            lh = ones64 if isden else (Vtok, Vtok[:, pb, vs])
                            S.mm((pp, pp[0:64, cs]), lh, (P, P[:, hh, 1, :]), start=(n == 0), stop=True)
                    t0 = c + r * 128 * n
                    sl = slice(t0, t0 + r * 127 + 1, r) if r > 1 else slice(t0, t0 + 128)
                    for (acc, pp, eng) in ((accN, po, "dve"), (accD, pd, "act")):
                        av = (acc, acc[:, :, sl])
                        pv = (pp, pp[0:64, 0:256].rearrange("p (h q) -> p h q", h=2))
                        if gi == 0:
                            S.copy(eng, av, pv, disjoint=True)
                        else:
                            S.tt("dve", av, av, pv, ALU.add, disjoint=True)
        S.recip(accD, accD)
        S.tt("dve", ybf, accN, accD, ALU.mult)
        for hh in range(2):
            r0 = Y_C + (2 * hp + hh) * 64
            S.dma(g.yT[r0:r0 + 64, :], (ybf, ybf[:, hh, :]))
    S.end_phase()


def load_cst_bf(S, g, name, c0, n):
    f = S.sb(name + "f", [128, n], F32)
    S.dma(f, g.cst[:, c0:c0 + n], disjoint=False)
    b = S.sb(name + "b", [128, n], BF16)
    S.copy("dve", b, f)
    return f, b


def mixB_phase(S, g, l):
    T = g.T
    NB = T // 128
    NQ = T // 512
    S.begin_phase()
    idb = load_ident(S, g)
    _, nti = load_cst_bf(S, g, "nti", CST_NTI, 128)
    _, nsl = load_cst_bf(S, g, "nsl", CST_NSL, 128)
    maskB = S.sb("maskB", [128, 4, 512], F32)
    S.dma((maskB, maskB[:].rearrange("p a b -> p (a b)")), g.cst[:, CST_MB:CST_MB + 2048], disjoint=False)
    oneb = S.sb("oneb", [128, 1], F32)
    S.memset("dve", oneb, 1.0)
    qh = S.sb("qh", [64, 2, T], BF16)
    kh = S.sb("kh", [64, 2, T], BF16)
    vT = S.sb("vT", [128, T], BF16)
    Vtok = S.sb("Vtok", [128, NB, 128], BF16)
    ybf = S.sb("ybf", [64, 2, T], BF16)
    NPS = int(os.environ.get("MK_BNPS", "3"))
    NST = int(os.environ.get("MK_BNST", "3" if NPS == 1 else "2"))
    pSs = [S.ps("pS%d" % i, [128, 512], F32) for i in range(NPS)]
    pT = [S.ps("pT%d" % i, [128, 128], BF16) for i in range(1)]
    sets = []
    pscnt = [0]
    for k in range(NST):
        st = G()
        st.pB = S.ps("pB%d" % k, [128, 512], F32)
        st.pO = S.ps("pO%d" % k, [128, 512], F32)
        st.e = [S.sb("e%d_%d" % (k, i), [128, 512], F32) for i in range(2)]
        st.sp = [S.sb("sp%d_%d" % (k, i), [128, 512], BF16) for i in range(2)]
        st.w = [S.sb("w%d_%d" % (k, i), [128, 512], F32) for i in range(2)]
        st.a = [S.sb("a%d_%d" % (k, i), [128, 512], BF16) for i in range(2)]
        sets.append(st)
    cnt = [0]

    def chain(st, hh, qt):
        qs = (qh, qh[:, hh, qt * 512:(qt + 1) * 512])
        kbs = list(range(4 * qt + 3, -1, -1))
        nk = len(kbs)

        def s_and_exp(i):
            kb = kbs[i]
            m = kb - 4 * qt
            pS = pSs[pscnt[0] % len(pSs)]
            pscnt[0] += 1
            e = st.e[i % 2]
            S.mm(pS, (kh, kh[:, hh, kb * 128:(kb + 1) * 128]), qs)
            S.actv(e, pS, AF.Exp, scale=0.125)
            if m >= 0:
                S.tt("dve", e, e, (maskB, maskB[:, m, :]), ALU.mult)

        s_and_exp(0)
        yield
        for i in range(nk):
            kb = kbs[i]
            e = st.e[i % 2]
            sp = st.sp[i % 2]
            w = st.w[i % 2]
            a = st.a[i % 2]
            first = (i == 0)
            last = (i == nk - 1)
            S.actv(sp, e, AF.Ln, bias=(oneb, oneb[:, 0:1]))
            yield
            if not last:
                s_and_exp(i + 1)
            yield
            S.mm(st.pB, nti, sp, start=first, stop=False)
            yield
            S.actv(w, st.pB, AF.Exp)
            yield
            S.mm(st.pB, nsl, sp, start=False, stop=last)
            S.tt("dve", a, w, e, ALU.mult)
            yield
            S.mm((st.pO, st.pO[0:64, :]), (Vtok, Vtok[:, kb, hh * 64:(hh + 1) * 64]), a, start=first, stop=last)
            yield
        S.copy("act", (ybf, ybf[:, hh, qt * 512:(qt + 1) * 512]), (st.pO, st.pO[0:64, :]), disjoint=True)
        yield

    for hp in range(4):
        for hh in range(2):
            S.dma((qh, qh[:, hh, :]), g.projT[R_BQ + hp * 128 + hh * 64:R_BQ + hp * 128 + (hh + 1) * 64, :], disjoint=(hh == 1))
            S.dma((kh, kh[:, hh, :]), g.projT[R_BK + hp * 128 + hh * 64:R_BK + hp * 128 + (hh + 1) * 64, :], disjoint=(hh == 1))
        S.dma(vT, g.projT[R_BV + hp * 128:R_BV + (hp + 1) * 128, :], disjoint=False)
        to_tokmajor(S, vT, Vtok, NB, idb, pT, cnt)
        work = [(hh, qt) for qt in range(NQ - 1, -1, -1) for hh in range(2)]
        active = []
        free_sets = list(sets)
        while work or active:
            while work and free_sets:
                hh, qt = work.pop(0)
                st = free_sets.pop(0)
                active.append((chain(st, hh, qt), st))
            nxt = []
            for gen, st in active:
                try:
                    next(gen)
                    nxt.append((gen, st))
                except StopIteration:
                    free_sets.append(st)
            active = nxt
        for hh in range(2):
            r0 = Y_B + hp * 128 + hh * 64
            S.dma(g.yT[r0:r0 + 64, :], (ybf, ybf[:, hh, :]))
    S.end_phase()


def mixA_phase(S, g, l):
    T = g.T
    SEG = min(1024, T)
    NSEG = T // SEG
    NBS = SEG // 128
    NCH = SEG // 64
    S.begin_phase()
    idb = load_ident(S, g)
    ones, epsb = phase_consts(S, g)
    vec = load_vec(S, g, l)
    m2 = S.sb("m2", [128, 128], F32)
    S.dma(m2, g.cst[:, CST_M2:CST_M2 + 128], disjoint=False)
    cmask = S.sb("cmask", [128, SEG], F32)
    S.memset("dve", cmask, 1.0)
    S.memset("dve", (cmask, cmask[:, 0:SEG:64]), 0.0)
    lbl = S.sb("lbl", [128, 4, LAYERS_A], F32)
    S.dma((lbl, lbl[:].rearrange("p a b -> p (a b)")), g.lbl, disjoint=False)
    le = S.sb("le", [128, 4, LAYERS_A], F32)
    S.actv(le, lbl, AF.Exp)
    lsum = S.sb("lsum", [128, 4], F32)
    S.tt("dve", lsum, (le, le[:, :, 0]), (le, le[:, :, 1]), ALU.add)
    for i in range(2, LAYERS_A):
        S.tt("dve", lsum, lsum, (le, le[:, :, i]), ALU.add)
    S.recip(lsum, lsum)
    lb = S.sb("lb", [128, 4], F32)
    oml = S.sb("oml", [128, 4], F32)
    S.memset("dve", lb, 0.0)
    for i in range(1, l + 1):
        S.tt("dve", lb, lb, (le, le[:, :, i]), ALU.add)
    S.tt("dve", lb, lb, lsum, ALU.mult)
    S.ts("dve", oml, lb, -1.0, 1.0, op0=ALU.mult, op1=ALU.add)
    qn = S.sb("qn", [128, SEG], BF16)
    fn = S.sb("fn", [128, SEG], BF16)
    vn = S.sb("vn", [128, SEG], BF16)
    khat = S.sb("khat", [128, SEG], BF16)
    t1 = S.sb("t1", [128, SEG], F32)
    t2 = S.sb("t2", [128, SEG], F32)
    t3 = S.sb("t3", [128, SEG], F32)
    t4 = S.sb("t4", [128, SEG], F32)
    bb = S.sb("bb", [128, SEG], F32)
    sqb = S.sb("sqb", [128, SEG], BF16)
    H = []
    for h in range(4):
        hs = G()
        hs.gn = S.sb("gn%d" % h, [128, SEG], BF16)
        hs.ebt = S.sb("ebt%d" % h, [128, SEG], F32)
        hs.qt = S.sb("qt%d" % h, [128, SEG], BF16)
        hs.kt = S.sb("kt%d" % h, [128, SEG], BF16)
        hs.qb = S.sb("qb%d" % h, [128, SEG], BF16)
        hs.Vt = S.sb("Vt%d" % h, [128, NBS, 128], BF16)
        hs.Kt = S.sb("Kt%d" % h, [128, NBS, 128], BF16)
        hs.oT = S.sb("oT%d" % h, [128, SEG], F32)
        hs.Sst = S.sb("Sst%d" % h, [128, 128], F32)
        hs.Sb = [S.sb("Sb%d_%d" % (h, i), [128, 128], BF16) for i in range(2)]
        hs.Pm = S.sb("Pm%d" % h, [128, 128], BF16)
        S.memset("dve", hs.Sst, 0.0)
        S.memset("dve", hs.Sb[0], 0.0)
        H.append(hs)
    pT = [S.ps("pT%d" % i, [128, 128], BF16) for i in range(2)]
    bsc = S.ps("bsc", [128, 4, 128], F32)
    bua = S.ps("bua", [128, 4, 128], F32)
    bub = S.ps("bub", [128, 4, 128], F32)
    bo = S.ps("bo", [128, 4, 128], F32)
    bss = S.ps("bss", [128, 512], F32)
    for h in range(4):
        H[h].psc = Buf("psc%d" % h, bsc.t, bsc.lock)
        H[h].pua = Buf("pua%d" % h, bua.t, bua.lock)
        H[h].pub = Buf("pub%d" % h, bub.t, bub.lock)
        H[h].po = Buf("po%d" % h, bo.t, bo.lock)
    cnt = [0]
    ysb = [S.sb("ysb%d" % i, [128, SEG], BF16) for i in range(2)]
    for seg in range(NSEG):
        cs = slice(seg * SEG, (seg + 1) * SEG)
        for h in range(4):
            hs = H[h]
            S.dma(qn, g.projT[R_AQ + h * 128:R_AQ + (h + 1) * 128, cs], disjoint=False)
            S.dma(fn, g.projT[R_AF + h * 128:R_AF + (h + 1) * 128, cs], disjoint=False)
            S.dma(vn, g.projT[R_AI + h * 128:R_AI + (h + 1) * 128, cs], disjoint=False)
            S.dma(hs.gn, g.projT[R_AG + h * 128:R_AG + (h + 1) * 128, cs], disjoint=False)
            S.actv(t1, fn, AF.Exp, scale=-1.0)
            S.ts("dve", t1, t1, 1.0, None, op0=ALU.add)
            S.recip(t1, t1)
            S.ts("dve", t1, t1, (oml, oml[:, h:h + 1]), (lb, lb[:, h:h + 1]), op0=ALU.mult, op1=ALU.add)
            S.actv(t2, t1, AF.Ln)
            S.ts("dve", t3, t1, -1.0, 1.0, op0=ALU.mult, op1=ALU.add)
            S.op("dve", "tensor_tensor_scan", dict(out=bb[:], data0=cmask[:], data1=t2[:], initial=0.0,
                                                   op0=ALU.mult, op1=ALU.add), reads=[cmask, t2], writes=[bb])
            bv = bb[:, :].rearrange("p (c i) -> p c i", i=64)
            bmid = bv[:, :, 31:32].to_broadcast([128, NCH, 64])
            blast = bv[:, :, 63:64].to_broadcast([128, NCH, 64])
            v3 = lambda b_: (b_, b_[:, :].rearrange("p (c i) -> p c i", i=64))
            S.tt("dve", v3(t2), (bb, bv), (bb, bmid), ALU.subtract)
            S.actv(t4, t2, AF.Exp)
            S.tt("dve", hs.qt, qn, t4, ALU.mult)
            S.actv(t4, t2, AF.Exp, scale=-1.0)
            S.tt("dve", hs.kt, t3, t4, ALU.mult)
            S.tt("dve", v3(t2), (bb, bv), (bb, blast), ALU.subtract)
            S.actv(t4, t2, AF.Exp, scale=-1.0)
            S.tt("dve", khat, t3, t4, ALU.mult)
            S.actv(hs.ebt, bb, AF.Exp)
            S.tt("dve", hs.qb, qn, hs.ebt, ALU.mult)
            if g.dbg is not None and h == 0 and seg == 0:
                S.dma(g.dbg[0], t1); S.dma(g.dbg[1], bb); S.dma(g.dbg[2], hs.ebt); S.dma(g.dbg[3], t3)
                S.copy("dve", t4, hs.qt); S.dma(g.dbg[4], t4)
            to_tokmajor(S, vn, hs.Vt, NBS, idb, pT, cnt)
            to_tokmajor(S, khat, hs.Kt, NBS, idb, pT, cnt)
        for n in range(NBS):
            bs = slice(n * 128, (n + 1) * 128)
            for h in range(4):
                hs = H[h]
                S.mm((hs.psc, bsc[:, h, :]), (hs.kt, hs.kt[:, bs]), (hs.qt, hs.qt[:, bs]))
                S.mm((hs.pua, bua[0:128, h, :]), (hs.Kt, hs.Kt[0:64, n, :]), (hs.Vt, hs.Vt[0:64, n, :]))
                S.mm((hs.pub, bub[0:128, h, :]), (hs.Kt, hs.Kt[64:128, n, :]), (hs.Vt, hs.Vt[64:128, n, :]))
            for h in range(4):
                hs = H[h]
                S.tt("dve", hs.Pm, (hs.psc, bsc[:, h, :]), m2, ALU.mult)
                cA = (2 * n) * 64 + 63
                S.stt(hs.Sst, hs.Sst, (hs.ebt, hs.ebt[:, cA:cA + 1]), (hs.pua, bua[:, h, :]), ALU.mult, ALU.add)
                S.copy("act", hs.Sb[1], hs.Sst)
            for h in range(4):
                hs = H[h]
                S.mm((hs.po, bo[:, h, :]), (hs.Vt, hs.Vt[:, n, :]), hs.Pm, start=True, stop=False)
                S.mm((hs.po, bo[:, h, 0:64]), hs.Sb[0], (hs.qb, hs.qb[:, n * 128:n * 128 + 64]), start=False, stop=False)
                S.mm((hs.po, bo[:, h, 64:128]), hs.Sb[1], (hs.qb, hs.qb[:, n * 128 + 64:(n + 1) * 128]), start=False, stop=True)
            for h in range(4):
                hs = H[h]
                cB = (2 * n + 1) * 64 + 63
                S.stt(hs.Sst, hs.Sst, (hs.ebt, hs.ebt[:, cB:cB + 1]), (hs.pub, bub[:, h, :]), ALU.mult, ALU.add)
                S.copy("act", hs.Sb[0], hs.Sst)
                S.copy("act", (hs.oT, hs.oT[:, bs]), (hs.po, bo[:, h, :]), disjoint=True)
        for h in range(4):
            hs = H[h]
            yb = ysb[h % 2]
            for c in range(SEG // 512):
                c5 = slice(c * 512, (c + 1) * 512)
                S.actv((sqb, sqb[:, c5]), (hs.oT, hs.oT[:, c5]), AF.Square)
                S.mm(bss, ones, (sqb, sqb[:, c5]))
                S.actv((t2, t2[:, c5]), bss, AF.Sqrt, scale=1.0 / 128, bias=(epsb, epsb[:, 0:1]))
            S.recip(t2, t2)
            if g.dbg is not None and h == 0 and seg == 0:
                S.dma(g.dbg[5], hs.oT); S.dma(g.dbg[6], t2)
            S.stt(t3, hs.oT, (vec, vec[:, 96:97]), t2, ALU.mult, ALU.mult)
            S.actv(t4, hs.gn, AF.Silu)
            S.tt("dve", yb, t3, t4, ALU.mult)
            S.dma(g.yT[Y_A + h * 128:Y_A + (h + 1) * 128, cs], yb)
    S.end_phase()


LAYERS = 4


def build(T, L, phases=None, debug=False, ext_in=()):
    nc = bass.Bass("TRN2", target_bir_lowering=False)
    g = G()
    g.nc = nc
    g.T = T
    g.L = L

    def din(name, shape, dt=F32):
        return nc.dram_tensor(name, list(shape), dt, kind="ExternalInput").ap()

    g.xT = din("xT", [D, T])
    g.w13t = [din("w13t_a", [L, 88, 128, 2048]), din("w13t_b", [L, 88, 128, 2048])]
    g.w2t = [din("w2t_a", [L, 16, 128, FF]), din("w2t_b", [L, 16, 128, FF])]
    g.wint = din("wint", [L, 116, 128, 2048])
    g.wbt = din("wbt", [L, 16, 128, 1792])
    g.wot = din("wot", [L, 16, 128, 2048])
    g.vecs = din("vecs", [L, 128, NVEC])
    g.lbl = din("lbl", [128, 4 * LAYERS])
    g.sinks = din("sinks", [128, LAYERS * 8])
    g.relb = din("relb", [128, 640])
    g.cst = din("cst", [128, NCST])
    g.outT = nc.dram_tensor("outT", [D, T], F32, kind="ExternalOutput").ap()
    sk = "ExternalOutput" if debug else "Internal"
    def scr(name, shape, dt):
        return nc.dram_tensor(name, shape, dt, kind=("ExternalInput" if name in ext_in else sk)).ap()
    g.projT = scr("projT", [6656, T], BF16)
    g.uT = scr("uT_s", [D, T], BF16)
    g.yT = scr("yT_s", [1792, T], BF16)
    g.ebD = nc.dram_tensor("ebD", [128, 2 * 8 * 128], F32, kind=sk).ap()
    g.ebC = nc.dram_tensor("ebC", [128, 3 * 2 * 2 * 2 * 128], F32, kind=sk).ap()
    g.lbs = nc.dram_tensor("lbs", [128, 4 * LAYERS], F32, kind=sk).ap()
    g.dbg = nc.dram_tensor('dbg', [16, 128, 1024], F32, kind='ExternalOutput').ap() if debug else None
    S = Sched(nc)
    g.S = S
    allp = ["ffn1", "inproj", "A", "B", "C", "D", "merge", "ffn2"]
    if phases is None:
        phases = allp
    if any(p in phases for p in ("A", "C", "D")):
        setup_phase(S, g)
    for l in range(L):
        first = (l == 0)
        for p in phases:
            if p == "ffn1":
                ffn_phase(S, g, l, 0, g.xT if first else g.outT, g.outT)
            elif p == "inproj":
                inproj_phase(S, g, l, g.outT)
            elif p == "A":
                mixA_phase(S, g, l)
            elif p == "B":
                mixB_phase(S, g, l)
            elif p == "C":
                mixC_phase(S, g, l)
            elif p == "D":
                mixD_phase(S, g, l)
            elif p == "merge":
                merge_phase(S, g, l, g.outT)
            elif p == "ffn2":
                ffn_phase(S, g, l, 1, g.outT, g.outT)
    return nc, g


def tile_w(w, kc, nc_):
    return np.ascontiguousarray(w.reshape(kc, 128, nc_, 128).transpose(2, 1, 0, 3).reshape(nc_, 128, kc * 128))


def prep_weights(inp, L):
    out = {}
    out["w13t_a"] = np.stack([tile_w(inp["ffn1_w13"][l], 16, 88) for l in range(L)])
    out["w13t_b"] = np.stack([tile_w(inp["ffn2_w13"][l], 16, 88) for l in range(L)])
    out["w2t_a"] = np.stack([tile_w(inp["ffn1_w2"][l], 44, 16) for l in range(L)])
    out["w2t_b"] = np.stack([tile_w(inp["ffn2_w2"][l], 44, 16) for l in range(L)])
    out["wint"] = np.stack([tile_w(inp["w_in"][l], 16, 116) for l in range(L)])
    out["wbt"] = np.stack([tile_w(inp["w_branch"][l], 14, 16) for l in range(L)])
    out["wot"] = np.stack([tile_w(inp["w_out"][l], 16, 16) for l in range(L)])
    vecs = np.zeros((L, 128, NVEC), np.float32)
    for l in range(L):
        for i, nm in enumerate(("ffn1_norm", "mix_norm", "ffn2_norm")):
            for j in range(2):
                vecs[l, :, (2 * i + j) * 16:(2 * i + j + 1) * 16] = inp[nm][l, j].reshape(16, 128).T
        vecs[l, :, 96] = inp["hgrn_out_norm"][l]
    out["vecs"] = vecs
    lb = np.asarray(inp["hgrn_lb_logits"])
    out["lbl"] = np.ascontiguousarray(lb.reshape(LAYERS, 4, 128).transpose(2, 1, 0).reshape(128, 4 * LAYERS))
    out["sinks"] = np.ascontiguousarray(np.broadcast_to(np.asarray(inp["attn_sinks"]).reshape(1, -1), (128, LAYERS * 8)))
    out["relb"] = np.ascontiguousarray(np.broadcast_to(np.asarray(inp["rel_bias"]).reshape(1, 640), (128, 640)))
    out["cst"] = make_consts()
    return out


_CACHE = {}


def kernel(**inputs):
    x = np.asarray(inputs["x"])
    B, T, _ = x.shape
    L = LAYERS
    key = (T, L)
    if key not in _CACHE:
        _CACHE[key] = build(T, L, None, debug=False)
    nc, g = _CACHE[key]
    w = prep_weights({k: np.asarray(v) for k, v in inputs.items() if k != "x"}, L)
    in_maps = []
    for b in range(B):
        m = dict(w)
        m["xT"] = np.ascontiguousarray(x[b].T)
        in_maps.append(m)
    res = run_bass_kernel_spmd(nc, in_maps, core_ids=list(range(B)))
    out = np.stack([np.ascontiguousarray(res.results[b]["outT"].T) for b in range(B)], axis=0)
    return out.astype(np.float32, copy=False)
```

```python
import numpy as np
import concourse.bass as bass
import concourse.mybir as mybir
from concourse.bass_utils import run_bass_kernel_spmd
from contextlib import ExitStack

F32 = mybir.dt.float32
BF16 = mybir.dt.bfloat16
AF = mybir.ActivationFunctionType
ALU = mybir.AluOpType

import os
SERIAL_PH = [int(x) for x in os.environ.get('MK_SERIAL', '').split(',') if x]
SEM_LIMIT = 30000
NDMA_SEMS = 10


class Buf:
    __slots__ = ("name", "writers", "readers", "t", "lock")

    def __init__(self, name, t=None, lock=None):
        self.name = name
        self.writers = []
        self.readers = []
        self.t = t
        self.lock = lock

    def __getitem__(self, k):
        return self.t[k]


class Op:
    __slots__ = ("eng", "fn", "deps", "is_dma", "needs_inc", "sem", "val", "clock", "idx", "phase")


def _ba(x):
    if isinstance(x, Buf):
        return x, x.t[:]
    return x


class Sched:
    COMPUTE = ("pe", "act", "dve", "pool")
    ALL = ("pe", "act", "dve", "pool", "sp")

    def __init__(self, nc):
        self.nc = nc
        self.ops = []
        self.engs = {"pe": nc.tensor, "act": nc.scalar, "dve": nc.vector,
                     "pool": nc.gpsimd, "sp": nc.sync}
        self.phase = 0
        self.nops = 0
        self.last = {}
        self.dmas = []
        self.clocks = {e: {} for e in self.ALL}
        self.cur_sem = {}
        self.cur_cnt = {}
        self.nsw = {}
        for e in self.COMPUTE:
            self.cur_sem[e] = nc.alloc_semaphore("s_%s_0" % e)
            self.cur_cnt[e] = 0
            self.nsw[e] = 0
        self.dma_sems = {}
        self.dma_cnt = {}
        self.dma_last = {}
        self.dma_rr = {}
        self.nwaits = 0
        self.ninst = {e: 0 for e in self.ALL}
        self.es = None

    def begin_phase(self):
        self.es = ExitStack()
        self.es.__enter__()

    def sb(self, name, shape, dt):
        t = self.es.enter_context(self.nc.sbuf_tensor("%s_p%d" % (name, self.phase), list(shape), dt))
        return Buf(name, t)

    def ps(self, name, shape, dt=F32):
        t = self.es.enter_context(self.nc.psum_tensor("%s_p%d" % (name, self.phase), list(shape), dt))
        b = Buf(name, t)
        b.lock = Buf(name + "_lock")
        return b

    def end_phase(self):
        self.barrier()
        self.emit()
        self.es.__exit__(None, None, None)
        self.es = None
        self.phase += 1

    def op(self, eng, name, kw, reads=(), writes=(), dma=False, disjoint=False):
        o = Op()
        o.eng = eng
        o.fn = (name, kw)
        o.is_dma = dma
        o.needs_inc = dma
        o.sem = None
        o.val = 0
        o.clock = None
        o.idx = self.nops
        o.phase = self.phase
        self.nops += 1
        deps = {}
        locks = []
        for b in reads:
            if b.lock is not None and b.lock not in locks:
                locks.append(b.lock)
        for b in writes:
            if b.lock is not None and b.lock not in locks:
                locks.append(b.lock)
        for b in locks:
            for w in b.writers:
                deps[w.idx] = w
        for b in reads:
            for w in b.writers:
                deps[w.idx] = w
        for b in writes:
            for r in b.readers:
                deps[r.idx] = r
            if (not disjoint) or b.readers:
                for w in b.writers:
                    deps[w.idx] = w
        ph = self.phase
        SERIAL = (ph in SERIAL_PH) or (-1 in SERIAL_PH)
        if SERIAL and self.ops:
            po = self.ops[-1]
            if po.fn is not None:
                deps[po.idx] = po
        if SERIAL:
            o.deps = [d for d in deps.values() if d.phase == ph]
        elif not dma:
            raw = set()
            if eng != "pe":
                for b in reads:
                    for w in b.writers:
                        if (not w.is_dma) and w.eng == eng:
                            raw.add(w.idx)
            o.deps = [d for d in deps.values() if d.phase == ph and (d.is_dma or d.eng != eng or d.idx in raw)]
        else:
            o.deps = [d for d in deps.values() if d.phase == ph]
        for d in o.deps:
            d.needs_inc = True
        for b in reads:
            if not dma:
                b.readers = [r for r in b.readers if r.is_dma or r.eng != eng]
            b.readers.append(o)
        for b in writes:
            if b.readers:
                b.writers = [o]
                b.readers = []
            elif disjoint:
                if not dma:
                    b.writers = [w for w in b.writers if w.is_dma or w.eng != eng]
                b.writers.append(o)
            else:
                b.writers = [o]
        for b in locks:
            b.writers = [o]
        self.ops.append(o)
        if dma:
            self.dmas.append(o)
        else:
            self.last[eng] = o
        return o

    def barrier(self):
        lasts = [o for o in self.last.values() if o.phase == self.phase]
        dmas = self.dmas
        self.dmas = []
        self.last = {}
        for o in lasts:
            o.needs_inc = True
        for e in self.ALL:
            o = Op()
            o.eng = e
            o.fn = None
            o.is_dma = False
            o.needs_inc = False
            o.sem = None
            o.val = 0
            o.clock = None
            o.idx = self.nops
            o.phase = self.phase
            self.nops += 1
            o.deps = [l for l in lasts if l.eng != e] + dmas
            self.ops.append(o)

    def emit(self):
        nc = self.nc
        for o in self.ops:
            e = o.eng
            eng = self.engs[e]
            clk = self.clocks[e]
            deps = list(o.deps)
            slot = None
            if o.is_dma:
                if e not in self.dma_sems:
                    self.dma_sems[e] = [nc.alloc_semaphore("d_%s_%d" % (e, i)) for i in range(NDMA_SEMS)]
                    self.dma_cnt[e] = [0] * NDMA_SEMS
                    self.dma_last[e] = [None] * NDMA_SEMS
                    self.dma_rr[e] = 0
                slot = self.dma_rr[e] % NDMA_SEMS
                self.dma_rr[e] += 1
                if self.dma_last[e][slot] is not None:
                    deps.append(self.dma_last[e][slot])
            if len(deps) > 1:
                deps.sort(key=lambda d: -d.idx)
            for d in deps:
                k = id(d.sem)
                if clk.get(k, (None, 0))[1] >= d.val:
                    continue
                eng.wait_ge(d.sem, d.val)
                self.nwaits += 1
                for kk, vv in d.clock.items():
                    if clk.get(kk, (None, 0))[1] < vv[1]:
                        clk[kk] = vv
            if o.fn is None:
                continue
            ins = getattr(eng, o.fn[0])(**o.fn[1])
            self.ninst[e] += 1
            if o.is_dma:
                sem = self.dma_sems[e][slot]
                self.dma_cnt[e][slot] += 16
                o.sem = sem
                o.val = self.dma_cnt[e][slot]
                ins.then_inc(sem, 16)
                self.dma_last[e][slot] = o
                c = dict(clk)
                c[id(sem)] = (sem, o.val)
                o.clock = c
            elif o.needs_inc:
                if self.cur_cnt[e] >= SEM_LIMIT:
                    self.nsw[e] += 1
                    self.cur_sem[e] = nc.alloc_semaphore("s_%s_%d" % (e, self.nsw[e]))
                    self.cur_cnt[e] = 0
                self.cur_cnt[e] += 1
                o.sem = self.cur_sem[e]
                o.val = self.cur_cnt[e]
                ins.then_inc(o.sem, 1)
                c = dict(clk)
                c[id(o.sem)] = (o.sem, o.val)
                o.clock = c
            o.fn = None
            o.deps = None
        self.ops = []

    def mm(self, out, lhsT, rhs, start=True, stop=True):
        ob, oa = _ba(out)
        lb, la = _ba(lhsT)
        rb, ra = _ba(rhs)
        return self.op("pe", "matmul", dict(out=oa, lhsT=la, rhs=ra, start=start, stop=stop),
                       reads=[lb, rb], writes=[ob], disjoint=True if not start else False)

    def tr(self, out, in_, ident):
        ob, oa = _ba(out)
        ib, ia = _ba(in_)
        db, da = _ba(ident)
        return self.op("pe", "transpose", dict(out=oa, in_=ia, identity=da), reads=[ib, db], writes=[ob], disjoint=True)

    def actv(self, out, in_, func, scale=1.0, bias=None, disjoint=False, eng="act"):
        ob, oa = _ba(out)
        ib, ia = _ba(in_)
        kw = dict(out=oa, in_=ia, func=func)
        reads = [ib]
        if isinstance(scale, tuple) or isinstance(scale, Buf):
            sb_, sa = _ba(scale)
            kw["scale"] = sa
            reads.append(sb_)
        elif scale != 1.0:
            kw["scale"] = float(scale)
        if bias is not None:
            if isinstance(bias, (tuple, Buf)):
                bb, ba = _ba(bias)
                kw["bias"] = ba
                reads.append(bb)
            else:
                kw["bias"] = float(bias)
        return self.op("act", "activation", kw, reads=reads, writes=[ob], disjoint=disjoint)

    def tt(self, eng, out, in0, in1, op, disjoint=False):
        ob, oa = _ba(out)
        ab, aa = _ba(in0)
        bb, ba = _ba(in1)
        return self.op(eng, "tensor_tensor", dict(out=oa, in0=aa, in1=ba, op=op), reads=[ab, bb], writes=[ob], disjoint=disjoint)

    def ts(self, eng, out, in0, s1, s2=None, op0=ALU.mult, op1=None, disjoint=False):
        ob, oa = _ba(out)
        ab, aa = _ba(in0)
        reads = [ab]
        kw = dict(out=oa, in0=aa, op0=op0)
        if isinstance(s1, (tuple, Buf)):
            b_, a_ = _ba(s1)
            reads.append(b_)
            kw["scalar1"] = a_
        else:
            kw["scalar1"] = float(s1)
        if s2 is None:
            kw["scalar2"] = None
        elif isinstance(s2, (tuple, Buf)):
            b_, a_ = _ba(s2)
            reads.append(b_)
            kw["scalar2"] = a_
        else:
            kw["scalar2"] = float(s2)
        if op1 is not None:
            kw["op1"] = op1
        return self.op(eng, "tensor_scalar", kw, reads=reads, writes=[ob], disjoint=disjoint)

    def stt(self, out, in0, scalar, in1, op0, op1, disjoint=False):
        ob, oa = _ba(out)
        ab, aa = _ba(in0)
        bb, ba = _ba(in1)
        reads = [ab, bb]
        if isinstance(scalar, (tuple, Buf)):
            b_, a_ = _ba(scalar)
            reads.append(b_)
            sc = a_
        else:
            sc = float(scalar)
        return self.op("dve", "scalar_tensor_tensor", dict(out=oa, in0=aa, scalar=sc, in1=ba, op0=op0, op1=op1),
                       reads=reads, writes=[ob], disjoint=disjoint)

    def copy(self, eng, out, in_, disjoint=False):
        ob, oa = _ba(out)
        ib, ia = _ba(in_)
        if eng == "act":
            return self.op("act", "activation", dict(out=oa, in_=ia, func=AF.Copy), reads=[ib], writes=[ob], disjoint=disjoint)
        return self.op(eng, "tensor_copy", dict(out=oa, in_=ia), reads=[ib], writes=[ob], disjoint=disjoint)

    def recip(self, out, in_, disjoint=False):
        ob, oa = _ba(out)
        ib, ia = _ba(in_)
        return self.op("dve", "reciprocal", dict(out=oa, in_=ia), reads=[ib], writes=[ob], disjoint=disjoint)

    def memset(self, eng, out, val, disjoint=False):
        ob, oa = _ba(out)
        return self.op(eng, "memset", dict(ap=oa, constant=float(val)), writes=[ob], disjoint=disjoint)

    def dma(self, out, in_, eng="sp", disjoint=True):
        reads, writes = [], []
        if isinstance(out, (tuple, Buf)):
            ob, oa = _ba(out)
            writes.append(ob)
        else:
            oa = out
        if isinstance(in_, (tuple, Buf)):
            ib, ia = _ba(in_)
            reads.append(ib)
        else:
            ia = in_
        return self.op(eng, "dma_start", dict(out=oa, in_=ia), reads=reads, writes=writes, dma=True, disjoint=disjoint)


D = 2048
KC = 16
FF = 5632
FC = 44
TT = 512
EPS = 1e-6
MIXC = 52
NVEC = 104

R_AQ, R_AF, R_AI, R_AG = 0, 512, 1024, 1536
R_BQ, R_BK, R_BV = 2048, 2560, 3072
R_CQ, R_CK, R_CV = 3584, 4352, 5120
R_DQ, R_DK, R_DV = 5888, 6400, 6528
Y_A, Y_B, Y_C, Y_D = 0, 512, 1024, 1280


class G:
    pass


def load_vec(S, g, l):
    vec = S.sb("vec", [128, NVEC + 48], F32)
    S.dma((vec, vec[:, 0:NVEC]), g.vecs[l])
    S.ts("dve", (vec, vec[:, NVEC:NVEC + 16]), (vec, vec[:, 16:32]), 0.5)
    S.ts("dve", (vec, vec[:, NVEC + 16:NVEC + 32]), (vec, vec[:, 80:96]), 0.5)
    return vec


def phase_consts(S, g):
    ones = S.sb("ones", [128, 128], BF16)
    S.memset("dve", ones, 1.0)
    epsb = S.sb("epsb", [128, 1], F32)
    S.memset("dve", epsb, EPS)
    return ones, epsb


def rms_rstd(S, ss_ps, rstd, tmp, epsb, n):
    S.actv(tmp, ss_ps, AF.Sqrt, scale=1.0 / n, bias=(epsb, epsb[:, 0:1]))
    S.recip(rstd, tmp)


def prenorm_tile(S, g, hsrc, tok, bufX, xn, sq, tmp, rstd, ones, epsb, ssb, vec, gcol):
    srcv = hsrc.rearrange("(kc p) t -> p kc t", p=128)
    S.dma(bufX, srcv[:, :, tok], disjoint=False)
    for kc in range(KC):
        q = sq[kc % 2]
        S.actv(q, (bufX, bufX[:, kc, :]), AF.Square)
        S.mm(ssb, ones, q, start=(kc == 0), stop=(kc == KC - 1))
    rms_rstd(S, ssb, rstd, tmp, epsb, D)
    for kc in range(KC):
        S.stt((xn, xn[:, kc, :]), (bufX, bufX[:, kc, :]), (vec, vec[:, gcol + kc:gcol + kc + 1]), rstd,
              ALU.mult, ALU.mult, disjoint=True)


def postnorm_residual(S, g, hsrc, hdst, tok, bufX, rstd, tmp, epsb, ssb, vec, gcol, hre, hout):
    rms_rstd(S, ssb, rstd, tmp, epsb, D)
    for dc in range(KC):
        hr = hre[dc % 2]
        ho = hout[dc % 2]
        S.dma(hr, hsrc[dc * 128:(dc + 1) * 128, tok], disjoint=False)
        S.stt(ho, (bufX, bufX[:, dc, :]), (vec, vec[:, gcol + dc:gcol + dc + 1]), rstd, ALU.mult, ALU.mult)
        S.tt("dve", ho, ho, hr, ALU.add)
        S.dma(hdst[dc * 128:(dc + 1) * 128, tok], ho)


def prenorm_compute(S, bufX, xn, sq, tmp, rstd, ones, epsb, ssb, vec, gcol):
    for kc in range(KC):
        q = sq[kc % 2]
        S.actv(q, (bufX, bufX[:, kc, :]), AF.Square)
        S.mm(ssb, ones, q, start=(kc == 0), stop=(kc == KC - 1))
    rms_rstd(S, ssb, rstd, tmp, epsb, D)
    for kc in range(KC):
        S.stt((xn, xn[:, kc, :]), (bufX, bufX[:, kc, :]), (vec, vec[:, gcol + kc:gcol + kc + 1]), rstd,
              ALU.mult, ALU.mult, disjoint=True)


class ResidualPipe:
    def __init__(self, S, hsrc, hdst, vec, gcol, hre, hout):
        self.S, self.hsrc, self.hdst, self.vec, self.gcol, self.hre, self.hout = S, hsrc, hdst, vec, gcol, hre, hout
        self.pending = None

    def start(self, tok, bufX, rstd):
        assert self.pending is None
        self.pending = [tok, bufX, rstd, 0, 0]

    def step(self):
        if self.pending is None:
            return False
        S = self.S
        tok, bufX, rstd, nl, ncp = self.pending
        while nl < KC and nl < ncp + 2:
            hr = self.hre[nl % 2]
            S.dma(hr, self.hsrc[nl * 128:(nl + 1) * 128, tok], disjoint=False)
            nl += 1
        if nl > ncp:
            dc = ncp
            hr = self.hre[dc % 2]
            ho = self.hout[dc % 2]
            S.stt(ho, (bufX, bufX[:, dc, :]), (self.vec, self.vec[:, self.gcol + dc:self.gcol + dc + 1]), rstd,
                  ALU.mult, ALU.mult)
            S.tt("dve", ho, ho, hr, ALU.add)
            S.dma(self.hdst[dc * 128:(dc + 1) * 128, tok], ho)
            ncp += 1
        self.pending[3], self.pending[4] = nl, ncp
        if ncp >= KC:
            self.pending = None
        return True

    def flush(self):
        while self.step():
            pass


def ffn_phase(S, g, l, which, hsrc, hdst):
    NT = g.T // TT
    w13t = g.w13t[which][l]
    w2t = g.w2t[which][l]
    S.begin_phase()
    ones, epsb = phase_consts(S, g)
    vec = load_vec(S, g, l)
    gpre = 0 if which == 0 else 64
    gpost = NVEC if which == 0 else NVEC + 16
    bufX = [S.sb("bufX%d" % i, [128, KC, TT], F32) for i in range(2)]
    xn = [S.sb("xn%d" % i, [128, KC, TT], BF16) for i in range(2)]
    act = S.sb("actT", [128, FC, TT], BF16)
    w13b = [S.sb("w13b%d" % i, [128, KC, 128], BF16) for i in range(4)]
    w2b = [S.sb("w2b%d" % i, [128, FC, 128], BF16) for i in range(2)]
    sq = [S.sb("sq%d" % i, [128, TT], BF16) for i in range(2)]
    sq2 = [S.sb("sqb%d" % i, [128, TT], BF16) for i in range(2)]
    tmp = [S.sb("tmp%d" % i, [128, TT], F32) for i in range(2)]
    tpre = S.sb("tpre", [128, TT], F32)
    tpost = S.sb("tpost", [128, TT], F32)
    rpre = S.sb("rpre", [128, TT], F32)
    rpost = S.sb("rpost", [128, TT], F32)
    hre = [S.sb("hre%d" % i, [128, TT], F32) for i in range(2)]
    hout = [S.sb("hout%d" % i, [128, TT], F32) for i in range(2)]
    psb = [S.ps("psb%d" % i, [128, 512], F32) for i in range(8)]
    ss_pre = psb[6]
    ss_post = psb[7]
    srcv = hsrc.rearrange("(kc p) t -> p kc t", p=128)
    res = ResidualPipe(S, hsrc, hdst, vec, gpost, hre, hout)

    def load_prenorm(tt):
        tok = slice(tt * TT, (tt + 1) * TT)
        X = bufX[tt % 2]
        S.dma(X, srcv[:, :, tok], disjoint=False)
        prenorm_compute(S, X, xn[tt % 2], sq, tpre, rpre, ones, epsb, ss_pre, vec, gpre)

    n13 = 0
    n2 = 0
    load_prenorm(0)
    for tt in range(NT):
        tok = slice(tt * TT, (tt + 1) * TT)
        X = bufX[tt % 2]
        XN = xn[tt % 2]
        for j in range(FC):
            wb = []
            for half in range(2):
                wbf = w13b[n13 % 4]
                n13 += 1
                S.dma((wbf, wbf[:].rearrange("p a b -> p (a b)")), w13t[half * FC + j], eng="pool", disjoint=False)
                wb.append(wbf)
            pg = psb[(j % 2) * 2]
            pu = psb[(j % 2) * 2 + 1]
            for half, pp in ((0, pg), (1, pu)):
                for kc in range(KC):
                    S.mm(pp, (wb[half], wb[half][:, kc, :]), (XN, XN[:, kc, :]), start=(kc == 0), stop=(kc == KC - 1))
            tm = tmp[j % 2]
            S.actv(tm, pg, AF.Silu)
            S.tt("dve", (act, act[:, j, :]), tm, pu, ALU.mult, disjoint=True)
            if j >= 2:
                res.step()
        res.flush()
        if tt + 1 < NT:
            load_prenorm(tt + 1)
        prev_sq = None
        for dc in range(KC):
            wbf = w2b[n2 % 2]
            n2 += 1
            for q in range(4):
                S.dma((wbf, wbf[:, q * 11:(q + 1) * 11, :].rearrange("p a b -> p (a b)")),
                      w2t[dc][:, q * 1408:(q + 1) * 1408], eng="pool")
            po = psb[4 + (dc % 2)]
            for fc in range(FC):
                S.mm(po, (wbf, wbf[:, fc, :]), (act, act[:, fc, :]), start=(fc == 0), stop=(fc == FC - 1))
            if prev_sq is not None:
                S.mm(ss_post, ones, prev_sq, start=(dc == 1), stop=False)
            S.actv((X, X[:, dc, :]), po, AF.Copy, disjoint=True)
            q_ = sq2[dc % 2]
            S.actv(q_, po, AF.Square)
            prev_sq = q_
        S.mm(ss_post, ones, prev_sq, start=False, stop=True)
        rms_rstd(S, ss_post, rpost, tpost, epsb, D)
        res.start(tok, X, rpost)
    res.flush()
    S.end_phase()


def inproj_phase(S, g, l, h):
    NT = g.T // TT
    S.begin_phase()
    ones, epsb = phase_consts(S, g)
    vec = load_vec(S, g, l)
    bufX = [S.sb("bufX%d" % i, [128, KC, TT], F32) for i in range(2)]
    xn = [S.sb("xn%d" % i, [128, KC, TT], BF16) for i in range(2)]
    wb = [S.sb("wb%d" % i, [128, KC, 128], BF16) for i in range(6)]
    sq = [S.sb("sq%d" % i, [128, TT], BF16) for i in range(2)]
    tmp = S.sb("tmp", [128, TT], F32)
    rstd = S.sb("rstd", [128, TT], F32)
    ost = [S.sb("ost%d" % i, [128, TT], BF16) for i in range(4)]
    psb = [S.ps("psb%d" % i, [128, 512], F32) for i in range(5)]
    ssb = psb[4]
    srcv = h.rearrange("(kc p) t -> p kc t", p=128)
    uv = g.uT.rearrange("(kc p) t -> p kc t", p=128)

    def load_prenorm(tt):
        tok = slice(tt * TT, (tt + 1) * TT)
        X = bufX[tt % 2]
        S.dma(X, srcv[:, :, tok], disjoint=False)
        prenorm_compute(S, X, xn[tt % 2], sq, tmp, rstd, ones, epsb, ssb, vec, 32)
        S.dma(uv[:, :, tok], xn[tt % 2])

    nw = 0
    load_prenorm(0)
    for tt in range(NT):
        tok = slice(tt * TT, (tt + 1) * TT)
        XN = xn[tt % 2]
        for c in range(MIXC):
            if c == MIXC - 8 and tt + 1 < NT:
                load_prenorm(tt + 1)
            w = wb[nw % 6]
            S.dma((w, w[:].rearrange("p a b -> p (a b)")), g.wint[l][c], eng="pool", disjoint=False)
            pp = psb[nw % 4]
            o = ost[nw % 4]
            for kc in range(KC):
                S.mm(pp, (w, w[:, kc, :]), (XN, XN[:, kc, :]), start=(kc == 0), stop=(kc == KC - 1))
            S.copy("act" if nw % 2 == 0 else "dve", o, pp)
            S.dma(g.projT[c * 128:(c + 1) * 128, tok], o)
            nw += 1
    S.end_phase()


BR_K = (4, 4, 2, 4)


def merge_phase(S, g, l, h):
    NT = g.T // TT
    S.begin_phase()
    ones, epsb = phase_consts(S, g)
    vec = load_vec(S, g, l)
    bufX = [S.sb("bufX%d" % i, [128, KC, TT], F32) for i in range(2)]
    uT = [S.sb("uT%d" % i, [128, KC, TT], BF16) for i in range(2)]
    yT = [S.sb("yT%d" % i, [128, 14, TT], BF16) for i in range(2)]
    mg = S.sb("mg", [128, KC, TT], BF16)
    wg = [S.sb("wg%d" % i, [128, KC, 128], BF16) for i in range(6)]
    wbr = [S.sb("wbr%d" % i, [128, 14, 128], BF16) for i in range(2)]
    wo = [S.sb("wo%d" % i, [128, KC, 128], BF16) for i in range(2)]
    sg = [S.sb("sg%d" % i, [128, TT], F32) for i in range(2)]
    acc = S.sb("acc", [128, TT], F32)
    t2 = S.sb("t2", [128, TT], F32)
    sq = [S.sb("sq%d" % i, [128, TT], BF16) for i in range(2)]
    tmp = S.sb("tmp", [128, TT], F32)
    rstd = S.sb("rstd", [128, TT], F32)
    hre = [S.sb("hre%d" % i, [128, TT], F32) for i in range(2)]
    hout = [S.sb("hout%d" % i, [128, TT], F32) for i in range(2)]
    psb = [S.ps("psb%d" % i, [128, 512], F32) for i in range(7)]
    ssb = psb[6]
    res = ResidualPipe(S, h, h, vec, 48, hre, hout)
    uv = g.uT.rearrange("(kc p) t -> p kc t", p=128)
    yv = g.yT.rearrange("(rc p) t -> p rc t", p=128)

    def load_in(tt):
        tok = slice(tt * TT, (tt + 1) * TT)
        S.dma(uT[tt % 2], uv[:, :, tok], disjoint=False)
        S.dma(yT[tt % 2], yv[:, :, tok], disjoint=False)

    ng = 0
    nb = 0
    no = 0
    load_in(0)
    for tt in range(NT):
        tok = slice(tt * TT, (tt + 1) * TT)
        U = uT[tt % 2]
        Y = yT[tt % 2]
        X = bufX[tt % 2]
        for dc in range(KC):
            wbt = wbr[nb % 2]
            nb += 1
            S.dma((wbt, wbt[:].rearrange("p a b -> p (a b)")), g.wbt[l][dc], eng="pool", disjoint=False)
            rc0 = 0
            for i in range(4):
                w = wg[ng % 6]
                S.dma((w, w[:].rearrange("p a b -> p (a b)")), g.wint[l][MIXC + i * 16 + dc], eng="pool", disjoint=False)
                pgt = psb[(ng % 2) * 2]
                ptm = psb[(ng % 2) * 2 + 1]
                s_ = sg[ng % 2]
                ng += 1
                for kc in range(KC):
                    S.mm(pgt, (w, w[:, kc, :]), (U, U[:, kc, :]), start=(kc == 0), stop=(kc == KC - 1))
                nk = BR_K[i]
                for r in range(nk):
                    S.mm(ptm, (wbt, wbt[:, rc0 + r, :]), (Y, Y[:, rc0 + r, :]), start=(r == 0), stop=(r == nk - 1))
                rc0 += nk
                S.actv(s_, pgt, AF.Sigmoid)
                if i == 0:
                    S.tt("dve", acc, s_, ptm, ALU.mult)
                elif i < 3:
                    S.tt("dve", t2, s_, ptm, ALU.mult)
                    S.tt("dve", acc, acc, t2, ALU.add)
                else:
                    S.tt("dve", t2, s_, ptm, ALU.mult)
                    S.tt("dve", (mg, mg[:, dc, :]), acc, t2, ALU.add, disjoint=True)
            res.step()
        res.flush()
        if tt + 1 < NT:
            load_in(tt + 1)
        prev_sq = None
        for dc in range(KC):
            w = wo[no % 2]
            no += 1
            S.dma((w, w[:].rearrange("p a b -> p (a b)")), g.wot[l][dc], eng="pool", disjoint=False)
            po = psb[4 + (dc % 2)]
            for kc in range(KC):
                S.mm(po, (w, w[:, kc, :]), (mg, mg[:, kc, :]), start=(kc == 0), stop=(kc == KC - 1))
            if prev_sq is not None:
                S.mm(ssb, ones, prev_sq, start=(dc == 1), stop=False)
            S.actv((X, X[:, dc, :]), po, AF.Copy, disjoint=True)
            q_ = sq[dc % 2]
            S.actv(q_, po, AF.Square)
            prev_sq = q_
        S.mm(ssb, ones, prev_sq, start=False, stop=True)
        rms_rstd(S, ssb, rstd, tmp, epsb, D)
        res.start(tok, X, rstd)
    res.flush()
    S.end_phase()


import math, os

C_PATTERNS = ((128, 1), (512, 4), (2048, 16))
CST_BI = 0
CST_ID = 1024
CST_NTI = 1152
CST_NSL = 1280
CST_MB = 1408
CST_M2 = 3456
NCST = 3584
NEG = -30000.0
LAYERS_A = 4


def _rel_bucket(dist):
    dist = np.asarray(dist)
    d = np.maximum(dist, 1).astype(np.float32)
    large = 16 + (np.log(d / np.float32(16)) / np.float32(math.log(2048 / 16)) * np.float32(16)).astype(np.int32)
    large = np.minimum(large, 31)
    return np.where(dist < 16, dist, large)


def bi_tile(kind, pc):
    k = np.arange(128)[:, None]
    q = np.arange(128)[None, :]
    du = q - k + (128 if pc == 0 else 0)
    if kind == 'D':
        valid = (du >= 0) & (du < 128)
        r = 1
    else:
        valid = (du >= 0) & (du <= 128)
        r = C_PATTERNS[kind][1]
    b = _rel_bucket(np.maximum(du, 0) * r)
    return np.where(valid, b, -1).astype(np.float32)


BI_KINDS = [('D', 0), ('D', 1), (0, 0), (0, 1), (1, 0), (1, 1), (2, 0), (2, 1)]


def make_consts():
    c = np.zeros((128, NCST), np.float32)
    for i, (kind, pc) in enumerate(BI_KINDS):
        c[:, CST_BI + i * 128:CST_BI + (i + 1) * 128] = bi_tile(kind, pc)
    c[:, CST_ID:CST_ID + 128] = np.eye(128, dtype=np.float32)
    j = np.arange(128)[:, None]
    s = np.arange(128)[None, :]
    c[:, CST_NTI:CST_NTI + 128] = np.where(j >= s, -1.0, 0.0)
    c[:, CST_NSL:CST_NSL + 128] = np.where(j < s, -1.0, 0.0)
    col = np.arange(512)[None, :]
    for m in range(4):
        c[:, CST_MB + m * 512:CST_MB + (m + 1) * 512] = np.where(128 * m + j < col, 1.0, 0.0)
    c[:, CST_M2:CST_M2 + 128] = np.where((j // 64 == s // 64) & (j <= s), 1.0, 0.0)
    return c


def setup_phase(S, g):
    S.begin_phase()
    bi = S.sb("bi", [128, 8, 128], F32)
    S.dma(bi, g.cst[:, CST_BI:CST_BI + 1024].rearrange("p (a b) -> p a b", b=128), disjoint=False)
    relb = S.sb("relb", [128, 640], F32)
    S.dma(relb, g.relb, disjoint=False)
    ebD = S.sb("ebD", [128, 2, 8, 128], F32)
    ebC = S.sb("ebC", [128, 3, 2, 2, 2, 128], F32)
    tmps = [S.sb("tb%d" % i, [128, 128], F32) for i in range(4)]
    n = 0
    for ti, (kind, pc) in enumerate(BI_KINDS):
        tile_np = bi_tile(kind, pc)
        buckets = sorted(set(int(v) for v in np.unique(tile_np) if v >= 0))
        nh = 8 if kind == 'D' else 4
        for h in range(nh):
            if kind == 'D':
                dst = (ebD, ebD[:, pc, h, :])
                col = 12 + h
                eng = "dve"
            else:
                dst = (ebC, ebC[:, kind, h // 2, h % 2, pc, :])
                col = kind * 4 + h
                eng = "dve"
            src = (bi, bi[:, ti, :])
            S.ts(eng, dst, src, 0.0, NEG, op0=ALU.is_lt, op1=ALU.mult, disjoint=True)
            for b in buckets:
                tm = tmps[(n % 2) + (0 if eng == "dve" else 2)]
                n += 1
                S.ts(eng, tm, src, float(b), (relb, relb[:, b * 20 + col:b * 20 + col + 1]), op0=ALU.is_equal, op1=ALU.mult)
                S.tt(eng, dst, dst, tm, ALU.add, disjoint=True)
    S.dma(g.ebD, (ebD, ebD[:].rearrange("p a b c -> p (a b c)")))
    S.dma(g.ebC, (ebC, ebC[:].rearrange("p a b c d e -> p (a b c d e)")))
    S.end_phase()


def load_ident(S, g):
    idf = S.sb("idf", [128, 128], F32)
    S.dma(idf, g.cst[:, CST_ID:CST_ID + 128], disjoint=False)
    idb = S.sb("idb", [128, 128], BF16)
    S.copy("dve", idb, idf)
    return idb


def to_tokmajor(S, src, dst, NB, idb, pT, cnt, engs=("act", "dve")):
    for n in range(NB):
        p = pT[cnt[0] % len(pT)]
        S.tr(p, (src, src[:, n * 128:(n + 1) * 128]), idb)
        S.copy(engs[cnt[0] % len(engs)], (dst, dst[:, n, :]), p, disjoint=True)
        cnt[0] += 1


def mixD_phase(S, g, l):
    T = g.T
    NB = T // 128
    S.begin_phase()
    idb = load_ident(S, g)
    ones64 = S.sb("ones64", [128, 64], BF16)
    S.memset("dve", ones64, 1.0)
    qD = S.sb("qD", [64, 8, T], BF16)
    kD = S.sb("kD", [64, 2, T], BF16)
    vT = S.sb("vT", [128, T], BF16)
    Vtok = S.sb("Vtok", [128, NB, 128], BF16)
    yD = S.sb("yD", [64, 8, T], BF16)
    eb = S.sb("eb", [128, 2, 8, 128], F32)
    S.dma((eb, eb[:].rearrange("p a b c -> p (a b c)")), g.ebD, disjoint=False)
    sk = S.sb("sk", [64, 8], F32)
    S.dma(sk, g.sinks[0:64, l * 8:(l + 1) * 8], disjoint=False)
    es = S.sb("es", [64, 8], F32)
    S.actv(es, sk, AF.Exp)
    esb = S.sb("esb", [64, 8, 128], F32)
    S.copy("dve", esb, (es, es[:, :].unsqueeze(2).to_broadcast([64, 8, 128])))
    for h in range(8):
        S.dma((qD, qD[:, h, :]), g.projT[R_DQ + h * 64:R_DQ + (h + 1) * 64, :])
    for kv in range(2):
        S.dma((kD, kD[:, kv, :]), g.projT[R_DK + kv * 64:R_DK + (kv + 1) * 64, :])
    S.dma(vT, g.projT[R_DV:R_DV + 128, :], disjoint=False)
    pT = [S.ps("pT%d" % i, [128, 128], BF16) for i in range(2)]
    pS = [S.ps("pS%d" % i, [128, 2, 512], F32) for i in range(2)]
    pO = S.ps("pO", [128, 512], F32)
    pD = S.ps("pD", [128, 512], F32)
    Zs = [S.sb("Zs%d" % i, [128, 2, 512], F32) for i in range(2)]
    Pb = [S.sb("Pb%d" % i, [128, 2, 512], BF16) for i in range(2)]
    dt = S.sb("dt", [64, 512], F32)
    cnt = [0]
    to_tokmajor(S, vT, Vtok, NB, idb, pT, cnt)
    blocks = [(n, gk) for n in range(NB) for gk in range(2)]

    def emit_s(i):
        n, gk = blocks[i]
        Sp = pS[i % 2]
        rq = (qD, qD[:, 4 * gk:4 * gk + 4, n * 128:(n + 1) * 128])
        if n > 0:
            S.mm((Sp, Sp[:, 0, :]), (kD, kD[:, gk, (n - 1) * 128:n * 128]), rq)
        S.mm((Sp, Sp[:, 1, :]), (kD, kD[:, gk, n * 128:(n + 1) * 128]), rq, start=True)

    def emit_z(i):
        n, gk = blocks[i]
        Sp = pS[i % 2]
        Z = Zs[i % 2]
        P = Pb[i % 2]
        lo = 0 if n > 0 else 1
        S.stt((Z, Z[:, lo:2, :].rearrange("p a (h q) -> p a h q", h=4)),
              (Sp, Sp[:, lo:2, :].rearrange("p a (h q) -> p a h q", h=4)), 0.125,
              (eb, eb[:, lo:2, 4 * gk:4 * gk + 4, :]), ALU.mult, ALU.add)
        S.actv((P, P[:, lo:2, :]), (Z, Z[:, lo:2, :]), AF.Exp)

    def emit_rest(i):
        n, gk = blocks[i]
        P = Pb[i % 2]
        if i + 1 < len(blocks):
            emit_s(i + 1)
        for (pp, lhs_of) in ((pO, None), (pD, ones64)):
            if n > 0:
                lh = (Vtok, Vtok[:, n - 1, gk * 64:(gk + 1) * 64]) if lhs_of is None else ones64
                S.mm((pp, pp[0:64, :]), lh, (P, P[:, 0, :]), start=True, stop=False)
            lh = (Vtok, Vtok[:, n, gk * 64:(gk + 1) * 64]) if lhs_of is None else ones64
            S.mm((pp, pp[0:64, :]), lh, (P, P[:, 1, :]), start=(n == 0), stop=True)
        if i + 1 < len(blocks):
            emit_z(i + 1)
        d_ = dts[i % 2]
        S.tt("dve", (d_, d_[:, :].rearrange("p (h q) -> p h q", h=4)),
             (pD, pD[0:64, :].rearrange("p (h q) -> p h q", h=4)), (esb, esb[:, 4 * gk:4 * gk + 4, :]), ALU.add)
        S.actv(d_, d_, AF.Ln)
        S.actv(d_, d_, AF.Exp, scale=-1.0)
        S.tt("dve", (yD, yD[:, 4 * gk:4 * gk + 4, n * 128:(n + 1) * 128]),
             (pO, pO[0:64, :].rearrange("p (h q) -> p h q", h=4)),
             (d_, d_[:, :].rearrange("p (h q) -> p h q", h=4)), ALU.mult, disjoint=True)

    dts = [dt, S.sb("dt2", [64, 512], F32)]
    emit_s(0)
    emit_z(0)
    for i in range(len(blocks)):
        emit_rest(i)
    for h in range(8):
        S.dma(g.yT[Y_D + h * 64:Y_D + (h + 1) * 64, :], (yD, yD[:, h, :]))
    S.end_phase()


def mixC_phase(S, g, l):
    T = g.T
    NB = T // 128
    S.begin_phase()
    idb = load_ident(S, g)
    ones64 = S.sb("ones64", [128, 64], BF16)
    S.memset("dve", ones64, 1.0)
    eb = S.sb("eb", [128, 3, 2, 2, 2, 128], F32)
    S.dma((eb, eb[:].rearrange("p a b c d e -> p (a b c d e)")), g.ebC, disjoint=False)
    natq = [S.sb("natq%d" % i, [64, 2, T], BF16) for i in range(2)]
    perq = [S.sb("perq%d" % i, [64, 2, T], BF16) for i in range(2)]
    natv = S.sb("natv", [128, T], BF16)
    perv = S.sb("perv", [128, T], BF16)
    Vtok = S.sb("Vtok", [128, NB, 128], BF16)
    accN = S.sb("accN", [64, 2, T], F32)
    accD = S.sb("accD", [64, 2, T], F32)
    ybf = S.sb("ybf", [64, 2, T], BF16)
    pT = [S.ps("pT%d" % i, [128, 128], BF16) for i in range(2)]
    pS = [S.ps("pS%d" % i, [128, 2, 2, 128], F32) for i in range(2)]
    pO = [S.ps("pO%d" % i, [128, 512], F32) for i in range(2)]
    pD = [S.ps("pD%d" % i, [128, 512], F32) for i in range(2)]
    Zs = [S.sb("Zs%d" % i, [128, 2, 2, 128], F32) for i in range(2)]
    Pb = [S.sb("Pb%d" % i, [128, 2, 2, 128], BF16) for i in range(2)]
    cnt = [0]
    it = 0
    rows = (R_CQ, R_CK, R_CV)
    for hp in range(2):
        for gi, (win, r) in enumerate(C_PATTERNS):
            cur = []
            for j in range(2):
                r0 = rows[j] + gi * 256 + hp * 128
                for hh in range(2):
                    S.dma((natq[j], natq[j][:, hh, :]), g.projT[r0 + hh * 64:r0 + (hh + 1) * 64, :], disjoint=(hh == 1))
                if r > 1:
                    S.copy("act" if j == 0 else "dve", (perq[j], perq[j][:, :, :].rearrange("p h (c i) -> p h c i", c=r)),
                           (natq[j], natq[j][:, :, :].rearrange("p h (i c) -> p h c i", c=r)))
                    cur.append(perq[j])
                else:
                    cur.append(natq[j])
            r0 = rows[2] + gi * 256 + hp * 128
            S.dma(natv, g.projT[r0:r0 + 128, :], disjoint=False)
            if r > 1:
                S.copy("act", (perv, perv[:, :].rearrange("p (c i) -> p c i", c=r)),
                       (natv, natv[:, :].rearrange("p (i c) -> p c i", c=r)))
                vp = perv
            else:
                vp = natv
            qp, kp = cur
            to_tokmajor(S, vp, Vtok, NB, idb, pT, cnt)
            Lb = NB // r
            cblocks = [(c, n) for c in range(r) for n in range(Lb)]

            def emit_s(i, it):
                c, n = cblocks[i]
                pb = c * Lb + n
                Sp = pS[it % 2]
                for hh in range(2):
                    rq = (qp, qp[:, hh, pb * 128:(pb + 1) * 128])
                    if n > 0:
                        S.mm((Sp, Sp[:, hh, 0, :]), (kp, kp[:, hh, (pb - 1) * 128:pb * 128]), rq)
                    S.mm((Sp, Sp[:, hh, 1, :]), (kp, kp[:, hh, pb * 128:(pb + 1) * 128]), rq)

            def emit_z(i, it):
                c, n = cblocks[i]
                Sp = pS[it % 2]
                Z = Zs[it % 2]
                P = Pb[it % 2]
                if n > 0:
                    S.stt(Z, Sp, 0.125, (eb, eb[:, gi, hp, :, :, :]), ALU.mult, ALU.add)
                    S.actv(P, Z, AF.Exp)
                else:
                    S.stt((Z, Z[:, :, 1, :]), (Sp, Sp[:, :, 1, :]), 0.125, (eb, eb[:, gi, hp, :, 1, :]), ALU.mult, ALU.add)
                    S.actv((P, P[:, :, 1, :]), (Z, Z[:, :, 1, :]), AF.Exp)

            def emit_rest(i, it):
                c, n = cblocks[i]
                pb = c * Lb + n
                P = Pb[it % 2]
                po = pO[it % 2]
                pd = pD[it % 2]
                if i + 1 < len(cblocks):
                    emit_s(i + 1, it + 1)
                for hh in range(2):
                    vs = slice(hh * 64, (hh + 1) * 64)
                    cs = slice(hh * 128, (hh + 1) * 128)
                    for (pp, isden) in ((po, False), (pd, True)):
                        if n > 0:
                            lh = ones64 if isden else (Vtok, Vtok[:, pb - 1, vs])
                            S.mm((pp, pp[0:64, cs]), lh, (P, P[:, hh, 0, :]), start=True, stop=False)
                        lh = ones64 if isden else (Vtok, Vtok[:, pb, vs])
                        S.mm((pp, pp[0:64, cs]), lh, (P, P[:, hh, 1, :]), start=(n == 0), stop=True)
                if i + 1 < len(cblocks):
                    emit_z(i + 1, it + 1)
                t0 = c + r * 128 * n
                sl = slice(t0, t0 + r * 127 + 1, r) if r > 1 else slice(t0, t0 + 128)
                for (acc, pp, eng) in ((accN, po, "dve"), (accD, pd, "act")):
                    av = (acc, acc[:, :, sl])
                    pv = (pp, pp[0:64, 0:256].rearrange("p (h q) -> p h q", h=2))
                    if gi == 0:
                        S.copy(eng, av, pv, disjoint=True)
                    else:
                        S.tt("dve", av, av, pv, ALU.add, disjoint=True)

            emit_s(0, it)
            emit_z(0, it)
            for i in range(len(cblocks)):
                emit_rest(i, it)
                it += 1
        S.actv(accD, accD, AF.Ln)
        S.actv(accD, accD, AF.Exp, scale=-1.0)
        S.tt("dve", ybf, accN, accD, ALU.mult)
        for hh in range(2):
            r0 = Y_C + (2 * hp + hh) * 64
            S.dma(g.yT[r0:r0 + 64, :], (ybf, ybf[:, hh, :]))
    S.end_phase()


def load_cst_bf(S, g, name, c0, n):
    f = S.sb(name + "f", [128, n], F32)
    S.dma(f, g.cst[:, c0:c0 + n], disjoint=False)
    b = S.sb(name + "b", [128, n], BF16)
    S.copy("dve", b, f)
    return f, b


def mixB_phase(S, g, l):
    T = g.T
    NB = T // 128
    NQ = T // 512
    S.begin_phase()
    idb = load_ident(S, g)
    _, nti = load_cst_bf(S, g, "nti", CST_NTI, 128)
    _, nsl = load_cst_bf(S, g, "nsl", CST_NSL, 128)
    maskB = S.sb("maskB", [128, 4, 512], F32)
    S.dma((maskB, maskB[:].rearrange("p a b -> p (a b)")), g.cst[:, CST_MB:CST_MB + 2048], disjoint=False)
    oneb = S.sb("oneb", [128, 1], F32)
    S.memset("dve", oneb, 1.0)
    qh = S.sb("qh", [64, 2, T], BF16)
    kh = S.sb("kh", [64, 2, T], BF16)
    vT = S.sb("vT", [128, T], BF16)
    Vtok = S.sb("Vtok", [128, NB, 128], BF16)
    ybf = S.sb("ybf", [64, 2, T], BF16)
    NPS = int(os.environ.get("MK_BNPS", "3"))
    NST = int(os.environ.get("MK_BNST", "3" if NPS == 1 else "2"))
    pSs = [S.ps("pS%d" % i, [128, 512], F32) for i in range(NPS)]
    pT = [S.ps("pT%d" % i, [128, 128], BF16) for i in range(1)]
    sets = []
    pscnt = [0]
    for k in range(NST):
        st = G()
        st.pB = S.ps("pB%d" % k, [128, 512], F32)
        st.pO = S.ps("pO%d" % k, [128, 512], F32)
        st.e = [S.sb("e%d_%d" % (k, i), [128, 512], F32) for i in range(2)]
        st.sp = [S.sb("sp%d_%d" % (k, i), [128, 512], BF16) for i in range(2)]
        st.w = [S.sb("w%d_%d" % (k, i), [128, 512], F32) for i in range(2)]
        st.a = [S.sb("a%d_%d" % (k, i), [128, 512], BF16) for i in range(2)]
        sets.append(st)
    cnt = [0]

    def chain(st, hh, qt):
        qs = (qh, qh[:, hh, qt * 512:(qt + 1) * 512])
        kbs = list(range(4 * qt + 3, -1, -1))
        nk = len(kbs)

        def s_and_exp(i):
            kb = kbs[i]
            m = kb - 4 * qt
            pS = pSs[pscnt[0] % len(pSs)]
            pscnt[0] += 1
            e = st.e[i % 2]
            S.mm(pS, (kh, kh[:, hh, kb * 128:(kb + 1) * 128]), qs)
            S.actv(e, pS, AF.Exp, scale=0.125)
            if m >= 0:
                S.tt("dve", e, e, (maskB, maskB[:, m, :]), ALU.mult)

        s_and_exp(0)
        yield
        for i in range(nk):
            kb = kbs[i]
            e = st.e[i % 2]
            sp = st.sp[i % 2]
            w = st.w[i % 2]
            a = st.a[i % 2]
            first = (i == 0)
            last = (i == nk - 1)
            S.actv(sp, e, AF.Ln, bias=(oneb, oneb[:, 0:1]))
            yield
            if not last:
                s_and_exp(i + 1)
            yield
            S.mm(st.pB, nti, sp, start=first, stop=False)
            yield
            S.actv(w, st.pB, AF.Exp)
            yield
            S.mm(st.pB, nsl, sp, start=False, stop=last)
            S.tt("dve", a, w, e, ALU.mult)
            yield
            S.mm((st.pO, st.pO[0:64, :]), (Vtok, Vtok[:, kb, hh * 64:(hh + 1) * 64]), a, start=first, stop=last)
            yield
        S.copy("act", (ybf, ybf[:, hh, qt * 512:(qt + 1) * 512]), (st.pO, st.pO[0:64, :]), disjoint=True)
        yield

    for hp in range(4):
        for hh in range(2):
            S.dma((qh, qh[:, hh, :]), g.projT[R_BQ + hp * 128 + hh * 64:R_BQ + hp * 128 + (hh + 1) * 64, :], disjoint=(hh == 1))
            S.dma((kh, kh[:, hh, :]), g.projT[R_BK + hp * 128 + hh * 64:R_BK + hp * 128 + (hh + 1) * 64, :], disjoint=(hh == 1))
        S.dma(vT, g.projT[R_BV + hp * 128:R_BV + (hp + 1) * 128, :], disjoint=False)
        to_tokmajor(S, vT, Vtok, NB, idb, pT, cnt)
        work = [(hh, qt) for qt in range(NQ - 1, -1, -1) for hh in range(2)]
        active = []
        free_sets = list(sets)
        while work or active:
            while work and free_sets:
                hh, qt = work.pop(0)
                st = free_sets.pop(0)
                active.append((chain(st, hh, qt), st))
            nxt = []
            for gen, st in active:
                try:
                    next(gen)
                    nxt.append((gen, st))
                except StopIteration:
                    free_sets.append(st)
            active = nxt
        for hh in range(2):
            r0 = Y_B + hp * 128 + hh * 64
            S.dma(g.yT[r0:r0 + 64, :], (ybf, ybf[:, hh, :]))
    S.end_phase()


def mixA_phase(S, g, l):
    T = g.T
    SEG = min(1024, T)
    NSEG = T // SEG
    NBS = SEG // 128
    NCH = SEG // 64
    S.begin_phase()
    idb = load_ident(S, g)
    ones, epsb = phase_consts(S, g)
    vec = load_vec(S, g, l)
    m2 = S.sb("m2", [128, 128], F32)
    S.dma(m2, g.cst[:, CST_M2:CST_M2 + 128], disjoint=False)
    oneb = S.sb("oneb", [128, 1], F32)
    S.memset("dve", oneb, 1.0)
    cmask = S.sb("cmask", [128, SEG], F32)
    S.memset("dve", cmask, 1.0)
    S.memset("dve", (cmask, cmask[:, 0:SEG:64]), 0.0)
    lbl = S.sb("lbl", [128, 4, LAYERS_A], F32)
    S.dma((lbl, lbl[:].rearrange("p a b -> p (a b)")), g.lbl, disjoint=False)
    le = S.sb("le", [128, 4, LAYERS_A], F32)
    S.actv(le, lbl, AF.Exp)
    lsum = S.sb("lsum", [128, 4], F32)
    S.tt("dve", lsum, (le, le[:, :, 0]), (le, le[:, :, 1]), ALU.add)
    for i in range(2, LAYERS_A):
        S.tt("dve", lsum, lsum, (le, le[:, :, i]), ALU.add)
    S.recip(lsum, lsum)
    lb = S.sb("lb", [128, 4], F32)
    oml = S.sb("oml", [128, 4], F32)
    S.memset("dve", lb, 0.0)
    for i in range(1, l + 1):
        S.tt("dve", lb, lb, (le, le[:, :, i]), ALU.add)
    S.tt("dve", lb, lb, lsum, ALU.mult)
    S.ts("dve", oml, lb, -1.0, 1.0, op0=ALU.mult, op1=ALU.add)
    qn = S.sb("qn", [128, SEG], BF16)
    fn = S.sb("fn", [128, SEG], BF16)
    vn = S.sb("vn", [128, SEG], BF16)
    khat = S.sb("khat", [128, SEG], BF16)
    t1 = S.sb("t1", [128, SEG], F32)
    t2 = S.sb("t2", [128, SEG], F32)
    t3 = S.sb("t3", [128, SEG], F32)
    t4 = S.sb("t4", [128, SEG], F32)
    bb = S.sb("bb", [128, SEG], F32)
    sqb = S.sb("sqb", [128, SEG], BF16)
    H = []
    for h in range(4):
        hs = G()
        hs.gn = S.sb("gn%d" % h, [128, SEG], BF16)
        hs.ebt = S.sb("ebt%d" % h, [128, SEG], F32)
        hs.qt = S.sb("qt%d" % h, [128, SEG], BF16)
        hs.kt = S.sb("kt%d" % h, [128, SEG], BF16)
        hs.qb = S.sb("qb%d" % h, [128, SEG], BF16)
        hs.Vt = S.sb("Vt%d" % h, [128, NBS, 128], BF16)
        hs.Kt = S.sb("Kt%d" % h, [128, NBS, 128], BF16)
        hs.oT = S.sb("oT%d" % h, [128, SEG], F32)
        hs.Sst = S.sb("Sst%d" % h, [128, 128], F32)
        hs.Sb = [S.sb("Sb%d_%d" % (h, i), [128, 128], BF16) for i in range(2)]
        hs.Pm = S.sb("Pm%d" % h, [128, 128], BF16)
        S.memset("dve", hs.Sst, 0.0)
        S.memset("dve", hs.Sb[0], 0.0)
        H.append(hs)
    pT = [S.ps("pT%d" % i, [128, 128], BF16) for i in range(2)]
    bsc = S.ps("bsc", [128, 4, 128], F32)
    bua = S.ps("bua", [128, 4, 128], F32)
    bub = S.ps("bub", [128, 4, 128], F32)
    bo = S.ps("bo", [128, 4, 128], F32)
    bss = S.ps("bss", [128, 512], F32)
    for h in range(4):
        H[h].psc = Buf("psc%d" % h, bsc.t, bsc.lock)
        H[h].pua = Buf("pua%d" % h, bua.t, bua.lock)
        H[h].pub = Buf("pub%d" % h, bub.t, bub.lock)
        H[h].po = Buf("po%d" % h, bo.t, bo.lock)
    cnt = [0]
    ysb = [S.sb("ysb%d" % i, [128, SEG], BF16) for i in range(2)]
    for seg in range(NSEG):
        cs = slice(seg * SEG, (seg + 1) * SEG)
        for h in range(4):
            hs = H[h]
            S.dma(qn, g.projT[R_AQ + h * 128:R_AQ + (h + 1) * 128, cs], disjoint=False)
            S.dma(fn, g.projT[R_AF + h * 128:R_AF + (h + 1) * 128, cs], disjoint=False)
            S.dma(vn, g.projT[R_AI + h * 128:R_AI + (h + 1) * 128, cs], disjoint=False)
            S.dma(hs.gn, g.projT[R_AG + h * 128:R_AG + (h + 1) * 128, cs], disjoint=False)
            S.actv(t4, fn, AF.Sigmoid)
            S.actv(t1, t4, AF.Identity, scale=(oml, oml[:, h:h + 1]), bias=(lb, lb[:, h:h + 1]))
            S.actv(t2, t1, AF.Ln)
            S.actv(t3, t1, AF.Identity, scale=-1.0, bias=(oneb, oneb[:, 0:1]))
            S.op("dve", "tensor_tensor_scan", dict(out=bb[:], data0=cmask[:], data1=t2[:], initial=0.0,
                                                   op0=ALU.mult, op1=ALU.add), reads=[cmask, t2], writes=[bb])
            bv = bb[:, :].rearrange("p (c i) -> p c i", i=64)
            bmid = bv[:, :, 31:32].to_broadcast([128, NCH, 64])
            blast = bv[:, :, 63:64].to_broadcast([128, NCH, 64])
            v3 = lambda b_: (b_, b_[:, :].rearrange("p (c i) -> p c i", i=64))
            S.tt("dve", v3(t2), (bb, bv), (bb, bmid), ALU.subtract)
            S.actv(t4, t2, AF.Exp)
            S.tt("dve", hs.qt, qn, t4, ALU.mult)
            S.actv(t4, t2, AF.Exp, scale=-1.0)
            S.tt("dve", hs.kt, t3, t4, ALU.mult)
            S.tt("dve", v3(t2), (bb, bv), (bb, blast), ALU.subtract)
            S.actv(t4, t2, AF.Exp, scale=-1.0)
            S.tt("dve", khat, t3, t4, ALU.mult)
            S.actv(hs.ebt, bb, AF.Exp)
            S.tt("dve", hs.qb, qn, hs.ebt, ALU.mult)
            if g.dbg is not None and h == 0 and seg == 0:
                S.dma(g.dbg[0], t1); S.dma(g.dbg[1], bb); S.dma(g.dbg[2], hs.ebt); S.dma(g.dbg[3], t3)
                S.copy("dve", t4, hs.qt); S.dma(g.dbg[4], t4)
            to_tokmajor(S, vn, hs.Vt, NBS, idb, pT, cnt)
            to_tokmajor(S, khat, hs.Kt, NBS, idb, pT, cnt)
        for n in range(NBS):
            bs = slice(n * 128, (n + 1) * 128)
            for h in range(4):
                hs = H[h]
                S.mm((hs.psc, bsc[:, h, :]), (hs.kt, hs.kt[:, bs]), (hs.qt, hs.qt[:, bs]))
                S.mm((hs.pua, bua[0:128, h, :]), (hs.Kt, hs.Kt[0:64, n, :]), (hs.Vt, hs.Vt[0:64, n, :]))
                S.mm((hs.pub, bub[0:128, h, :]), (hs.Kt, hs.Kt[64:128, n, :]), (hs.Vt, hs.Vt[64:128, n, :]))
            for h in range(4):
                hs = H[h]
                S.tt("dve", hs.Pm, (hs.psc, bsc[:, h, :]), m2, ALU.mult)
                cA = (2 * n) * 64 + 63
                S.stt(hs.Sst, hs.Sst, (hs.ebt, hs.ebt[:, cA:cA + 1]), (hs.pua, bua[:, h, :]), ALU.mult, ALU.add)
                S.copy("act", hs.Sb[1], hs.Sst)
            for h in range(4):
                hs = H[h]
                S.mm((hs.po, bo[:, h, :]), (hs.Vt, hs.Vt[:, n, :]), hs.Pm, start=True, stop=False)
                S.mm((hs.po, bo[:, h, 0:64]), hs.Sb[0], (hs.qb, hs.qb[:, n * 128:n * 128 + 64]), start=False, stop=False)
                S.mm((hs.po, bo[:, h, 64:128]), hs.Sb[1], (hs.qb, hs.qb[:, n * 128 + 64:(n + 1) * 128]), start=False, stop=True)
            for h in range(4):
                hs = H[h]
                cB = (2 * n + 1) * 64 + 63
                S.stt(hs.Sst, hs.Sst, (hs.ebt, hs.ebt[:, cB:cB + 1]), (hs.pub, bub[:, h, :]), ALU.mult, ALU.add)
                S.copy("act", hs.Sb[0], hs.Sst)
                S.copy("act", (hs.oT, hs.oT[:, bs]), (hs.po, bo[:, h, :]), disjoint=True)
        for h in range(4):
            hs = H[h]
            yb = ysb[h % 2]
            for c in range(SEG // 512):
                c5 = slice(c * 512, (c + 1) * 512)
                S.actv((sqb, sqb[:, c5]), (hs.oT, hs.oT[:, c5]), AF.Square)
                S.mm(bss, ones, (sqb, sqb[:, c5]))
                S.actv((t1, t1[:, c5]), bss, AF.Ln, scale=1.0 / 128, bias=(epsb, epsb[:, 0:1]))
            S.actv(t2, t1, AF.Exp, scale=-0.5)
            if g.dbg is not None and h == 0 and seg == 0:
                S.dma(g.dbg[5], hs.oT); S.dma(g.dbg[6], t2)
            S.stt(t3, hs.oT, (vec, vec[:, 96:97]), t2, ALU.mult, ALU.mult)
            S.actv(t4, hs.gn, AF.Silu)
            S.tt("dve", yb, t3, t4, ALU.mult)
            S.dma(g.yT[Y_A + h * 128:Y_A + (h + 1) * 128, cs], yb)
    S.end_phase()


LAYERS = 4


def build(T, L, phases=None, debug=False, ext_in=()):
    nc = bass.Bass("TRN2", target_bir_lowering=False)
    g = G()
    g.nc = nc
    g.T = T
    g.L = L

    def din(name, shape, dt=F32):
        return nc.dram_tensor(name, list(shape), dt, kind="ExternalInput").ap()

    g.xT = din("xT", [D, T])
    g.w13t = [din("w13t_a", [L, 88, 128, 2048]), din("w13t_b", [L, 88, 128, 2048])]
    g.w2t = [din("w2t_a", [L, 16, 128, FF]), din("w2t_b", [L, 16, 128, FF])]
    g.wint = din("wint", [L, 116, 128, 2048])
    g.wbt = din("wbt", [L, 16, 128, 1792])
    g.wot = din("wot", [L, 16, 128, 2048])
    g.vecs = din("vecs", [L, 128, NVEC])
    g.lbl = din("lbl", [128, 4 * LAYERS])
    g.sinks = din("sinks", [128, LAYERS * 8])
    g.relb = din("relb", [128, 640])
    g.cst = din("cst", [128, NCST])
    g.outT = nc.dram_tensor("outT", [D, T], F32, kind="ExternalOutput").ap()
    sk = "ExternalOutput" if debug else "Internal"
    def scr(name, shape, dt):
        return nc.dram_tensor(name, shape, dt, kind=("ExternalInput" if name in ext_in else sk)).ap()
    g.projT = scr("projT", [6656, T], BF16)
    g.uT = scr("uT_s", [D, T], BF16)
    g.yT = scr("yT_s", [1792, T], BF16)
    g.ebD = nc.dram_tensor("ebD", [128, 2 * 8 * 128], F32, kind=sk).ap()
    g.ebC = nc.dram_tensor("ebC", [128, 3 * 2 * 2 * 2 * 128], F32, kind=sk).ap()
    g.lbs = nc.dram_tensor("lbs", [128, 4 * LAYERS], F32, kind=sk).ap()
    g.dbg = nc.dram_tensor('dbg', [16, 128, 1024], F32, kind='ExternalOutput').ap() if debug else None
    S = Sched(nc)
    g.S = S
    allp = ["ffn1", "inproj", "A", "B", "C", "D", "merge", "ffn2"]
    if phases is None:
        phases = allp
    if any(p in phases for p in ("A", "C", "D")):
        setup_phase(S, g)
    for l in range(L):
        first = (l == 0)
        for p in phases:
            if p == "ffn1":
                ffn_phase(S, g, l, 0, g.xT if first else g.outT, g.outT)
            elif p == "inproj":
                inproj_phase(S, g, l, g.outT)
            elif p == "A":
                mixA_phase(S, g, l)
            elif p == "B":
                mixB_phase(S, g, l)
            elif p == "C":
                mixC_phase(S, g, l)
            elif p == "D":
                mixD_phase(S, g, l)
            elif p == "merge":
                merge_phase(S, g, l, g.outT)
            elif p == "ffn2":
                ffn_phase(S, g, l, 1, g.outT, g.outT)
    return nc, g


def tile_w(w, kc, nc_):
    return np.ascontiguousarray(w.reshape(kc, 128, nc_, 128).transpose(2, 1, 0, 3).reshape(nc_, 128, kc * 128))


def prep_weights(inp, L):
    out = {}
    out["w13t_a"] = np.stack([tile_w(inp["ffn1_w13"][l], 16, 88) for l in range(L)])
    out["w13t_b"] = np.stack([tile_w(inp["ffn2_w13"][l], 16, 88) for l in range(L)])
    out["w2t_a"] = np.stack([tile_w(inp["ffn1_w2"][l], 44, 16) for l in range(L)])
    out["w2t_b"] = np.stack([tile_w(inp["ffn2_w2"][l], 44, 16) for l in range(L)])
    out["wint"] = np.stack([tile_w(inp["w_in"][l], 16, 116) for l in range(L)])
    out["wbt"] = np.stack([tile_w(inp["w_branch"][l], 14, 16) for l in range(L)])
    out["wot"] = np.stack([tile_w(inp["w_out"][l], 16, 16) for l in range(L)])
    vecs = np.zeros((L, 128, NVEC), np.float32)
    for l in range(L):
        for i, nm in enumerate(("ffn1_norm", "mix_norm", "ffn2_norm")):
            for j in range(2):
                vecs[l, :, (2 * i + j) * 16:(2 * i + j + 1) * 16] = inp[nm][l, j].reshape(16, 128).T
        vecs[l, :, 96] = inp["hgrn_out_norm"][l]
    out["vecs"] = vecs
    lb = np.asarray(inp["hgrn_lb_logits"])
    out["lbl"] = np.ascontiguousarray(lb.reshape(LAYERS, 4, 128).transpose(2, 1, 0).reshape(128, 4 * LAYERS))
    out["sinks"] = np.ascontiguousarray(np.broadcast_to(np.asarray(inp["attn_sinks"]).reshape(1, -1), (128, LAYERS * 8)))
    out["relb"] = np.ascontiguousarray(np.broadcast_to(np.asarray(inp["rel_bias"]).reshape(1, 640), (128, 640)))
    out["cst"] = make_consts()
    return out


_CACHE = {}


def kernel(**inputs):
    x = np.asarray(inputs["x"])
    B, T, _ = x.shape
    L = LAYERS
    key = (T, L)
    if key not in _CACHE:
        _CACHE[key] = build(T, L, None, debug=False)
    nc, g = _CACHE[key]
    w = prep_weights({k: np.asarray(v) for k, v in inputs.items() if k != "x"}, L)
    in_maps = []
    for b in range(B):
        m = dict(w)
        m["xT"] = np.ascontiguousarray(x[b].T)
        in_maps.append(m)
    res = run_bass_kernel_spmd(nc, in_maps, core_ids=list(range(B)))
    out = np.stack([np.ascontiguousarray(res.results[b]["outT"].T) for b in range(B)], axis=0)
    return out.astype(np.float32, copy=False)
```

```python
import numpy as np
import concourse.bass as bass
import concourse.mybir as mybir
from concourse.bass_utils import run_bass_kernel_spmd
from contextlib import ExitStack

F32 = mybir.dt.float32
BF16 = mybir.dt.bfloat16
AF = mybir.ActivationFunctionType
ALU = mybir.AluOpType

import os
SERIAL_PH = [int(x) for x in os.environ.get('MK_SERIAL', '').split(',') if x]
SEM_LIMIT = 30000
NDMA_SEMS = 10


class Buf:
    __slots__ = ("name", "writers", "readers", "t", "lock")

    def __init__(self, name, t=None, lock=None):
        self.name = name
        self.writers = []
        self.readers = []
        self.t = t
        self.lock = lock

    def __getitem__(self, k):
        return self.t[k]


class Op:
    __slots__ = ("eng", "fn", "deps", "is_dma", "needs_inc", "sem", "val", "clock", "idx", "phase")


def _ba(x):
    if isinstance(x, Buf):
        return x, x.t[:]
    return x


class Sched:
    COMPUTE = ("pe", "act", "dve", "pool")
    ALL = ("pe", "act", "dve", "pool", "sp")

    def __init__(self, nc):
        self.nc = nc
        self.ops = []
        self.engs = {"pe": nc.tensor, "act": nc.scalar, "dve": nc.vector,
                     "pool": nc.gpsimd, "sp": nc.sync}
        self.phase = 0
        self.nops = 0
        self.last = {}
        self.dmas = []
        self.clocks = {e: {} for e in self.ALL}
        self.cur_sem = {}
        self.cur_cnt = {}
        self.nsw = {}
        for e in self.COMPUTE:
            self.cur_sem[e] = nc.alloc_semaphore("s_%s_0" % e)
            self.cur_cnt[e] = 0
            self.nsw[e] = 0
        self.dma_sems = {}
        self.dma_cnt = {}
        self.dma_last = {}
        self.dma_rr = {}
        self.nwaits = 0
        self.ninst = {e: 0 for e in self.ALL}
        self.es = None

    def begin_phase(self):
        self.es = ExitStack()
        self.es.__enter__()

    def sb(self, name, shape, dt):
        t = self.es.enter_context(self.nc.sbuf_tensor("%s_p%d" % (name, self.phase), list(shape), dt))
        return Buf(name, t)

    def ps(self, name, shape, dt=F32):
        t = self.es.enter_context(self.nc.psum_tensor("%s_p%d" % (name, self.phase), list(shape), dt))
        b = Buf(name, t)
        b.lock = Buf(name + "_lock")
        return b

    def end_phase(self):
        self.barrier()
        self.emit()
        self.es.__exit__(None, None, None)
        self.es = None
        self.phase += 1

    def op(self, eng, name, kw, reads=(), writes=(), dma=False, disjoint=False):
        o = Op()
        o.eng = eng
        o.fn = (name, kw)
        o.is_dma = dma
        o.needs_inc = dma
        o.sem = None
        o.val = 0
        o.clock = None
        o.idx = self.nops
        o.phase = self.phase
        self.nops += 1
        deps = {}
        locks = []
        for b in reads:
            if b.lock is not None and b.lock not in locks:
                locks.append(b.lock)
        for b in writes:
            if b.lock is not None and b.lock not in locks:
                locks.append(b.lock)
        for b in locks:
            for w in b.writers:
                deps[w.idx] = w
        for b in reads:
            for w in b.writers:
                deps[w.idx] = w
        for b in writes:
            for r in b.readers:
                deps[r.idx] = r
            if (not disjoint) or b.readers:
                for w in b.writers:
                    deps[w.idx] = w
        ph = self.phase
        SERIAL = (ph in SERIAL_PH) or (-1 in SERIAL_PH)
        if SERIAL and self.ops:
            po = self.ops[-1]
            if po.fn is not None:
                deps[po.idx] = po
        if SERIAL:
            o.deps = [d for d in deps.values() if d.phase == ph]
        elif not dma:
            raw = set()
            if eng != "pe":
                for b in reads:
                    for w in b.writers:
                        if (not w.is_dma) and w.eng == eng:
                            raw.add(w.idx)
            o.deps = [d for d in deps.values() if d.phase == ph and (d.is_dma or d.eng != eng or d.idx in raw)]
        else:
            o.deps = [d for d in deps.values() if d.phase == ph]
        for d in o.deps:
            d.needs_inc = True
        for b in reads:
            if not dma:
                b.readers = [r for r in b.readers if r.is_dma or r.eng != eng]
            b.readers.append(o)
        for b in writes:
            if b.readers:
                b.writers = [o]
                b.readers = []
            elif disjoint:
                if not dma:
                    b.writers = [w for w in b.writers if w.is_dma or w.eng != eng]
                b.writers.append(o)
            else:
                b.writers = [o]
        for b in locks:
            b.writers = [o]
        self.ops.append(o)
        if dma:
            self.dmas.append(o)
        else:
            self.last[eng] = o
        return o

    def barrier(self):
        lasts = [o for o in self.last.values() if o.phase == self.phase]
        dmas = self.dmas
        self.dmas = []
        self.last = {}
        for o in lasts:
            o.needs_inc = True
        for e in self.ALL:
            o = Op()
            o.eng = e
            o.fn = None
            o.is_dma = False
            o.needs_inc = False
            o.sem = None
            o.val = 0
            o.clock = None
            o.idx = self.nops
            o.phase = self.phase
            self.nops += 1
            o.deps = [l for l in lasts if l.eng != e] + dmas
            self.ops.append(o)

    def emit(self):
        nc = self.nc
        for o in self.ops:
            e = o.eng
            eng = self.engs[e]
            clk = self.clocks[e]
            deps = list(o.deps)
            slot = None
            if o.is_dma:
                if e not in self.dma_sems:
                    self.dma_sems[e] = [nc.alloc_semaphore("d_%s_%d" % (e, i)) for i in range(NDMA_SEMS)]
                    self.dma_cnt[e] = [0] * NDMA_SEMS
                    self.dma_last[e] = [None] * NDMA_SEMS
                    self.dma_rr[e] = 0
                slot = self.dma_rr[e] % NDMA_SEMS
                self.dma_rr[e] += 1
                if self.dma_last[e][slot] is not None:
                    deps.append(self.dma_last[e][slot])
            if len(deps) > 1:
                deps.sort(key=lambda d: -d.idx)
            for d in deps:
                k = id(d.sem)
                if clk.get(k, (None, 0))[1] >= d.val:
                    continue
                eng.wait_ge(d.sem, d.val)
                self.nwaits += 1
                for kk, vv in d.clock.items():
                    if clk.get(kk, (None, 0))[1] < vv[1]:
                        clk[kk] = vv
            if o.fn is None:
                continue
            ins = getattr(eng, o.fn[0])(**o.fn[1])
            self.ninst[e] += 1
            if o.is_dma:
                sem = self.dma_sems[e][slot]
                self.dma_cnt[e][slot] += 16
                o.sem = sem
                o.val = self.dma_cnt[e][slot]
                ins.then_inc(sem, 16)
                self.dma_last[e][slot] = o
                c = dict(clk)
                c[id(sem)] = (sem, o.val)
                o.clock = c
            elif o.needs_inc:
                if self.cur_cnt[e] >= SEM_LIMIT:
                    self.nsw[e] += 1
                    self.cur_sem[e] = nc.alloc_semaphore("s_%s_%d" % (e, self.nsw[e]))
                    self.cur_cnt[e] = 0
                self.cur_cnt[e] += 1
                o.sem = self.cur_sem[e]
                o.val = self.cur_cnt[e]
                ins.then_inc(o.sem, 1)
                c = dict(clk)
                c[id(o.sem)] = (o.sem, o.val)
                o.clock = c
            o.fn = None
            o.deps = None
        self.ops = []

    def mm(self, out, lhsT, rhs, start=True, stop=True):
        ob, oa = _ba(out)
        lb, la = _ba(lhsT)
        rb, ra = _ba(rhs)
        return self.op("pe", "matmul", dict(out=oa, lhsT=la, rhs=ra, start=start, stop=stop),
                       reads=[lb, rb], writes=[ob], disjoint=True if not start else False)

    def tr(self, out, in_, ident):
        ob, oa = _ba(out)
        ib, ia = _ba(in_)
        db, da = _ba(ident)
        return self.op("pe", "transpose", dict(out=oa, in_=ia, identity=da), reads=[ib, db], writes=[ob], disjoint=True)

    def actv(self, out, in_, func, scale=1.0, bias=None, disjoint=False, eng="act"):
        ob, oa = _ba(out)
        ib, ia = _ba(in_)
        kw = dict(out=oa, in_=ia, func=func)
        reads = [ib]
        if isinstance(scale, tuple) or isinstance(scale, Buf):
            sb_, sa = _ba(scale)
            kw["scale"] = sa
            reads.append(sb_)
        elif scale != 1.0:
            kw["scale"] = float(scale)
        if bias is not None:
            if isinstance(bias, (tuple, Buf)):
                bb, ba = _ba(bias)
                kw["bias"] = ba
                reads.append(bb)
            else:
                kw["bias"] = float(bias)
        return self.op("act", "activation", kw, reads=reads, writes=[ob], disjoint=disjoint)

    def tt(self, eng, out, in0, in1, op, disjoint=False):
        ob, oa = _ba(out)
        ab, aa = _ba(in0)
        bb, ba = _ba(in1)
        return self.op(eng, "tensor_tensor", dict(out=oa, in0=aa, in1=ba, op=op), reads=[ab, bb], writes=[ob], disjoint=disjoint)

    def ts(self, eng, out, in0, s1, s2=None, op0=ALU.mult, op1=None, disjoint=False):
        ob, oa = _ba(out)
        ab, aa = _ba(in0)
        reads = [ab]
        kw = dict(out=oa, in0=aa, op0=op0)
        if isinstance(s1, (tuple, Buf)):
            b_, a_ = _ba(s1)
            reads.append(b_)
            kw["scalar1"] = a_
        else:
            kw["scalar1"] = float(s1)
        if s2 is None:
            kw["scalar2"] = None
        elif isinstance(s2, (tuple, Buf)):
            b_, a_ = _ba(s2)
            reads.append(b_)
            kw["scalar2"] = a_
        else:
            kw["scalar2"] = float(s2)
        if op1 is not None:
            kw["op1"] = op1
        return self.op(eng, "tensor_scalar", kw, reads=reads, writes=[ob], disjoint=disjoint)

    def stt(self, out, in0, scalar, in1, op0, op1, disjoint=False):
        ob, oa = _ba(out)
        ab, aa = _ba(in0)
        bb, ba = _ba(in1)
        reads = [ab, bb]
        if isinstance(scalar, (tuple, Buf)):
            b_, a_ = _ba(scalar)
            reads.append(b_)
            sc = a_
        else:
            sc = float(scalar)
        return self.op("dve", "scalar_tensor_tensor", dict(out=oa, in0=aa, scalar=sc, in1=ba, op0=op0, op1=op1),
                       reads=reads, writes=[ob], disjoint=disjoint)

    def copy(self, eng, out, in_, disjoint=False):
        ob, oa = _ba(out)
        ib, ia = _ba(in_)
        if eng == "act":
            return self.op("act", "activation", dict(out=oa, in_=ia, func=AF.Copy), reads=[ib], writes=[ob], disjoint=disjoint)
        return self.op(eng, "tensor_copy", dict(out=oa, in_=ia), reads=[ib], writes=[ob], disjoint=disjoint)

    def recip(self, out, in_, disjoint=False):
        ob, oa = _ba(out)
        ib, ia = _ba(in_)
        return self.op("dve", "reciprocal", dict(out=oa, in_=ia), reads=[ib], writes=[ob], disjoint=disjoint)

    def memset(self, eng, out, val, disjoint=False):
        ob, oa = _ba(out)
        return self.op(eng, "memset", dict(ap=oa, constant=float(val)), writes=[ob], disjoint=disjoint)

    def dma(self, out, in_, eng="sp", disjoint=True):
        reads, writes = [], []
        if isinstance(out, (tuple, Buf)):
            ob, oa = _ba(out)
            writes.append(ob)
        else:
            oa = out
        if isinstance(in_, (tuple, Buf)):
            ib, ia = _ba(in_)
            reads.append(ib)
        else:
            ia = in_
        return self.op(eng, "dma_start", dict(out=oa, in_=ia), reads=reads, writes=writes, dma=True, disjoint=disjoint)


D = 2048
KC = 16
FF = 5632
FC = 44
TT = 512
EPS = 1e-6
MIXC = 52
NVEC = 104

R_AQ, R_AF, R_AI, R_AG = 0, 512, 1024, 1536
R_BQ, R_BK, R_BV = 2048, 2560, 3072
R_CQ, R_CK, R_CV = 3584, 4352, 5120
R_DQ, R_DK, R_DV = 5888, 6400, 6528
Y_A, Y_B, Y_C, Y_D = 0, 512, 1024, 1280


class G:
    pass


def load_vec(S, g, l):
    vec = S.sb("vec", [128, NVEC + 48], F32)
    S.dma((vec, vec[:, 0:NVEC]), g.vecs[l])
    S.ts("dve", (vec, vec[:, NVEC:NVEC + 16]), (vec, vec[:, 16:32]), 0.5)
    S.ts("dve", (vec, vec[:, NVEC + 16:NVEC + 32]), (vec, vec[:, 80:96]), 0.5)
    return vec


def phase_consts(S, g):
    ones = S.sb("ones", [128, 128], BF16)
    S.memset("dve", ones, 1.0)
    epsb = S.sb("epsb", [128, 1], F32)
    S.memset("dve", epsb, EPS)
    return ones, epsb


def rms_rstd(S, ss_ps, rstd, tmp, epsb, n):
    S.actv(tmp, ss_ps, AF.Sqrt, scale=1.0 / n, bias=(epsb, epsb[:, 0:1]))
    S.recip(rstd, tmp)


def prenorm_tile(S, g, hsrc, tok, bufX, xn, sq, tmp, rstd, ones, epsb, ssb, vec, gcol):
    srcv = hsrc.rearrange("(kc p) t -> p kc t", p=128)
    S.dma(bufX, srcv[:, :, tok], disjoint=False)
    for kc in range(KC):
        q = sq[kc % 2]
        S.actv(q, (bufX, bufX[:, kc, :]), AF.Square)
        S.mm(ssb, ones, q, start=(kc == 0), stop=(kc == KC - 1))
    rms_rstd(S, ssb, rstd, tmp, epsb, D)
    for kc in range(KC):
        S.stt((xn, xn[:, kc, :]), (bufX, bufX[:, kc, :]), (vec, vec[:, gcol + kc:gcol + kc + 1]), rstd,
              ALU.mult, ALU.mult, disjoint=True)


def postnorm_residual(S, g, hsrc, hdst, tok, bufX, rstd, tmp, epsb, ssb, vec, gcol, hre, hout):
    rms_rstd(S, ssb, rstd, tmp, epsb, D)
    for dc in range(KC):
        hr = hre[dc % 2]
        ho = hout[dc % 2]
        S.dma(hr, hsrc[dc * 128:(dc + 1) * 128, tok], disjoint=False)
        S.stt(ho, (bufX, bufX[:, dc, :]), (vec, vec[:, gcol + dc:gcol + dc + 1]), rstd, ALU.mult, ALU.mult)
        S.tt("dve", ho, ho, hr, ALU.add)
        S.dma(hdst[dc * 128:(dc + 1) * 128, tok], ho)


def prenorm_compute(S, bufX, xn, sq, tmp, rstd, ones, epsb, ssb, vec, gcol):
    for kc in range(KC):
        q = sq[kc % 2]
        S.actv(q, (bufX, bufX[:, kc, :]), AF.Square)
        S.mm(ssb, ones, q, start=(kc == 0), stop=(kc == KC - 1))
    rms_rstd(S, ssb, rstd, tmp, epsb, D)
    for kc in range(KC):
        S.stt((xn, xn[:, kc, :]), (bufX, bufX[:, kc, :]), (vec, vec[:, gcol + kc:gcol + kc + 1]), rstd,
              ALU.mult, ALU.mult, disjoint=True)


class ResidualPipe:
    def __init__(self, S, hsrc, hdst, vec, gcol, hre, hout):
        self.S, self.hsrc, self.hdst, self.vec, self.gcol, self.hre, self.hout = S, hsrc, hdst, vec, gcol, hre, hout
        self.pending = None

    def start(self, tok, bufX, rstd):
        assert self.pending is None
        self.pending = [tok, bufX, rstd, 0, 0]

    def step(self):
        if self.pending is None:
            return False
        S = self.S
        tok, bufX, rstd, nl, ncp = self.pending
        while nl < KC and nl < ncp + 2:
            hr = self.hre[nl % 2]
            S.dma(hr, self.hsrc[nl * 128:(nl + 1) * 128, tok], disjoint=False)
            nl += 1
        if nl > ncp:
            dc = ncp
            hr = self.hre[dc % 2]
            ho = self.hout[dc % 2]
            S.stt(ho, (bufX, bufX[:, dc, :]), (self.vec, self.vec[:, self.gcol + dc:self.gcol + dc + 1]), rstd,
                  ALU.mult, ALU.mult)
            S.tt("dve", ho, ho, hr, ALU.add)
            S.dma(self.hdst[dc * 128:(dc + 1) * 128, tok], ho)
            ncp += 1
        self.pending[3], self.pending[4] = nl, ncp
        if ncp >= KC:
            self.pending = None
        return True

    def flush(self):
        while self.step():
            pass


def ffn_phase(S, g, l, which, hsrc, hdst):
    NT = g.T // TT
    w13t = g.w13t[which][l]
    w2t = g.w2t[which][l]
    S.begin_phase()
    ones, epsb = phase_consts(S, g)
    vec = load_vec(S, g, l)
    gpre = 0 if which == 0 else 64
    gpost = NVEC if which == 0 else NVEC + 16
    bufX = [S.sb("bufX%d" % i, [128, KC, TT], F32) for i in range(2)]
    xn = [S.sb("xn%d" % i, [128, KC, TT], BF16) for i in range(2)]
    act = S.sb("actT", [128, FC, TT], BF16)
    w13b = [S.sb("w13b%d" % i, [128, KC, 128], BF16) for i in range(4)]
    w2b = [S.sb("w2b%d" % i, [128, FC, 128], BF16) for i in range(2)]
    sq = [S.sb("sq%d" % i, [128, TT], BF16) for i in range(2)]
    sq2 = [S.sb("sqb%d" % i, [128, TT], BF16) for i in range(2)]
    tmp = [S.sb("tmp%d" % i, [128, TT], F32) for i in range(2)]
    tpre = S.sb("tpre", [128, TT], F32)
    tpost = S.sb("tpost", [128, TT], F32)
    rpre = S.sb("rpre", [128, TT], F32)
    rpost = S.sb("rpost", [128, TT], F32)
    hre = [S.sb("hre%d" % i, [128, TT], F32) for i in range(2)]
    hout = [S.sb("hout%d" % i, [128, TT], F32) for i in range(2)]
    psb = [S.ps("psb%d" % i, [128, 512], F32) for i in range(8)]
    ss_pre = psb[6]
    ss_post = psb[7]
    srcv = hsrc.rearrange("(kc p) t -> p kc t", p=128)
    res = ResidualPipe(S, hsrc, hdst, vec, gpost, hre, hout)

    def load_prenorm(tt):
        tok = slice(tt * TT, (tt + 1) * TT)
        X = bufX[tt % 2]
        S.dma(X, srcv[:, :, tok], disjoint=False)
        prenorm_compute(S, X, xn[tt % 2], sq, tpre, rpre, ones, epsb, ss_pre, vec, gpre)

    n13 = 0
    n2 = 0
    load_prenorm(0)
    for tt in range(NT):
        tok = slice(tt * TT, (tt + 1) * TT)
        X = bufX[tt % 2]
        XN = xn[tt % 2]
        for j in range(FC):
            wb = []
            for half in range(2):
                wbf = w13b[n13 % 4]
                n13 += 1
                S.dma((wbf, wbf[:].rearrange("p a b -> p (a b)")), w13t[half * FC + j], eng="pool", disjoint=False)
                wb.append(wbf)
            pg = psb[(j % 2) * 2]
            pu = psb[(j % 2) * 2 + 1]
            for half, pp in ((0, pg), (1, pu)):
                for kc in range(KC):
                    S.mm(pp, (wb[half], wb[half][:, kc, :]), (XN, XN[:, kc, :]), start=(kc == 0), stop=(kc == KC - 1))
            tm = tmp[j % 2]
            S.actv(tm, pg, AF.Silu)
            S.tt("dve", (act, act[:, j, :]), tm, pu, ALU.mult, disjoint=True)
            if j >= 2:
                res.step()
        res.flush()
        if tt + 1 < NT:
            load_prenorm(tt + 1)
        prev_sq = None
        for dc in range(KC):
            wbf = w2b[n2 % 2]
            n2 += 1
            for q in range(4):
                S.dma((wbf, wbf[:, q * 11:(q + 1) * 11, :].rearrange("p a b -> p (a b)")),
                      w2t[dc][:, q * 1408:(q + 1) * 1408], eng="pool")
            po = psb[4 + (dc % 2)]
            for fc in range(FC):
                S.mm(po, (wbf, wbf[:, fc, :]), (act, act[:, fc, :]), start=(fc == 0), stop=(fc == FC - 1))
            if prev_sq is not None:
                S.mm(ss_post, ones, prev_sq, start=(dc == 1), stop=False)
            S.actv((X, X[:, dc, :]), po, AF.Copy, disjoint=True)
            q_ = sq2[dc % 2]
            S.actv(q_, po, AF.Square)
            prev_sq = q_
        S.mm(ss_post, ones, prev_sq, start=False, stop=True)
        rms_rstd(S, ss_post, rpost, tpost, epsb, D)
        res.start(tok, X, rpost)
    res.flush()
    S.end_phase()


def inproj_phase(S, g, l, h):
    NT = g.T // TT
    S.begin_phase()
    ones, epsb = phase_consts(S, g)
    vec = load_vec(S, g, l)
    bufX = [S.sb("bufX%d" % i, [128, KC, TT], F32) for i in range(2)]
    xn = [S.sb("xn%d" % i, [128, KC, TT], BF16) for i in range(2)]
    wb = [S.sb("wb%d" % i, [128, KC, 128], BF16) for i in range(6)]
    sq = [S.sb("sq%d" % i, [128, TT], BF16) for i in range(2)]
    tmp = S.sb("tmp", [128, TT], F32)
    rstd = S.sb("rstd", [128, TT], F32)
    ost = [S.sb("ost%d" % i, [128, TT], BF16) for i in range(4)]
    psb = [S.ps("psb%d" % i, [128, 512], F32) for i in range(5)]
    ssb = psb[4]
    srcv = h.rearrange("(kc p) t -> p kc t", p=128)
    uv = g.uT.rearrange("(kc p) t -> p kc t", p=128)

    def load_prenorm(tt):
        tok = slice(tt * TT, (tt + 1) * TT)
        X = bufX[tt % 2]
        S.dma(X, srcv[:, :, tok], disjoint=False)
        prenorm_compute(S, X, xn[tt % 2], sq, tmp, rstd, ones, epsb, ssb, vec, 32)
        S.dma(uv[:, :, tok], xn[tt % 2])

    nw = 0
    load_prenorm(0)
    for tt in range(NT):
        tok = slice(tt * TT, (tt + 1) * TT)
        XN = xn[tt % 2]
        for c in range(MIXC):
            if c == MIXC - 8 and tt + 1 < NT:
                load_prenorm(tt + 1)
            w = wb[nw % 6]
            S.dma((w, w[:].rearrange("p a b -> p (a b)")), g.wint[l][c], eng="pool", disjoint=False)
            pp = psb[nw % 4]
            o = ost[nw % 4]
            for kc in range(KC):
                S.mm(pp, (w, w[:, kc, :]), (XN, XN[:, kc, :]), start=(kc == 0), stop=(kc == KC - 1))
            S.copy("act" if nw % 2 == 0 else "dve", o, pp)
            S.dma(g.projT[c * 128:(c + 1) * 128, tok], o)
            nw += 1
    S.end_phase()


BR_K = (4, 4, 2, 4)


def merge_phase(S, g, l, h):
    NT = g.T // TT
    S.begin_phase()
    ones, epsb = phase_consts(S, g)
    vec = load_vec(S, g, l)
    bufX = [S.sb("bufX%d" % i, [128, KC, TT], F32) for i in range(2)]
    uT = [S.sb("uT%d" % i, [128, KC, TT], BF16) for i in range(2)]
    yT = [S.sb("yT%d" % i, [128, 14, TT], BF16) for i in range(2)]
    mg = S.sb("mg", [128, KC, TT], BF16)
    wg = [S.sb("wg%d" % i, [128, KC, 128], BF16) for i in range(6)]
    wbr = [S.sb("wbr%d" % i, [128, 14, 128], BF16) for i in range(2)]
    wo = [S.sb("wo%d" % i, [128, KC, 128], BF16) for i in range(2)]
    sg = [S.sb("sg%d" % i, [128, TT], F32) for i in range(2)]
    acc = S.sb("acc", [128, TT], F32)
    t2 = S.sb("t2", [128, TT], F32)
    sq = [S.sb("sq%d" % i, [128, TT], BF16) for i in range(2)]
    tmp = S.sb("tmp", [128, TT], F32)
    rstd = S.sb("rstd", [128, TT], F32)
    hre = [S.sb("hre%d" % i, [128, TT], F32) for i in range(2)]
    hout = [S.sb("hout%d" % i, [128, TT], F32) for i in range(2)]
    psb = [S.ps("psb%d" % i, [128, 512], F32) for i in range(7)]
    ssb = psb[6]
    res = ResidualPipe(S, h, h, vec, 48, hre, hout)
    uv = g.uT.rearrange("(kc p) t -> p kc t", p=128)
    yv = g.yT.rearrange("(rc p) t -> p rc t", p=128)

    def load_in(tt):
        tok = slice(tt * TT, (tt + 1) * TT)
        S.dma(uT[tt % 2], uv[:, :, tok], disjoint=False)
        S.dma(yT[tt % 2], yv[:, :, tok], disjoint=False)

    ng = 0
    nb = 0
    no = 0
    load_in(0)
    for tt in range(NT):
        tok = slice(tt * TT, (tt + 1) * TT)
        U = uT[tt % 2]
        Y = yT[tt % 2]
        X = bufX[tt % 2]
        for dc in range(KC):
            wbt = wbr[nb % 2]
            nb += 1
            S.dma((wbt, wbt[:].rearrange("p a b -> p (a b)")), g.wbt[l][dc], eng="pool", disjoint=False)
            rc0 = 0
            for i in range(4):
                w = wg[ng % 6]
                S.dma((w, w[:].rearrange("p a b -> p (a b)")), g.wint[l][MIXC + i * 16 + dc], eng="pool", disjoint=False)
                pgt = psb[(ng % 2) * 2]
                ptm = psb[(ng % 2) * 2 + 1]
                s_ = sg[ng % 2]
                ng += 1
                for kc in range(KC):
                    S.mm(pgt, (w, w[:, kc, :]), (U, U[:, kc, :]), start=(kc == 0), stop=(kc == KC - 1))
                nk = BR_K[i]
                for r in range(nk):
                    S.mm(ptm, (wbt, wbt[:, rc0 + r, :]), (Y, Y[:, rc0 + r, :]), start=(r == 0), stop=(r == nk - 1))
                rc0 += nk
                S.actv(s_, pgt, AF.Sigmoid)
                if i == 0:
                    S.tt("dve", acc, s_, ptm, ALU.mult)
                elif i < 3:
                    S.tt("dve", t2, s_, ptm, ALU.mult)
                    S.tt("dve", acc, acc, t2, ALU.add)
                else:
                    S.tt("dve", t2, s_, ptm, ALU.mult)
                    S.tt("dve", (mg, mg[:, dc, :]), acc, t2, ALU.add, disjoint=True)
            res.step()
        res.flush()
        if tt + 1 < NT:
            load_in(tt + 1)
        prev_sq = None
        for dc in range(KC):
            w = wo[no % 2]
            no += 1
            S.dma((w, w[:].rearrange("p a b -> p (a b)")), g.wot[l][dc], eng="pool", disjoint=False)
            po = psb[4 + (dc % 2)]
            for kc in range(KC):
                S.mm(po, (w, w[:, kc, :]), (mg, mg[:, kc, :]), start=(kc == 0), stop=(kc == KC - 1))
            if prev_sq is not None:
                S.mm(ssb, ones, prev_sq, start=(dc == 1), stop=False)
            S.actv((X, X[:, dc, :]), po, AF.Copy, disjoint=True)
            q_ = sq[dc % 2]
            S.actv(q_, po, AF.Square)
            prev_sq = q_
        S.mm(ssb, ones, prev_sq, start=False, stop=True)
        rms_rstd(S, ssb, rstd, tmp, epsb, D)
        res.start(tok, X, rstd)
    res.flush()
    S.end_phase()


import math, os

C_PATTERNS = ((128, 1), (512, 4), (2048, 16))
CST_BI = 0
CST_ID = 1024
CST_NTI = 1152
CST_NSL = 1280
CST_MB = 1408
CST_M2 = 3456
NCST = 3584
NEG = -30000.0
LAYERS_A = 4


def _rel_bucket(dist):
    dist = np.asarray(dist)
    d = np.maximum(dist, 1).astype(np.float32)
    large = 16 + (np.log(d / np.float32(16)) / np.float32(math.log(2048 / 16)) * np.float32(16)).astype(np.int32)
    large = np.minimum(large, 31)
    return np.where(dist < 16, dist, large)


def bi_tile(kind, pc):
    k = np.arange(128)[:, None]
    q = np.arange(128)[None, :]
    du = q - k + (128 if pc == 0 else 0)
    if kind == 'D':
        valid = (du >= 0) & (du < 128)
        r = 1
    else:
        valid = (du >= 0) & (du <= 128)
        r = C_PATTERNS[kind][1]
    b = _rel_bucket(np.maximum(du, 0) * r)
    return np.where(valid, b, -1).astype(np.float32)


BI_KINDS = [('D', 0), ('D', 1), (0, 0), (0, 1), (1, 0), (1, 1), (2, 0), (2, 1)]


def make_consts():
    c = np.zeros((128, NCST), np.float32)
    for i, (kind, pc) in enumerate(BI_KINDS):
        c[:, CST_BI + i * 128:CST_BI + (i + 1) * 128] = bi_tile(kind, pc)
    c[:, CST_ID:CST_ID + 128] = np.eye(128, dtype=np.float32)
    j = np.arange(128)[:, None]
    s = np.arange(128)[None, :]
    c[:, CST_NTI:CST_NTI + 128] = np.where(j >= s, -1.0, 0.0)
    c[:, CST_NSL:CST_NSL + 128] = np.where(j < s, -1.0, 0.0)
    col = np.arange(512)[None, :]
    for m in range(4):
        c[:, CST_MB + m * 512:CST_MB + (m + 1) * 512] = np.where(128 * m + j < col, 1.0, 0.0)
    c[:, CST_M2:CST_M2 + 128] = np.where((j // 64 == s // 64) & (j <= s), 1.0, 0.0)
    return c


def setup_phase(S, g):
    S.begin_phase()
    bi = S.sb("bi", [128, 8, 128], F32)
    S.dma(bi, g.cst[:, CST_BI:CST_BI + 1024].rearrange("p (a b) -> p a b", b=128), disjoint=False)
    relb = S.sb("relb", [128, 640], F32)
    S.dma(relb, g.relb, disjoint=False)
    ebD = S.sb("ebD", [128, 2, 8, 128], F32)
    ebC = S.sb("ebC", [128, 3, 2, 2, 2, 128], F32)
    tmps = [S.sb("tb%d" % i, [128, 128], F32) for i in range(4)]
    n = 0
    for ti, (kind, pc) in enumerate(BI_KINDS):
        tile_np = bi_tile(kind, pc)
        buckets = sorted(set(int(v) for v in np.unique(tile_np) if v >= 0))
        nh = 8 if kind == 'D' else 4
        for h in range(nh):
            if kind == 'D':
                dst = (ebD, ebD[:, pc, h, :])
                col = 12 + h
                eng = "dve"
            else:
                dst = (ebC, ebC[:, kind, h // 2, h % 2, pc, :])
                col = kind * 4 + h
                eng = "dve"
            src = (bi, bi[:, ti, :])
            S.ts(eng, dst, src, 0.0, NEG, op0=ALU.is_lt, op1=ALU.mult, disjoint=True)
            for b in buckets:
                tm = tmps[(n % 2) + (0 if eng == "dve" else 2)]
                n += 1
                S.ts(eng, tm, src, float(b), (relb, relb[:, b * 20 + col:b * 20 + col + 1]), op0=ALU.is_equal, op1=ALU.mult)
                S.tt(eng, dst, dst, tm, ALU.add, disjoint=True)
    S.dma(g.ebD, (ebD, ebD[:].rearrange("p a b c -> p (a b c)")))
    S.dma(g.ebC, (ebC, ebC[:].rearrange("p a b c d e -> p (a b c d e)")))
    S.end_phase()


def load_ident(S, g):
    idf = S.sb("idf", [128, 128], F32)
    S.dma(idf, g.cst[:, CST_ID:CST_ID + 128], disjoint=False)
    idb = S.sb("idb", [128, 128], BF16)
    S.copy("dve", idb, idf)
    return idb


def to_tokmajor(S, src, dst, NB, idb, pT, cnt, engs=("act", "dve")):
    for n in range(NB):
        p = pT[cnt[0] % len(pT)]
        S.tr(p, (src, src[:, n * 128:(n + 1) * 128]), idb)
        S.copy(engs[cnt[0] % len(engs)], (dst, dst[:, n, :]), p, disjoint=True)
        cnt[0] += 1


def mixD_phase(S, g, l):
    T = g.T
    NB = T // 128
    S.begin_phase()
    idb = load_ident(S, g)
    ones64 = S.sb("ones64", [128, 64], BF16)
    S.memset("dve", ones64, 1.0)
    qD = S.sb("qD", [64, 8, T], BF16)
    kD = S.sb("kD", [64, 2, T], BF16)
    vT = S.sb("vT", [128, T], BF16)
    Vtok = S.sb("Vtok", [128, NB, 128], BF16)
    yD = S.sb("yD", [64, 8, T], BF16)
    eb = S.sb("eb", [128, 2, 8, 128], F32)
    S.dma((eb, eb[:].rearrange("p a b c -> p (a b c)")), g.ebD, disjoint=False)
    sk = S.sb("sk", [64, 8], F32)
    S.dma(sk, g.sinks[0:64, l * 8:(l + 1) * 8], disjoint=False)
    es = S.sb("es", [64, 8], F32)
    S.actv(es, sk, AF.Exp)
    esb = S.sb("esb", [64, 8, 128], F32)
    S.copy("dve", esb, (es, es[:, :].unsqueeze(2).to_broadcast([64, 8, 128])))
    for h in range(8):
        S.dma((qD, qD[:, h, :]), g.projT[R_DQ + h * 64:R_DQ + (h + 1) * 64, :])
    for kv in range(2):
        S.dma((kD, kD[:, kv, :]), g.projT[R_DK + kv * 64:R_DK + (kv + 1) * 64, :])
    S.dma(vT, g.projT[R_DV:R_DV + 128, :], disjoint=False)
    pT = [S.ps("pT%d" % i, [128, 128], BF16) for i in range(2)]
    pS = [S.ps("pS%d" % i, [128, 2, 512], F32) for i in range(2)]
    pO = S.ps("pO", [128, 512], F32)
    pD = S.ps("pD", [128, 512], F32)
    Zs = [S.sb("Zs%d" % i, [128, 2, 512], F32) for i in range(2)]
    Pb = [S.sb("Pb%d" % i, [128, 2, 512], BF16) for i in range(2)]
    dt = S.sb("dt", [64, 512], F32)
    cnt = [0]
    to_tokmajor(S, vT, Vtok, NB, idb, pT, cnt)
    blocks = [(n, gk) for n in range(NB) for gk in range(2)]

    def emit_s(i):
        n, gk = blocks[i]
        Sp = pS[i % 2]
        rq = (qD, qD[:, 4 * gk:4 * gk + 4, n * 128:(n + 1) * 128])
        if n > 0:
            S.mm((Sp, Sp[:, 0, :]), (kD, kD[:, gk, (n - 1) * 128:n * 128]), rq)
        S.mm((Sp, Sp[:, 1, :]), (kD, kD[:, gk, n * 128:(n + 1) * 128]), rq, start=True)

    def emit_z(i):
        n, gk = blocks[i]
        Sp = pS[i % 2]
        Z = Zs[i % 2]
        P = Pb[i % 2]
        lo = 0 if n > 0 else 1
        S.stt((Z, Z[:, lo:2, :].rearrange("p a (h q) -> p a h q", h=4)),
              (Sp, Sp[:, lo:2, :].rearrange("p a (h q) -> p a h q", h=4)), 0.125,
              (eb, eb[:, lo:2, 4 * gk:4 * gk + 4, :]), ALU.mult, ALU.add)
        S.actv((P, P[:, lo:2, :]), (Z, Z[:, lo:2, :]), AF.Exp)

    def emit_rest(i):
        n, gk = blocks[i]
        P = Pb[i % 2]
        if i + 1 < len(blocks):
            emit_s(i + 1)
        for (pp, lhs_of) in ((pO, None), (pD, ones64)):
            if n > 0:
                lh = (Vtok, Vtok[:, n - 1, gk * 64:(gk + 1) * 64]) if lhs_of is None else ones64
                S.mm((pp, pp[0:64, :]), lh, (P, P[:, 0, :]), start=True, stop=False)
            lh = (Vtok, Vtok[:, n, gk * 64:(gk + 1) * 64]) if lhs_of is None else ones64
            S.mm((pp, pp[0:64, :]), lh, (P, P[:, 1, :]), start=(n == 0), stop=True)
        if i + 1 < len(blocks):
            emit_z(i + 1)
        d_ = dts[i % 2]
        S.tt("dve", (d_, d_[:, :].rearrange("p (h q) -> p h q", h=4)),
             (pD, pD[0:64, :].rearrange("p (h q) -> p h q", h=4)), (esb, esb[:, 4 * gk:4 * gk + 4, :]), ALU.add)
        S.actv(d_, d_, AF.Ln)
        S.actv(d_, d_, AF.Exp, scale=-1.0)
        S.tt("dve", (yD, yD[:, 4 * gk:4 * gk + 4, n * 128:(n + 1) * 128]),
             (pO, pO[0:64, :].rearrange("p (h q) -> p h q", h=4)),
             (d_, d_[:, :].rearrange("p (h q) -> p h q", h=4)), ALU.mult, disjoint=True)

    dts = [dt, S.sb("dt2", [64, 512], F32)]
    emit_s(0)
    emit_z(0)
    for i in range(len(blocks)):
        emit_rest(i)
    for h in range(8):
        S.dma(g.yT[Y_D + h * 64:Y_D + (h + 1) * 64, :], (yD, yD[:, h, :]))
    S.end_phase()


def mixC_phase(S, g, l):
    T = g.T
    NB = T // 128
    S.begin_phase()
    idb = load_ident(S, g)
    ones64 = S.sb("ones64", [128, 64], BF16)
    S.memset("dve", ones64, 1.0)
    eb = S.sb("eb", [128, 3, 2, 2, 2, 128], F32)
    S.dma((eb, eb[:].rearrange("p a b c d e -> p (a b c d e)")), g.ebC, disjoint=False)
    natq = [S.sb("natq%d" % i, [64, 2, T], BF16) for i in range(2)]
    perq = [S.sb("perq%d" % i, [64, 2, T], BF16) for i in range(2)]
    natv = S.sb("natv", [128, T], BF16)
    perv = S.sb("perv", [128, T], BF16)
    Vtok = S.sb("Vtok", [128, NB, 128], BF16)
    accN = S.sb("accN", [64, 2, T], F32)
    accD = S.sb("accD", [64, 2, T], F32)
    ybf = S.sb("ybf", [64, 2, T], BF16)
    pT = [S.ps("pT%d" % i, [128, 128], BF16) for i in range(2)]
    pS = [S.ps("pS%d" % i, [128, 2, 2, 128], F32) for i in range(2)]
    pO = [S.ps("pO%d" % i, [128, 512], F32) for i in range(2)]
    pD = [S.ps("pD%d" % i, [128, 512], F32) for i in range(2)]
    Zs = [S.sb("Zs%d" % i, [128, 2, 2, 128], F32) for i in range(2)]
    Pb = [S.sb("Pb%d" % i, [128, 2, 2, 128], BF16) for i in range(2)]
    cnt = [0]
    it = 0
    rows = (R_CQ, R_CK, R_CV)
    for hp in range(2):
        for gi, (win, r) in enumerate(C_PATTERNS):
            cur = []
            for j in range(2):
                r0 = rows[j] + gi * 256 + hp * 128
                for hh in range(2):
                    S.dma((natq[j], natq[j][:, hh, :]), g.projT[r0 + hh * 64:r0 + (hh + 1) * 64, :], disjoint=(hh == 1))
                if r > 1:
                    S.copy("act" if j == 0 else "dve", (perq[j], perq[j][:, :, :].rearrange("p h (c i) -> p h c i", c=r)),
                           (natq[j], natq[j][:, :, :].rearrange("p h (i c) -> p h c i", c=r)))
                    cur.append(perq[j])
                else:
                    cur.append(natq[j])
            r0 = rows[2] + gi * 256 + hp * 128
            S.dma(natv, g.projT[r0:r0 + 128, :], disjoint=False)
            if r > 1:
                S.copy("act", (perv, perv[:, :].rearrange("p (c i) -> p c i", c=r)),
                       (natv, natv[:, :].rearrange("p (i c) -> p c i", c=r)))
                vp = perv
            else:
                vp = natv
            qp, kp = cur
            to_tokmajor(S, vp, Vtok, NB, idb, pT, cnt)
            Lb = NB // r
            cblocks = [(c, n) for c in range(r) for n in range(Lb)]

            def emit_s(i, it):
                c, n = cblocks[i]
                pb = c * Lb + n
                Sp = pS[it % 2]
                for hh in range(2):
                    rq = (qp, qp[:, hh, pb * 128:(pb + 1) * 128])
                    if n > 0:
                        S.mm((Sp, Sp[:, hh, 0, :]), (kp, kp[:, hh, (pb - 1) * 128:pb * 128]), rq)
                    S.mm((Sp, Sp[:, hh, 1, :]), (kp, kp[:, hh, pb * 128:(pb + 1) * 128]), rq)

            def emit_z(i, it):
                c, n = cblocks[i]
                Sp = pS[it % 2]
                Z = Zs[it % 2]
                P = Pb[it % 2]
                if n > 0:
                    S.stt(Z, Sp, 0.125, (eb, eb[:, gi, hp, :, :, :]), ALU.mult, ALU.add)
                    S.actv(P, Z, AF.Exp)
                else:
                    S.stt((Z, Z[:, :, 1, :]), (Sp, Sp[:, :, 1, :]), 0.125, (eb, eb[:, gi, hp, :, 1, :]), ALU.mult, ALU.add)
                    S.actv((P, P[:, :, 1, :]), (Z, Z[:, :, 1, :]), AF.Exp)

            def emit_rest(i, it):
                c, n = cblocks[i]
                pb = c * Lb + n
                P = Pb[it % 2]
                po = pO[it % 2]
                pd = pD[it % 2]
                if i + 1 < len(cblocks):
                    emit_s(i + 1, it + 1)
                for hh in range(2):
                    vs = slice(hh * 64, (hh + 1) * 64)
                    cs = slice(hh * 128, (hh + 1) * 128)
                    for (pp, isden) in ((po, False), (pd, True)):
                        if n > 0:
                            lh = ones64 if isden else (Vtok, Vtok[:, pb - 1, vs])
                            S.mm((pp, pp[0:64, cs]), lh, (P, P[:, hh, 0, :]), start=True, stop=False)
                        lh = ones64 if isden else (Vtok, Vtok[:, pb, vs])
                        S.mm((pp, pp[0:64, cs]), lh, (P, P[:, hh, 1, :]), start=(n == 0), stop=True)
                if i + 1 < len(cblocks):
                    emit_z(i + 1, it + 1)
                t0 = c + r * 128 * n
                sl = slice(t0, t0 + r * 127 + 1, r) if r > 1 else slice(t0, t0 + 128)
                for (acc, pp, eng) in ((accN, po, "dve"), (accD, pd, "act")):
                    av = (acc, acc[:, :, sl])
                    pv = (pp, pp[0:64, 0:256].rearrange("p (h q) -> p h q", h=2))
                    if gi == 0:
                        S.copy(eng, av, pv, disjoint=True)
                    else:
                        S.tt("dve", av, av, pv, ALU.add, disjoint=True)

            emit_s(0, it)
            emit_z(0, it)
            for i in range(len(cblocks)):
                emit_rest(i, it)
                it += 1
        S.actv(accD, accD, AF.Ln)
        S.actv(accD, accD, AF.Exp, scale=-1.0)
        S.tt("dve", ybf, accN, accD, ALU.mult)
        for hh in range(2):
            r0 = Y_C + (2 * hp + hh) * 64
            S.dma(g.yT[r0:r0 + 64, :], (ybf, ybf[:, hh, :]))
    S.end_phase()


def load_cst_bf(S, g, name, c0, n):
    f = S.sb(name + "f", [128, n], F32)
    S.dma(f, g.cst[:, c0:c0 + n], disjoint=False)
    b = S.sb(name + "b", [128, n], BF16)
    S.copy("dve", b, f)
    return f, b


def mixB_phase(S, g, l):
    T = g.T
    NB = T // 128
    NQ = T // 512
    S.begin_phase()
    idb = load_ident(S, g)
    _, nti = load_cst_bf(S, g, "nti", CST_NTI, 128)
    _, nsl = load_cst_bf(S, g, "nsl", CST_NSL, 128)
    maskB = S.sb("maskB", [128, 4, 512], F32)
    S.dma((maskB, maskB[:].rearrange("p a b -> p (a b)")), g.cst[:, CST_MB:CST_MB + 2048], disjoint=False)
    oneb = S.sb("oneb", [128, 1], F32)
    S.memset("dve", oneb, 1.0)
    qh = S.sb("qh", [64, 2, T], BF16)
    kh = S.sb("kh", [64, 2, T], BF16)
    vT = S.sb("vT", [128, T], BF16)
    Vtok = S.sb("Vtok", [128, NB, 128], BF16)
    ybf = S.sb("ybf", [64, 2, T], BF16)
    NPS = int(os.environ.get("MK_BNPS", "2"))
    NST = int(os.environ.get("MK_BNST", "3" if NPS == 1 else "2"))
    pSs = [S.ps("pS%d" % i, [128, 512], F32) for i in range(NPS)]
    pT = [S.ps("pT%d" % i, [128, 128], BF16) for i in range(2 if (NPS <= 2 and NST <= 2) else 1)]
    sets = []
    pscnt = [0]
    NDUM = int(os.environ.get("MK_BDUM", "0"))
    if NDUM:
        pdum = S.ps("pdum", [128, 512], F32)
        dsrc = S.sb("dsrc", [128, 512], BF16)
        S.memset("dve", dsrc, 0.5)

    def dummies():
        for _ in range(NDUM):
            S.mm(pdum, nti, dsrc)
    for k in range(NST):
        st = G()
        st.pB = S.ps("pB%d" % k, [128, 512], F32)
        st.pO = S.ps("pO%d" % k, [128, 512], F32)
        st.e = [S.sb("e%d_%d" % (k, i), [128, 512], F32) for i in range(2)]
        st.sp = [S.sb("sp%d_%d" % (k, i), [128, 512], BF16) for i in range(2)]
        st.w = [S.sb("w%d_%d" % (k, i), [128, 512], F32) for i in range(2)]
        st.a = [S.sb("a%d_%d" % (k, i), [128, 512], BF16) for i in range(2)]
        sets.append(st)
    cnt = [0]

    def chain(st, hh, qt):
        qs = (qh, qh[:, hh, qt * 512:(qt + 1) * 512])
        kbs = list(range(4 * qt + 3, -1, -1))
        nk = len(kbs)

        def s_and_exp(i):
            kb = kbs[i]
            m = kb - 4 * qt
            pS = pSs[pscnt[0] % len(pSs)]
            pscnt[0] += 1
            e = st.e[i % 2]
            S.mm(pS, (kh, kh[:, hh, kb * 128:(kb + 1) * 128]), qs)
            S.actv(e, pS, AF.Exp, scale=0.125)
            if m >= 0:
                S.tt("dve", e, e, (maskB, maskB[:, m, :]), ALU.mult)

        s_and_exp(0)
        yield
        for i in range(nk):
            kb = kbs[i]
            e = st.e[i % 2]
            sp = st.sp[i % 2]
            w = st.w[i % 2]
            a = st.a[i % 2]
            first = (i == 0)
            last = (i == nk - 1)
            S.actv(sp, e, AF.Ln, bias=1.0)
            yield
            if not last:
                s_and_exp(i + 1)
            yield
            S.mm(st.pB, nti, sp, start=first, stop=False)
            dummies()
            yield
            S.actv(w, st.pB, AF.Exp)
            yield
            S.mm(st.pB, nsl, sp, start=False, stop=last)
            dummies()
            S.tt("dve", a, w, e, ALU.mult)
            yield
            S.mm((st.pO, st.pO[0:64, :]), (Vtok, Vtok[:, kb, hh * 64:(hh + 1) * 64]), a, start=first, stop=last)
            dummies()
            yield
        S.copy("act", (ybf, ybf[:, hh, qt * 512:(qt + 1) * 512]), (st.pO, st.pO[0:64, :]), disjoint=True)
        yield

    for hp in range(4):
        for hh in range(2):
            S.dma((qh, qh[:, hh, :]), g.projT[R_BQ + hp * 128 + hh * 64:R_BQ + hp * 128 + (hh + 1) * 64, :], disjoint=(hh == 1))
            S.dma((kh, kh[:, hh, :]), g.projT[R_BK + hp * 128 + hh * 64:R_BK + hp * 128 + (hh + 1) * 64, :], disjoint=(hh == 1))
        S.dma(vT, g.projT[R_BV + hp * 128:R_BV + (hp + 1) * 128, :], disjoint=False)
        to_tokmajor(S, vT, Vtok, NB, idb, pT, cnt)
        work = [(hh, qt) for qt in range(NQ - 1, -1, -1) for hh in range(2)]
        active = []
        free_sets = list(sets)
        while work or active:
            while work and free_sets:
                hh, qt = work.pop(0)
                st = free_sets.pop(0)
                active.append((chain(st, hh, qt), st))
            nxt = []
            for gen, st in active:
                try:
                    next(gen)
                    nxt.append((gen, st))
                except StopIteration:
                    free_sets.append(st)
            active = nxt
        for hh in range(2):
            r0 = Y_B + hp * 128 + hh * 64
            S.dma(g.yT[r0:r0 + 64, :], (ybf, ybf[:, hh, :]))
    S.end_phase()


def mixA_phase(S, g, l):
    T = g.T
    SEG = min(1024, T)
    NSEG = T // SEG
    NBS = SEG // 128
    NCH = SEG // 64
    S.begin_phase()
    idb = load_ident(S, g)
    ones, epsb = phase_consts(S, g)
    vec = load_vec(S, g, l)
    m2 = S.sb("m2", [128, 128], F32)
    S.dma(m2, g.cst[:, CST_M2:CST_M2 + 128], disjoint=False)
    oneb = S.sb("oneb", [128, 1], F32)
    S.memset("dve", oneb, 1.0)
    cmask = S.sb("cmask", [128, SEG], F32)
    S.memset("dve", cmask, 1.0)
    S.memset("dve", (cmask, cmask[:, 0:SEG:64]), 0.0)
    lbl = S.sb("lbl", [128, 4, LAYERS_A], F32)
    S.dma((lbl, lbl[:].rearrange("p a b -> p (a b)")), g.lbl, disjoint=False)
    le = S.sb("le", [128, 4, LAYERS_A], F32)
    S.actv(le, lbl, AF.Exp)
    lsum = S.sb("lsum", [128, 4], F32)
    S.tt("dve", lsum, (le, le[:, :, 0]), (le, le[:, :, 1]), ALU.add)
    for i in range(2, LAYERS_A):
        S.tt("dve", lsum, lsum, (le, le[:, :, i]), ALU.add)
    S.recip(lsum, lsum)
    lb = S.sb("lb", [128, 4], F32)
    oml = S.sb("oml", [128, 4], F32)
    S.memset("dve", lb, 0.0)
    for i in range(1, l + 1):
        S.tt("dve", lb, lb, (le, le[:, :, i]), ALU.add)
    S.tt("dve", lb, lb, lsum, ALU.mult)
    S.ts("dve", oml, lb, -1.0, 1.0, op0=ALU.mult, op1=ALU.add)
    qn = S.sb("qn", [128, SEG], BF16)
    fn = S.sb("fn", [128, SEG], BF16)
    vn = S.sb("vn", [128, SEG], BF16)
    khat = S.sb("khat", [128, SEG], BF16)
    t1 = S.sb("t1", [128, SEG], F32)
    t2 = S.sb("t2", [128, SEG], F32)
    t3 = S.sb("t3", [128, SEG], F32)
    t4 = S.sb("t4", [128, SEG], F32)
    bb = S.sb("bb", [128, SEG], F32)
    sqb = S.sb("sqb", [128, SEG], BF16)
    H = []
    for h in range(4):
        hs = G()
        hs.gn = S.sb("gn%d" % h, [128, SEG], BF16)
        hs.ebt = S.sb("ebt%d" % h, [128, SEG], F32)
        hs.qt = S.sb("qt%d" % h, [128, SEG], BF16)
        hs.kt = S.sb("kt%d" % h, [128, SEG], BF16)
        hs.qb = S.sb("qb%d" % h, [128, SEG], BF16)
        hs.Vt = S.sb("Vt%d" % h, [128, NBS, 128], BF16)
        hs.Kt = S.sb("Kt%d" % h, [128, NBS, 128], BF16)
        hs.oT = S.sb("oT%d" % h, [128, SEG], F32)
        hs.Sst = S.sb("Sst%d" % h, [128, 128], F32)
        hs.Sb = [S.sb("Sb%d_%d" % (h, i), [128, 128], BF16) for i in range(2)]
        hs.Pm = S.sb("Pm%d" % h, [128, 128], BF16)
        S.memset("dve", hs.Sst, 0.0)
        S.memset("dve", hs.Sb[0], 0.0)
        H.append(hs)
    pT = [S.ps("pT%d" % i, [128, 128], BF16) for i in range(2)]
    bsc = S.ps("bsc", [128, 4, 128], F32)
    bua = S.ps("bua", [128, 4, 128], F32)
    bub = S.ps("bub", [128, 4, 128], F32)
    bo = S.ps("bo", [128, 4, 128], F32)
    bss = S.ps("bss", [128, 512], F32)
    for h in range(4):
        H[h].psc = Buf("psc%d" % h, bsc.t, bsc.lock)
        H[h].pua = Buf("pua%d" % h, bua.t, bua.lock)
        H[h].pub = Buf("pub%d" % h, bub.t, bub.lock)
        H[h].po = Buf("po%d" % h, bo.t, bo.lock)
    cnt = [0]
    ysb = [S.sb("ysb%d" % i, [128, SEG], BF16) for i in range(2)]
    for seg in range(NSEG):
        cs = slice(seg * SEG, (seg + 1) * SEG)
        for h in range(4):
            hs = H[h]
            S.dma(qn, g.projT[R_AQ + h * 128:R_AQ + (h + 1) * 128, cs], disjoint=False)
            S.dma(fn, g.projT[R_AF + h * 128:R_AF + (h + 1) * 128, cs], disjoint=False)
            S.dma(vn, g.projT[R_AI + h * 128:R_AI + (h + 1) * 128, cs], disjoint=False)
            S.dma(hs.gn, g.projT[R_AG + h * 128:R_AG + (h + 1) * 128, cs], disjoint=False)
            S.actv(t4, fn, AF.Sigmoid)
            S.actv(t1, t4, AF.Identity, scale=(oml, oml[:, h:h + 1]), bias=(lb, lb[:, h:h + 1]))
            S.actv(t2, t1, AF.Ln)
            S.actv(t3, t1, AF.Identity, scale=-1.0, bias=(oneb, oneb[:, 0:1]))
            S.op("dve", "tensor_tensor_scan", dict(out=bb[:], data0=cmask[:], data1=t2[:], initial=0.0,
                                                   op0=ALU.mult, op1=ALU.add), reads=[cmask, t2], writes=[bb])
            bv = bb[:, :].rearrange("p (c i) -> p c i", i=64)
            bmid = bv[:, :, 31:32].to_broadcast([128, NCH, 64])
            blast = bv[:, :, 63:64].to_broadcast([128, NCH, 64])
            v3 = lambda b_: (b_, b_[:, :].rearrange("p (c i) -> p c i", i=64))
            S.tt("dve", v3(t2), (bb, bv), (bb, bmid), ALU.subtract)
            S.actv(t4, t2, AF.Exp)
            S.tt("dve", hs.qt, qn, t4, ALU.mult)
            S.actv(t4, t2, AF.Exp, scale=-1.0)
            S.tt("dve", hs.kt, t3, t4, ALU.mult)
            S.tt("dve", v3(t2), (bb, bv), (bb, blast), ALU.subtract)
            S.actv(t4, t2, AF.Exp, scale=-1.0)
            S.tt("dve", khat, t3, t4, ALU.mult)
            S.actv(hs.ebt, bb, AF.Exp)
            S.tt("dve", hs.qb, qn, hs.ebt, ALU.mult)
            if g.dbg is not None and h == 0 and seg == 0:
                S.dma(g.dbg[0], t1); S.dma(g.dbg[1], bb); S.dma(g.dbg[2], hs.ebt); S.dma(g.dbg[3], t3)
                S.copy("dve", t4, hs.qt); S.dma(g.dbg[4], t4)
            to_tokmajor(S, vn, hs.Vt, NBS, idb, pT, cnt)
            to_tokmajor(S, khat, hs.Kt, NBS, idb, pT, cnt)
        for n in range(NBS):
            bs = slice(n * 128, (n + 1) * 128)
            for h in range(4):
                hs = H[h]
                S.mm((hs.psc, bsc[:, h, :]), (hs.kt, hs.kt[:, bs]), (hs.qt, hs.qt[:, bs]))
                S.mm((hs.pua, bua[0:128, h, :]), (hs.Kt, hs.Kt[0:64, n, :]), (hs.Vt, hs.Vt[0:64, n, :]))
                S.mm((hs.pub, bub[0:128, h, :]), (hs.Kt, hs.Kt[64:128, n, :]), (hs.Vt, hs.Vt[64:128, n, :]))
            for h in range(4):
                hs = H[h]
                S.tt("dve", hs.Pm, (hs.psc, bsc[:, h, :]), m2, ALU.mult)
                cA = (2 * n) * 64 + 63
                S.stt(hs.Sst, hs.Sst, (hs.ebt, hs.ebt[:, cA:cA + 1]), (hs.pua, bua[:, h, :]), ALU.mult, ALU.add)
                S.copy("act", hs.Sb[1], hs.Sst)
            for h in range(4):
                hs = H[h]
                S.mm((hs.po, bo[:, h, :]), (hs.Vt, hs.Vt[:, n, :]), hs.Pm, start=True, stop=False)
                S.mm((hs.po, bo[:, h, 0:64]), hs.Sb[0], (hs.qb, hs.qb[:, n * 128:n * 128 + 64]), start=False, stop=False)
                S.mm((hs.po, bo[:, h, 64:128]), hs.Sb[1], (hs.qb, hs.qb[:, n * 128 + 64:(n + 1) * 128]), start=False, stop=True)
            for h in range(4):
                hs = H[h]
                cB = (2 * n + 1) * 64 + 63
                S.stt(hs.Sst, hs.Sst, (hs.ebt, hs.ebt[:, cB:cB + 1]), (hs.pub, bub[:, h, :]), ALU.mult, ALU.add)
                S.copy("act", hs.Sb[0], hs.Sst)
                S.copy("act", (hs.oT, hs.oT[:, bs]), (hs.po, bo[:, h, :]), disjoint=True)
        for h in range(4):
            hs = H[h]
            yb = ysb[h % 2]
            for c in range(SEG // 512):
                c5 = slice(c * 512, (c + 1) * 512)
                S.actv((sqb, sqb[:, c5]), (hs.oT, hs.oT[:, c5]), AF.Square)
                S.mm(bss, ones, (sqb, sqb[:, c5]))
                S.actv((t1, t1[:, c5]), bss, AF.Ln, scale=1.0 / 128, bias=(epsb, epsb[:, 0:1]))
            S.actv(t2, t1, AF.Exp, scale=-0.5)
            if g.dbg is not None and h == 0 and seg == 0:
                S.dma(g.dbg[5], hs.oT); S.dma(g.dbg[6], t2)
            S.stt(t3, hs.oT, (vec, vec[:, 96:97]), t2, ALU.mult, ALU.mult)
            S.actv(t4, hs.gn, AF.Silu)
            S.tt("dve", yb, t3, t4, ALU.mult)
            S.dma(g.yT[Y_A + h * 128:Y_A + (h + 1) * 128, cs], yb)
    S.end_phase()


LAYERS = 4


def build(T, L, phases=None, debug=False, ext_in=()):
    nc = bass.Bass("TRN2", target_bir_lowering=False)
    g = G()
    g.nc = nc
    g.T = T
    g.L = L

    def din(name, shape, dt=F32):
        return nc.dram_tensor(name, list(shape), dt, kind="ExternalInput").ap()

    g.xT = din("xT", [D, T])
    g.w13t = [din("w13t_a", [L, 88, 128, 2048]), din("w13t_b", [L, 88, 128, 2048])]
    g.w2t = [din("w2t_a", [L, 16, 128, FF]), din("w2t_b", [L, 16, 128, FF])]
    g.wint = din("wint", [L, 116, 128, 2048])
    g.wbt = din("wbt", [L, 16, 128, 1792])
    g.wot = din("wot", [L, 16, 128, 2048])
    g.vecs = din("vecs", [L, 128, NVEC])
    g.lbl = din("lbl", [128, 4 * LAYERS])
    g.sinks = din("sinks", [128, LAYERS * 8])
    g.relb = din("relb", [128, 640])
    g.cst = din("cst", [128, NCST])
    g.outT = nc.dram_tensor("outT", [D, T], F32, kind="ExternalOutput").ap()
    sk = "ExternalOutput" if debug else "Internal"
    def scr(name, shape, dt):
        return nc.dram_tensor(name, shape, dt, kind=("ExternalInput" if name in ext_in else sk)).ap()
    g.projT = scr("projT", [6656, T], BF16)
    g.uT = scr("uT_s", [D, T], BF16)
    g.yT = scr("yT_s", [1792, T], BF16)
    g.ebD = nc.dram_tensor("ebD", [128, 2 * 8 * 128], F32, kind=sk).ap()
    g.ebC = nc.dram_tensor("ebC", [128, 3 * 2 * 2 * 2 * 128], F32, kind=sk).ap()
    g.lbs = nc.dram_tensor("lbs", [128, 4 * LAYERS], F32, kind=sk).ap()
    g.dbg = nc.dram_tensor('dbg', [16, 128, 1024], F32, kind='ExternalOutput').ap() if debug else None
    S = Sched(nc)
    g.S = S
    allp = ["ffn1", "inproj", "A", "B", "C", "D", "merge", "ffn2"]
    if phases is None:
        phases = allp
    if any(p in phases for p in ("A", "C", "D")):
        setup_phase(S, g)
    for l in range(L):
        first = (l == 0)
        for p in phases:
            if p == "ffn1":
                ffn_phase(S, g, l, 0, g.xT if first else g.outT, g.outT)
            elif p == "inproj":
                inproj_phase(S, g, l, g.outT)
            elif p == "A":
                mixA_phase(S, g, l)
            elif p == "B":
                mixB_phase(S, g, l)
            elif p == "C":
                mixC_phase(S, g, l)
            elif p == "D":
                mixD_phase(S, g, l)
            elif p == "merge":
                merge_phase(S, g, l, g.outT)
            elif p == "ffn2":
                ffn_phase(S, g, l, 1, g.outT, g.outT)
    return nc, g


def tile_w(w, kc, nc_):
    return np.ascontiguousarray(w.reshape(kc, 128, nc_, 128).transpose(2, 1, 0, 3).reshape(nc_, 128, kc * 128))


def prep_weights(inp, L):
    out = {}
    out["w13t_a"] = np.stack([tile_w(inp["ffn1_w13"][l], 16, 88) for l in range(L)])
    out["w13t_b"] = np.stack([tile_w(inp["ffn2_w13"][l], 16, 88) for l in range(L)])
    out["w2t_a"] = np.stack([tile_w(inp["ffn1_w2"][l], 44, 16) for l in range(L)])
    out["w2t_b"] = np.stack([tile_w(inp["ffn2_w2"][l], 44, 16) for l in range(L)])
    out["wint"] = np.stack([tile_w(inp["w_in"][l], 16, 116) for l in range(L)])
    out["wbt"] = np.stack([tile_w(inp["w_branch"][l], 14, 16) for l in range(L)])
    out["wot"] = np.stack([tile_w(inp["w_out"][l], 16, 16) for l in range(L)])
    vecs = np.zeros((L, 128, NVEC), np.float32)
    for l in range(L):
        for i, nm in enumerate(("ffn1_norm", "mix_norm", "ffn2_norm")):
            for j in range(2):
                vecs[l, :, (2 * i + j) * 16:(2 * i + j + 1) * 16] = inp[nm][l, j].reshape(16, 128).T
        vecs[l, :, 96] = inp["hgrn_out_norm"][l]
    out["vecs"] = vecs
    lb = np.asarray(inp["hgrn_lb_logits"])
    out["lbl"] = np.ascontiguousarray(lb.reshape(LAYERS, 4, 128).transpose(2, 1, 0).reshape(128, 4 * LAYERS))
    out["sinks"] = np.ascontiguousarray(np.broadcast_to(np.asarray(inp["attn_sinks"]).reshape(1, -1), (128, LAYERS * 8)))
    out["relb"] = np.ascontiguousarray(np.broadcast_to(np.asarray(inp["rel_bias"]).reshape(1, 640), (128, 640)))
    out["cst"] = make_consts()
    return out


_CACHE = {}


def kernel(**inputs):
    x = np.asarray(inputs["x"])
    B, T, _ = x.shape
    L = LAYERS
    key = (T, L)
    if key not in _CACHE:
        _CACHE[key] = build(T, L, None, debug=False)
    nc, g = _CACHE[key]
    w = prep_weights({k: np.asarray(v) for k, v in inputs.items() if k != "x"}, L)
    in_maps = []
    for b in range(B):
        m = dict(w)
        m["xT"] = np.ascontiguousarray(x[b].T)
        in_maps.append(m)
    res = run_bass_kernel_spmd(nc, in_maps, core_ids=list(range(B)))
    out = np.stack([np.ascontiguousarray(res.results[b]["outT"].T) for b in range(B)], axis=0)
    return out.astype(np.float32, copy=False)
```
